# Optimizing a Trainium2 kernel written in Bass

```python
import math
import jax, jax.numpy as jnp
from jax import lax
import numpy as np

D_MODEL = 1024
BATCH = 4
SEQ = 4096
DEPTH = 2

HEAD_DIM = 64
N_Q_HEADS = D_MODEL // HEAD_DIM
GQA_GROUP = 4
N_KV_HEADS = N_Q_HEADS // GQA_GROUP
WINDOW = 128
BLOCK = 128
NUM_BUCKETS = 32
MAX_DISTANCE = 128
ATTN_IN = (N_Q_HEADS + 2 * N_KV_HEADS) * HEAD_DIM
MLSTM_HEADS = 8
MLSTM_DV = D_MODEL // MLSTM_HEADS
MLSTM_DQK = MLSTM_DV // 2
MLSTM_CHUNK = 64
MLSTM_IN = 2 * MLSTM_HEADS * MLSTM_DQK + 2 * MLSTM_HEADS * MLSTM_DV + 4 * MLSTM_HEADS
N_EXPERTS = 16
CAPACITY_FACTOR = 2
D_EXPERT = 2 * D_MODEL
N_MIXERS = 2
N_ATTN_LAYERS = (DEPTH + 1) // 2
N_MLSTM_LAYERS = DEPTH // 2
RMS_EPS = 1e-6

kernel_name = "hybrid_swa_mlstm_ecmoe_encoder"


def rms_norm(x, g):
    xf = x.astype(jnp.float32)
    y = xf * lax.rsqrt(jnp.mean(xf * xf, axis=-1, keepdims=True) + RMS_EPS)
    return (y * g.astype(jnp.float32)).astype(x.dtype)


def t5_bucket(rel):
    nb = NUM_BUCKETS // 2
    ret = (rel > 0).astype(jnp.int32) * nb
    n = jnp.abs(rel)
    max_exact = nb // 2
    nf = jnp.maximum(n, 1).astype(jnp.float32)
    large = max_exact + (jnp.log(nf / max_exact) / math.log(MAX_DISTANCE / max_exact)
                         * (nb - max_exact)).astype(jnp.int32)
    large = jnp.minimum(large, nb - 1)
    return ret + jnp.where(n < max_exact, n, large)


def windowed_gqa(h, w_in, q_g, k_g, sink, w_out, rel_bias):
    B, S, _ = h.shape
    nb = S // BLOCK
    proj = h @ w_in
    q = proj[..., :N_Q_HEADS * HEAD_DIM].reshape(B, S, N_KV_HEADS, GQA_GROUP, HEAD_DIM)
    k = proj[..., N_Q_HEADS * HEAD_DIM:(N_Q_HEADS + N_KV_HEADS) * HEAD_DIM].reshape(B, S, N_KV_HEADS, HEAD_DIM)
    v = proj[..., (N_Q_HEADS + N_KV_HEADS) * HEAD_DIM:].reshape(B, S, N_KV_HEADS, HEAD_DIM)
    q = rms_norm(q, q_g) * (HEAD_DIM ** -0.5)
    k = rms_norm(k, k_g)
    qb = q.reshape(B, nb, BLOCK, N_KV_HEADS, GQA_GROUP, HEAD_DIM)

    def band(t):
        tp = jnp.pad(t, ((0, 0), (BLOCK, BLOCK), (0, 0), (0, 0))).reshape(B, nb + 2, BLOCK, N_KV_HEADS, HEAD_DIM)
        return jnp.concatenate([tp[:, :-2], tp[:, 1:-1], tp[:, 2:]], axis=2)

    kw, vw = band(k), band(v)
    qq = jnp.arange(BLOCK)[:, None]
    kk = jnp.arange(3 * BLOCK)[None, :]
    rel = kk - BLOCK - qq
    bias = rel_bias.astype(jnp.float32)[t5_bucket(rel)]
    bias = bias.transpose(2, 0, 1).reshape(N_KV_HEADS, GQA_GROUP, BLOCK, 3 * BLOCK)
    kpos = jnp.arange(nb)[:, None] * BLOCK + jnp.arange(3 * BLOCK)[None, :] - BLOCK
    mask = (jnp.abs(rel) <= WINDOW)[None] & ((kpos >= 0) & (kpos < S))[:, None, :]

    s = jnp.einsum('bnqhgd,bnkhd->bnhgqk', qb, kw).astype(jnp.float32) + bias
    s = jnp.where(mask[None, :, None, None], s, -jnp.inf)
    sk = sink.astype(jnp.float32).reshape(1, 1, N_KV_HEADS, GQA_GROUP, 1)
    m = jnp.maximum(jnp.max(s, axis=-1), sk)
    p = jnp.exp(s - m[..., None])
    den = jnp.sum(p, axis=-1) + jnp.exp(sk - m)
    p = p / den[..., None]
    o = jnp.einsum('bnhgqk,bnkhd->bnqhgd', p, vw.astype(jnp.float32))
    o = o.reshape(B, S, N_Q_HEADS * HEAD_DIM).astype(h.dtype)
    return o @ w_out


def mlstm_chunk_scan(q, k, v, ig, lf):
    B, H, S, dqk = q.shape
    dv = v.shape[-1]
    nc = S // MLSTM_CHUNK
    L = MLSTM_CHUNK

    def chunks(t):
        t = t.reshape((B, H, nc, L) + t.shape[3:])
        return jnp.moveaxis(t, 2, 0)

    tril = jnp.tril(jnp.ones((L, L), dtype=bool))

    def step(carry, inp):
        C, n, m = carry
        qc, kc, vc, ic, fc = inp
        b = jnp.cumsum(fc, axis=-1)
        Dm = jnp.where(tril, b[..., :, None] - b[..., None, :] + ic[..., None, :], -jnp.inf)
        a = b + m[..., None]
        mt = jnp.maximum(a, jnp.max(Dm, axis=-1))
        W = jnp.exp(Dm - mt[..., None])
        ea = jnp.exp(a - mt)
        Sw = W * jnp.einsum('bhld,bhsd->bhls', qc, kc)
        num = ea[..., None] * jnp.einsum('bhld,bhde->bhle', qc, C) + jnp.einsum('bhls,bhse->bhle', Sw, vc)
        den = ea * jnp.einsum('bhld,bhd->bhl', qc, n) + jnp.sum(Sw, axis=-1)
        hc = num / jnp.maximum(jnp.abs(den), jnp.exp(-mt))[..., None]
        bL = b[..., -1]
        g = bL[..., None] - b + ic
        m_new = jnp.maximum(bL + m, jnp.max(g, axis=-1))
        wC = jnp.exp(g - m_new[..., None])
        decay = jnp.exp(bL + m - m_new)
        C_new = decay[..., None, None] * C + jnp.einsum('bhs,bhsd,bhse->bhde', wC, kc, vc)
        n_new = decay[..., None] * n + jnp.einsum('bhs,bhsd->bhd', wC, kc)
        return (C_new, n_new, m_new), hc

    init = (jnp.zeros((B, H, dqk, dv), jnp.float32), jnp.zeros((B, H, dqk), jnp.float32),
            jnp.zeros((B, H), jnp.float32))
    _, hs = lax.scan(step, init, (chunks(q), chunks(k), chunks(v), chunks(ig), chunks(lf)))
    return jnp.moveaxis(hs, 0, 2).reshape(B, H, S, dv)


def bidir_mlstm(h, w_in, b_i, b_f, out_g, w_out):
    B, S, _ = h.shape
    proj = (h @ w_in).astype(jnp.float32)
    o1 = MLSTM_HEADS * MLSTM_DQK
    o2 = 2 * o1
    o3 = o2 + MLSTM_HEADS * MLSTM_DV
    o4 = o3 + MLSTM_HEADS * MLSTM_DV
    heads = lambda t, d: t.reshape(B, S, MLSTM_HEADS, d).transpose(0, 2, 1, 3)
    q = heads(proj[..., :o1], MLSTM_DQK)
    k = heads(proj[..., o1:o2], MLSTM_DQK) * (MLSTM_DQK ** -0.5)
    v = heads(proj[..., o2:o3], MLSTM_DV)
    og = proj[..., o3:o4]
    gates = proj[..., o4:].reshape(B, S, 4, MLSTM_HEADS).transpose(2, 0, 3, 1)
    bi = b_i.astype(jnp.float32)[:, None, :, None]
    bf = b_f.astype(jnp.float32)[:, None, :, None]
    ig_f, ig_b = gates[0] + bi[0], gates[2] + bi[1]
    lf_f, lf_b = jax.nn.log_sigmoid(gates[1] + bf[0]), jax.nn.log_sigmoid(gates[3] + bf[1])
    h_f = mlstm_chunk_scan(q, k, v, ig_f, lf_f)
    fl = lambda t: jnp.flip(t, axis=2)
    h_b = fl(mlstm_chunk_scan(fl(q), fl(k), fl(v), fl(ig_b), fl(lf_b)))
    hs = (h_f + h_b).transpose(0, 2, 1, 3)
    hs = rms_norm(hs, out_g.reshape(MLSTM_HEADS, MLSTM_DV)).reshape(B, S, MLSTM_HEADS * MLSTM_DV)
    y = (hs * jax.nn.sigmoid(og)).astype(h.dtype)
    return y @ w_out


def expert_choice_moe(h, w_r, w1, w3, w2):
    B, T, D = h.shape
    cap = CAPACITY_FACTOR * T // N_EXPERTS
    aff = jax.nn.softmax(jnp.einsum('btd,de->bte', h, w_r).astype(jnp.float32), axis=-1)
    g, idx = lax.top_k(jnp.swapaxes(aff, 1, 2), cap)
    xs = jax.vmap(lambda hb, ib: hb[ib])(h, idx)
    a = jnp.einsum('becd,edf->becf', xs, w1)
    u = jnp.einsum('becd,edf->becf', xs, w3)
    y = jnp.einsum('becf,efd->becd', jax.nn.silu(a) * u, w2)
    y = y * g[..., None].astype(y.dtype)
    return jax.vmap(lambda ib, yb: jnp.zeros((T, D), yb.dtype).at[ib.reshape(-1)].add(yb.reshape(-1, D)))(idx, y)


def setup_inputs(seed: int = 0) -> dict:
    key = jax.random.key(seed)
    ks = jax.random.split(key, 20)
    nrm = lambda k, shape, scale: jax.random.normal(k, shape, jnp.float32) * scale
    NA, NM, D = N_ATTN_LAYERS, N_MLSTM_LAYERS, D_MODEL
    return {
        "x": nrm(ks[0], (BATCH, SEQ, D), 1.0),
        "rel_bias": nrm(ks[1], (NUM_BUCKETS, N_Q_HEADS), 0.5),
        "attn_norm_g": 1.0 + nrm(ks[2], (NA, D), 0.02),
        "attn_w_in": nrm(ks[3], (NA, D, ATTN_IN), D ** -0.5),
        "attn_q_norm_g": 1.0 + nrm(ks[4], (NA, HEAD_DIM), 0.02),
        "attn_k_norm_g": 1.0 + nrm(ks[5], (NA, HEAD_DIM), 0.02),
        "attn_sink": nrm(ks[6], (NA, N_Q_HEADS), 0.5),
        "attn_w_out": nrm(ks[7], (NA, N_Q_HEADS * HEAD_DIM, D), (N_Q_HEADS * HEAD_DIM) ** -0.5),
        "mlstm_norm_g": 1.0 + nrm(ks[8], (NM, D), 0.02),
        "mlstm_w_in": nrm(ks[9], (NM, D, MLSTM_IN), D ** -0.5),
        "mlstm_b_i": nrm(ks[10], (NM, 2, MLSTM_HEADS), 0.1),
        "mlstm_b_f": jnp.linspace(3.0, 6.0, MLSTM_HEADS, dtype=jnp.float32)[None, None]
                      + nrm(ks[11], (NM, 2, MLSTM_HEADS), 0.1),
        "mlstm_out_norm_g": 1.0 + nrm(ks[12], (NM, MLSTM_HEADS * MLSTM_DV), 0.02),
        "mlstm_w_out": nrm(ks[13], (NM, MLSTM_HEADS * MLSTM_DV, D), (MLSTM_HEADS * MLSTM_DV) ** -0.5),
        "ffn_norm_g": 1.0 + nrm(ks[14], (DEPTH, D), 0.02),
        "router_w": nrm(ks[15], (DEPTH, D, N_EXPERTS), D ** -0.5),
        "expert_w1": nrm(ks[16], (DEPTH, N_EXPERTS, D, D_EXPERT), D ** -0.5),
        "expert_w3": nrm(ks[17], (DEPTH, N_EXPERTS, D, D_EXPERT), D ** -0.5),
        "expert_w2": nrm(ks[18], (DEPTH, N_EXPERTS, D_EXPERT, D), D_EXPERT ** -0.5),
    }


def reference(x, rel_bias, attn_norm_g, attn_w_in, attn_q_norm_g, attn_k_norm_g, attn_sink, attn_w_out,
              mlstm_norm_g, mlstm_w_in, mlstm_b_i, mlstm_b_f, mlstm_out_norm_g, mlstm_w_out,
              ffn_norm_g, router_w, expert_w1, expert_w3, expert_w2):
    for i in range(DEPTH):
        j = i // N_MIXERS
        if i % N_MIXERS == 0:
            x = x + windowed_gqa(rms_norm(x, attn_norm_g[j]), attn_w_in[j], attn_q_norm_g[j],
                                 attn_k_norm_g[j], attn_sink[j], attn_w_out[j], rel_bias)
        else:
            x = x + bidir_mlstm(rms_norm(x, mlstm_norm_g[j]), mlstm_w_in[j], mlstm_b_i[j], mlstm_b_f[j],
                                mlstm_out_norm_g[j], mlstm_w_out[j])
        x = x + expert_choice_moe(rms_norm(x, ffn_norm_g[i]), router_w[i], expert_w1[i], expert_w3[i],
                                  expert_w2[i])
    return x
```

```python
import math
from contextlib import ExitStack

import numpy as np
import concourse.bass as bass
import concourse.mybir as mybir
from concourse.bass_utils import run_bass_kernel_spmd

F32 = mybir.dt.float32
BF16 = mybir.dt.bfloat16
I32 = mybir.dt.int32
ALU = mybir.AluOpType
AF = mybir.ActivationFunctionType
AX = mybir.AxisListType

ENGS = ["pe", "dve", "act", "pool", "sp"]

S = 4096
D = 1024
NT = S // 128
KC = D // 128
NEG = -30000.0
EPS = 1e-6


class Buf:
    __slots__ = ("name", "w", "r")

    def __init__(self, name=""):
        self.name = name
        self.w = None
        self.r = []


class KB:
    N_DMA_SEMS = 56
    N_HW_SEMS = 40

    def __init__(self, nc, stack):
        self.nc = nc
        self.stack = stack
        self.handles = {"pe": nc.tensor, "dve": nc.vector, "act": nc.scalar,
                        "pool": nc.gpsimd, "sp": nc.sync}
        self.prog = {e: [] for e in ENGS}
        self.epoch = 0
        self.sems = {}
        self.count = {}
        self._new_epoch_sems()
        self.dma_sems = [stack.enter_context(nc.semaphore("d%d" % i)) for i in range(self.N_DMA_SEMS)]
        self.dma_val = [0] * self.N_DMA_SEMS
        self.dma_next = 0
        self.dma_next_sw = 0
        self.seen = {e: {} for e in ENGS}
        self.n_inst = 0
        self.n_wait = 0
        self._rec = None

    def _new_epoch_sems(self):
        for e in ["pe", "dve", "act", "pool"]:
            self.sems[(e, self.epoch)] = self.stack.enter_context(self.nc.semaphore("s_%s_%d" % (e, self.epoch)))
            self.count[e] = 0

    def _sem_of(self, key):
        return self.sems[key] if isinstance(key, tuple) else self.dma_sems[key]

    def _wait(self, eng, toks):
        need = {}
        for t in toks:
            if t is None:
                continue
            k, v = t
            if isinstance(k, tuple):
                if k[1] < self.epoch:
                    continue
                if k[0] == "pe" and eng == "pe":
                    continue
            if need.get(k, 0) < v:
                need[k] = v
        for k, v in need.items():
            if self.seen[eng].get(k, 0) >= v:
                continue
            self.seen[eng][k] = v
            self.prog[eng].append(("wait", self._sem_of(k), v))
            self.n_wait += 1

    def _deps(self, eng, reads, writes):
        toks = []
        for b in reads:
            toks.append(b.w)
        for b in writes:
            toks.append(b.w)
            for t in b.r:
                if isinstance(t[0], tuple) and t[0][0] == eng:
                    continue
                toks.append(t)
        return toks

    def _commit(self, tok, reads, writes):
        for b in reads:
            b.r.append(tok)
            if len(b.r) > 64:
                best = {}
                for k, v in b.r:
                    if best.get(k, 0) < v:
                        best[k] = v
                b.r = list(best.items())
        for b in writes:
            b.w = tok
            b.r = []
        self.n_inst += 1

    def begin_record(self):
        self._rec = [[]]

    def step(self):
        if self._rec is not None and self._rec[-1]:
            self._rec.append([])

    def end_record(self):
        r, self._rec = self._rec, None
        return [st_ for st_ in r if st_]

    def replay(self, step_):
        for (kind, eng, fn, reads, writes) in step_:
            (self.op if kind == "op" else self.dma)(eng, fn, reads, writes)

    def replay_merged(self, a, b):
        ia = ib = 0
        while ia < len(a) or ib < len(b):
            if ib >= len(b) or (ia < len(a) and ia * len(b) <= ib * len(a)):
                self.replay(a[ia]); ia += 1
            else:
                self.replay(b[ib]); ib += 1

    def op(self, eng, fn, reads=(), writes=()):
        if self._rec is not None:
            self._rec[-1].append(("op", eng, fn, list(reads), list(writes)))
            return None
        self._wait(eng, self._deps(eng, reads, writes))
        self.count[eng] += 1
        key = (eng, self.epoch)
        tok = (key, self.count[eng])
        self.prog[eng].append(("op", fn, self.sems[key], 1))
        self._commit(tok, reads, writes)
        return tok

    def dma(self, eng, fn, reads=(), writes=()):
        if self._rec is not None:
            self._rec[-1].append(("dma", eng, fn, list(reads), list(writes)))
            return None
        if eng == "pool":
            j = self.N_HW_SEMS + self.dma_next_sw
            self.dma_next_sw = (self.dma_next_sw + 1) % (self.N_DMA_SEMS - self.N_HW_SEMS)
        else:
            j = self.dma_next
            self.dma_next = (j + 1) % self.N_HW_SEMS
        toks = self._deps("dma", reads, writes)
        if self.dma_val[j] > 0:
            toks.append((j, self.dma_val[j]))
        self._wait(eng, toks)
        self.dma_val[j] += 16
        tok = (j, self.dma_val[j])
        self.prog[eng].append(("op", fn, self.dma_sems[j], 16))
        self._commit(tok, reads, writes)
        return tok

    def wait_bufs(self, eng, bufs):
        toks = []
        for b in bufs:
            toks.append(b.w)
            toks.extend(b.r)
        self._wait(eng, toks)

    def barrier(self):
        toks = [((e, self.epoch), self.count[e]) for e in ["pe", "dve", "act", "pool"] if self.count[e] > 0]
        toks += [(j, v) for j, v in enumerate(self.dma_val) if v > 0]
        for e in ENGS:
            self._wait(e, [t for t in toks if not (isinstance(t[0], tuple) and t[0][0] == e)])
        self.epoch += 1
        self._new_epoch_sems()

    def emit(self):
        nc = self.nc
        prog = self.prog

        def run(e):
            def body(h):
                for item in prog[e]:
                    if item[0] == "wait":
                        h.wait_ge(item[1], item[2])
                    else:
                        item[1](h).then_inc(item[2], item[3])
            return body

        with nc.Block() as block:
            block.tensor(run("pe"))
            block.vector(run("dve"))
            block.scalar(run("act"))
            block.gpsimd(run("pool"))
            block.sync(run("sp"))


class Ctx:
    def __init__(self, nc, stack):
        self.nc = nc
        self.k = KB(nc, stack)
        self.uid = 0

    def sb(self, st, shape, dt, name=None):
        self.uid += 1
        t = st.enter_context(self.nc.sbuf_tensor("%s_%d" % (name or "t", self.uid), list(shape), dt))
        return t

    def ps(self, st, shape, dt, name=None):
        self.uid += 1
        t = st.enter_context(self.nc.psum_tensor("%s_%d" % (name or "p", self.uid), list(shape), dt))
        return t


def stage_attn(cx, XIN, XOUT, Bxin, Bxout, P):
    nc, k = cx.nc, cx.k
    with ExitStack() as st:
        sb = lambda shape, dt, name=None: cx.sb(st, shape, dt, name)
        ps = lambda shape, dt, name=None: cx.ps(st, shape, dt, name)
        w_in = sb([128, KC, 1536], BF16, "w_in")
        w_out = sb([128, KC, 1024], BF16, "w_out")
        g_bc = sb([128, D], F32, "g_bc")
        ident = sb([128, 128], BF16, "ident")
        BM = sb([128, 3, 16, 128], F32, "BM")
        gqk = sb([128, 20, 64], F32, "gqk")
        esink = sb([128, 16], F32, "esink")
        eps_t = sb([128, 1], F32, "eps")
        V_all = sb([128, NT, 4, 65], BF16, "V_all")
        kT_all = sb([64, 4, S], BF16, "kT_all")
        xt = [sb([128, D], F32, "xt%d" % i) for i in range(3)]
        sq = sb([128, 1280], F32, "sq")
        junk = sb([128, D], BF16, "junk")
        xn = sb([128, D], BF16, "xn")
        xnT = sb([128, KC, 128], BF16, "xnT")
        tmpq = sb([128, 20, 64], F32, "tmpq")
        qkn = sb([128, 20, 64], BF16, "qkn")
        qT = [sb([64, 16, 128], BF16, "qT%d" % i) for i in range(3)]
        tS = [sb([128, 512], F32, "tS%d" % i) for i in range(2)]
        PT = [sb([128, 512], BF16, "PT%d" % i) for i in range(2)]
        ot = sb([128, 16, 64], BF16, "ot")
        otT = sb([128, KC, 128], BF16, "otT")
        x1t = [sb([128, D], F32, "x1t%d" % i) for i in range(2)]
        ss = sb([128, 1], F32, "ss")
        rstd = sb([128, 1], F32, "rstd")
        ssq = sb([128, 20], F32, "ssq")
        rqk = sb([128, 20], F32, "rqk")
        den = sb([128, 4], F32, "den")
        rden = sb([128, 4], F32, "rden")
        pA = ps([128, KC, 128], BF16, "pA")
        pB = ps([128, 512], F32, "pB")
        pC = ps([128, 512], F32, "pC")
        pD = ps([128, 512], F32, "pD")
        pE = ps([64, 8, 128], BF16, "pE")
        pGs = [ps([128, 512], F32, "pG%d" % i) for i in range(2)]
        pH = ps([128, 4, 65], F32, "pH")

        B = {}
        for n in ["w_in", "w_out", "g_bc", "ident", "BM", "gqk", "esink", "eps", "V1", "sq", "junk",
                  "xn", "xnT", "tmpq", "qkn", "ot", "otT", "ss", "rstd", "ssq", "rqk", "den", "rden",
                  "pA", "pB", "pC", "pD", "pE", "pH", "gq_s", "gk_s", "mask_s", "sink_s"]:
            B[n] = Buf(n)
        Bxt = [Buf("xt%d" % i) for i in range(3)]
        BqT = [Buf("qT%d" % i) for i in range(3)]
        BtS = [Buf() for _ in range(2)]
        BpG = [Buf() for _ in range(2)]
        BPT = [Buf() for _ in range(2)]
        Bx1 = [Buf() for _ in range(2)]
        BV = [Buf("V%d" % i) for i in range(NT)]
        BkT = [Buf("kT%d" % i) for i in range(NT)]

        k.dma("pool", lambda h: h.dma_start(out=w_in[:], in_=P["attn_w_in"].rearrange("(kc p) n -> p kc n", p=128)),
              writes=[B["w_in"]])
        k.dma("pool", lambda h: h.dma_start(out=w_out[:], in_=P["attn_w_out"].rearrange("(kc p) n -> p kc n", p=128)),
              writes=[B["w_out"]])
        k.dma("pool", lambda h: h.dma_start(out=ident[:], in_=P["ident"]), writes=[B["ident"]])
        k.dma("sp", lambda h: h.dma_start(out=g_bc[:], in_=P["attn_norm_g"].to_broadcast([128, D])), writes=[B["g_bc"]])
        k.dma("sp", lambda h: h.dma_start(out=BM[:], in_=P["attn_bias"]), writes=[B["BM"]])
        mstage = [xt[0], xt[1], x1t[0], x1t[1], xt[2], sq]
        for j in range(3):
            for hh in range(2):
                buf = mstage[j * 2 + hh]
                bb = Buf()
                k.dma("sp", lambda h, j=j, hh=hh, buf=buf: h.dma_start(
                    out=buf[:, 0:1024], in_=P["attn_mask"][:, j, hh * 8:(hh + 1) * 8, :].rearrange("p h q -> p (h q)")),
                    writes=[bb])
                k.op("dve", lambda h, j=j, hh=hh, buf=buf: h.tensor_tensor(
                    out=BM[:, j, hh * 8:(hh + 1) * 8, :], in0=BM[:, j, hh * 8:(hh + 1) * 8, :],
                    in1=buf[:, 0:1024].rearrange("p (h q) -> p h q", q=128), op=ALU.add),
                    reads=[bb], writes=[B["BM"]])
                for tb in (Bxt + Bx1 + [B["sq"]]):
                    pass
        for tb in Bxt + Bx1 + [B["sq"]]:
            tb.r.append(B["BM"].w)
        gq_s = sb([128, 64], F32, "gq_s")
        gk_s = sb([128, 64], F32, "gk_s")
        k.dma("sp", lambda h: h.dma_start(out=gq_s[:], in_=P["attn_q_norm_g"].to_broadcast([128, 64])), writes=[B["gq_s"]])
        k.dma("sp", lambda h: h.dma_start(out=gk_s[:], in_=P["attn_k_norm_g"].to_broadcast([128, 64])), writes=[B["gk_s"]])
        k.op("dve", lambda h: h.tensor_scalar(out=gqk[:, 0:16, :], in0=gq_s[:, :].unsqueeze(1).to_broadcast([128, 16, 64]),
                                              scalar1=0.125, scalar2=None, op0=ALU.mult),
             reads=[B["gq_s"]], writes=[B["gqk"]])
        k.op("dve", lambda h: h.tensor_copy(out=gqk[:, 16:20, :], in_=gk_s[:, :].unsqueeze(1).to_broadcast([128, 4, 64])),
             reads=[B["gk_s"]], writes=[B["gqk"]])
        k.dma("sp", lambda h: h.dma_start(out=esink[:], in_=P["attn_sink"].to_broadcast([128, 16])), writes=[B["esink"]])
        k.op("act", lambda h: h.activation(out=esink[:], in_=esink[:], func=AF.Exp), reads=[B["esink"]], writes=[B["esink"]])
        k.op("dve", lambda h: h.memset(eps_t[:], EPS), writes=[B["eps"]])
        k.op("pool", lambda h: h.memset(V_all[:, :, :, 64:65], 1.0), writes=[B["V1"]])

        def phase1(i):
            s3 = i % 3
            x_t, bx = xt[s3], Bxt[s3]
            k.dma("sp", lambda h: h.dma_start(out=x_t[:], in_=XIN[i * 128:(i + 1) * 128, :]), reads=[Bxin], writes=[bx])
            k.op("act", lambda h: h.activation(out=junk[:], in_=x_t[:], func=AF.Square, accum_out=ss[:]),
                 reads=[bx], writes=[B["junk"], B["ss"]])
            k.op("act", lambda h: h.activation(out=rstd[:], in_=ss[:], func=AF.Sqrt, bias=eps_t[:], scale=1.0 / D),
                 reads=[B["ss"], B["eps"]], writes=[B["rstd"]])
            k.op("dve", lambda h: h.reciprocal(out=rstd[:], in_=rstd[:]), reads=[B["rstd"]], writes=[B["rstd"]])
            k.op("dve", lambda h: h.scalar_tensor_tensor(out=xn[:], in0=x_t[:], scalar=rstd[:, 0:1], in1=g_bc[:],
                                                         op0=ALU.mult, op1=ALU.mult),
                 reads=[bx, B["rstd"], B["g_bc"]], writes=[B["xn"]])
            k.step()
            for kc in range(KC):
                k.op("pe", lambda h, kc=kc: h.transpose(out=pA[:, kc, :], in_=xn[:, kc * 128:(kc + 1) * 128], identity=ident[:]),
                     reads=[B["xn"], B["ident"]], writes=[B["pA"]])
            k.op("act", lambda h: h.copy(out=xnT[:], in_=pA[:]), reads=[B["pA"]], writes=[B["xnT"]])
            k.step()
            for c, (pp, bn) in enumerate([(pB, "pB"), (pC, "pC"), (pD, "pD")]):
                for kc in range(KC):
                    k.op("pe", lambda h, kc=kc, c=c, pp=pp: h.matmul(
                        out=pp[:], lhsT=xnT[:, kc, :], rhs=w_in[:, kc, c * 512:(c + 1) * 512],
                        start=(kc == 0), stop=(kc == KC - 1)),
                        reads=[B["xnT"], B["w_in"]], writes=[B[bn]])
            k.op("act", lambda h: h.activation(out=sq[:, 0:512], in_=pB[:], func=AF.Square), reads=[B["pB"]], writes=[B["sq"]])
            k.op("act", lambda h: h.activation(out=sq[:, 512:1024], in_=pC[:], func=AF.Square), reads=[B["pC"]], writes=[B["sq"]])
            k.op("act", lambda h: h.activation(out=sq[:, 1024:1280], in_=pD[:, 0:256], func=AF.Square), reads=[B["pD"]], writes=[B["sq"]])
            k.op("dve", lambda h: h.tensor_reduce(out=ssq[:], in_=sq[:].rearrange("p (h d) -> p h d", d=64), axis=AX.X, op=ALU.add),
                 reads=[B["sq"]], writes=[B["ssq"]])
            k.op("act", lambda h: h.activation(out=rqk[:], in_=ssq[:], func=AF.Sqrt, bias=eps_t[:], scale=1.0 / 64),
                 reads=[B["ssq"], B["eps"]], writes=[B["rqk"]])
            k.op("dve", lambda h: h.reciprocal(out=rqk[:], in_=rqk[:]), reads=[B["rqk"]], writes=[B["rqk"]])
            for (pp, bn, h0, nh) in [(pB, "pB", 0, 8), (pC, "pC", 8, 8), (pD, "pD", 16, 4)]:
                k.op("dve", lambda h, pp=pp, h0=h0, nh=nh: h.tensor_tensor(
                    out=tmpq[:, h0:h0 + nh, :], in0=pp[:, 0:nh * 64].rearrange("p (h d) -> p h d", d=64),
                    in1=rqk[:, h0:h0 + nh].unsqueeze(2).to_broadcast([128, nh, 64]), op=ALU.mult),
                    reads=[B[bn], B["rqk"]], writes=[B["tmpq"]])
            k.op("act", lambda h: h.copy(out=V_all[:, i, :, 0:64], in_=pD[:, 256:512].rearrange("p (g d) -> p g d", d=64)),
                 reads=[B["pD"]], writes=[BV[i]])
            k.step()
            k.op("pool", lambda h: h.tensor_tensor(out=qkn[:], in0=tmpq[:], in1=gqk[:], op=ALU.mult),
                 reads=[B["tmpq"], B["gqk"]], writes=[B["qkn"]])
            q_t, bq = qT[i % 3], BqT[i % 3]
            for half in range(2):
                k.step()
                for hh in range(8):
                    k.op("pe", lambda h, half=half, hh=hh: h.transpose(out=pE[:, hh, :], in_=qkn[:, half * 8 + hh, :], identity=ident[:]),
                         reads=[B["qkn"], B["ident"]], writes=[B["pE"]])
                k.op("act", lambda h, half=half: h.copy(out=q_t[:, half * 8:(half + 1) * 8, :], in_=pE[:]),
                     reads=[B["pE"]], writes=[bq])
            k.step()
            for g in range(4):
                k.op("pe", lambda h, g=g: h.transpose(out=pE[:, g, :], in_=qkn[:, 16 + g, :], identity=ident[:]),
                     reads=[B["qkn"], B["ident"]], writes=[B["pE"]])
            k.op("dve", lambda h: h.tensor_copy(out=kT_all[:, :, i * 128:(i + 1) * 128], in_=pE[:, 0:4, :]),
                 reads=[B["pE"]], writes=[BkT[i]])

        cnt = [0]

        def phase2(i):
            q_t, bq = qT[i % 3], BqT[i % 3]
            blocks = [j for j in (i - 1, i, i + 1) if 0 <= j < NT]
            items = [(g, bi, j) for g in range(4) for bi, j in enumerate(blocks)]
            slots = []

            def emit_S(n):
                g, bi, j = items[n]
                c2 = cnt[0] % 2
                cnt[0] += 1
                slots.append(c2)
                k.op("pe", lambda h: h.matmul(
                    out=pGs[c2][:], lhsT=kT_all[:, g, j * 128:(j + 1) * 128],
                    rhs=q_t[:, 4 * g:4 * g + 4, :].rearrange("p h q -> p (h q)"), start=True, stop=True),
                    reads=[BkT[j], bq], writes=[BpG[c2]])
                k.op("dve", lambda h: h.tensor_tensor(
                    out=tS[c2][:], in0=pGs[c2][:], in1=BM[:, j - i + 1, 4 * g:4 * g + 4, :].rearrange("p h q -> p (h q)"), op=ALU.add),
                    reads=[BpG[c2], B["BM"]], writes=[BtS[c2]])
                k.op("act", lambda h: h.activation(out=PT[c2][:], in_=tS[c2][:], func=AF.Exp),
                     reads=[BtS[c2]], writes=[BPT[c2]])

            def emit_O(n):
                g, bi, j = items[n]
                c2 = slots[n]
                for hh in range(4):
                    k.op("pe", lambda h, hh=hh: h.matmul(
                        out=pH[:, hh, :], lhsT=PT[c2][:, hh * 128:(hh + 1) * 128], rhs=V_all[:, j, g, :],
                        start=(bi == 0 and hh == 0), stop=(bi == len(blocks) - 1 and hh == 3)),
                        reads=[BPT[c2], BV[j], B["V1"]], writes=[B["pH"]])
                if bi == len(blocks) - 1:
                    k.op("dve", lambda h: h.tensor_tensor(out=den[:], in0=pH[:, :, 64], in1=esink[:, 4 * g:4 * g + 4], op=ALU.add),
                         reads=[B["pH"], B["esink"]], writes=[B["den"]])
                    k.op("dve", lambda h: h.reciprocal(out=rden[:], in_=den[:]), reads=[B["den"]], writes=[B["rden"]])
                    k.op("dve", lambda h: h.tensor_tensor(
                        out=ot[:, 4 * g:4 * g + 4, :], in0=pH[:, :, 0:64],
                        in1=rden[:, :].unsqueeze(2).to_broadcast([128, 4, 64]), op=ALU.mult),
                        reads=[B["pH"], B["rden"]], writes=[B["ot"]])

            emit_S(0)
            for n in range(len(items)):
                k.step()
                if n + 1 < len(items):
                    emit_S(n + 1)
                emit_O(n)
            otf = ot[:].rearrange("p h d -> p (h d)")
            k.step()
            for kc in range(KC):
                k.op("pe", lambda h, kc=kc: h.transpose(out=pA[:, kc, :], in_=otf[:, kc * 128:(kc + 1) * 128], identity=ident[:]),
                     reads=[B["ot"], B["ident"]], writes=[B["pA"]])
            k.op("act", lambda h: h.copy(out=otT[:], in_=pA[:]), reads=[B["pA"]], writes=[B["otT"]])
            x_t, bx = xt[i % 3], Bxt[i % 3]
            xo, bxo = x1t[i % 2], Bx1[i % 2]
            for c, (pp, bn) in enumerate([(pB, "pB"), (pC, "pC")]):
                k.step()
                for kc in range(KC):
                    k.op("pe", lambda h, kc=kc, c=c, pp=pp: h.matmul(
                        out=pp[:], lhsT=otT[:, kc, :], rhs=w_out[:, kc, c * 512:(c + 1) * 512],
                        start=(kc == 0), stop=(kc == KC - 1)),
                        reads=[B["otT"], B["w_out"]], writes=[B[bn]])
                k.op("dve", lambda h, c=c, pp=pp: h.tensor_tensor(out=xo[:, c * 512:(c + 1) * 512], in0=pp[:],
                                                                  in1=x_t[:, c * 512:(c + 1) * 512], op=ALU.add),
                     reads=[B[bn], bx], writes=[bxo])
            k.dma("sp", lambda h: h.dma_start(out=XOUT[i * 128:(i + 1) * 128, :], in_=xo[:]), reads=[bxo], writes=[Bxout])

        for i in range(NT + 2):
            sa, sb_ = [], []
            if i < NT:
                k.begin_record()
                phase1(i)
                sa = k.end_record()
            if i >= 2:
                k.begin_record()
                phase2(i - 2)
                sb_ = k.end_record()
            k.replay_merged(sa, sb_)
        allb = []
        return allb


NE = 16
CAP = 512
N_BISECT = 32


def stage_moe(cx, XIN, XOUT, H, Bxin, Bxout, BH, P, layer):
    nc, k = cx.nc, cx.k
    W1, W3, W2 = P["expert_w1"], P["expert_w3"], P["expert_w2"]
    with ExitStack() as st0:
        sb0 = lambda shape, dt, name=None: cx.sb(st0, shape, dt, name)
        AFF = sb0([128, NE, NT], F32, "AFF")
        selm = sb0([128, NE, NT], F32, "selm")
        pos = sb0([128, NE, NT], F32, "pos")
        RH = sb0([128, NE, NT, 4], BF16, "RH")
        iota = sb0([128, CAP], F32, "iota")
        identb = sb0([128, 128], BF16, "identb")
        BA, Bsel, Bpos, BRH, Biota, Bidb = Buf("AFF"), Buf("selm"), Buf("pos"), Buf("RH"), Buf("iota"), Buf("identb")
        k.dma("sp", lambda h: h.dma_start(out=iota[:], in_=P["iota512"]), writes=[Biota])
        k.dma("pool", lambda h: h.dma_start(out=identb[:], in_=P["ident"]), writes=[Bidb])
        k.dma("pool", lambda h: h.dma_start(out=RH[:].rearrange("p e i c -> p (e i c)"), in_=P["rh_const"]), writes=[BRH])

        with ExitStack() as st:
            sb = lambda shape, dt, name=None: cx.sb(st, shape, dt, name)
            ps = lambda shape, dt, name=None: cx.ps(st, shape, dt, name)
            g_bc = sb([128, D], F32, "g_bc")
            wr = sb([128, KC, NE], F32, "wr")
            identf = sb([128, 128], F32, "identf")
            onesf = sb([128, 128], F32, "onesf")
            ustr = sb([128, 128], F32, "ustr")
            eps_t = sb([128, 1], F32, "eps")
            ones32 = sb([128, NT], F32, "ones32")
            xt = [sb([128, D], F32, "xt%d" % i) for i in range(2)]
            hn = [sb([128, D], F32, "hn%d" % i) for i in range(2)]
            hb = [sb([128, D], BF16, "hb%d" % i) for i in range(2)]
            hnT = sb([128, KC, 128], F32, "hnT")
            junk = sb([128, D], BF16, "junk")
            ss = sb([128, 1], F32, "ss")
            rstd = sb([128, 1], F32, "rstd")
            mx = sb([128, 1], F32, "mx")
            ex = sb([128, NE], F32, "ex")
            sm = sb([128, 1], F32, "sm")
            lo = sb([128, NE], F32, "lo")
            hi = sb([128, NE], F32, "hi")
            mid = sb([128, NE], F32, "mid")
            cmp_t = sb([128, NE, NT], F32, "cmp")
            cntp = sb([128, NE], F32, "cntp")
            mge = sb([128, NE], mybir.dt.uint32, "mge")
            mlt = sb([128, NE], mybir.dt.uint32, "mlt")
            incl = sb([128, NE, NT], F32, "incl")
            ahi = sb([128, NE, NT], BF16, "ahi")
            pR0 = ps([128, 4, 128], F32, "pR0")
            pR1 = ps([128, 4, 128], F32, "pR1")
            pL_full = ps([128, 512], F32, "pL")
            pCn_full = ps([128, 512], F32, "pCn")
            pL = pL_full[:, 0:NE]
            pCn = pCn_full[:, 0:NE]
            pP = ps([128, NE * NT], F32, "pP")
            B = {n: Buf(n) for n in ["g_bc", "wr", "identf", "onesf", "ustr", "eps", "ones32", "hnT", "junk", "ss", "rstd",
                                     "mx", "ex", "sm", "lo", "hi", "mid", "cmp", "cntp", "mge", "mlt", "incl", "ahi",
                                     "pR0", "pR1", "pL", "pCn", "pP"]}
            Bxt = [Buf() for _ in range(2)]
            Bhn = [Buf() for _ in range(2)]
            Bhb = [Buf() for _ in range(2)]
            k.dma("sp", lambda h: h.dma_start(out=g_bc[:], in_=P["ffn_norm_g"][layer:layer + 1, :].to_broadcast([128, D])), writes=[B["g_bc"]])
            k.dma("sp", lambda h: h.dma_start(out=wr[:], in_=P["router_w"][layer].rearrange("(kc p) e -> p kc e", p=128)), writes=[B["wr"]])
            k.dma("sp", lambda h: h.dma_start(out=identf[:], in_=P["ident"]), writes=[B["identf"]])
            k.dma("sp", lambda h: h.dma_start(out=ustr[:], in_=P["ustrict"]), writes=[B["ustr"]])
            k.op("dve", lambda h: h.memset(onesf[:], 1.0), writes=[B["onesf"]])
            k.op("dve", lambda h: h.memset(ones32[:], 1.0), writes=[B["ones32"]])
            k.op("dve", lambda h: h.memset(eps_t[:], EPS), writes=[B["eps"]])
            def pre_tile(i):
                s2 = i % 2
                x_t, bx = xt[s2], Bxt[s2]
                k.dma("sp", lambda h, x_t=x_t: h.dma_start(out=x_t[:], in_=XIN[i * 128:(i + 1) * 128, :]), reads=[Bxin], writes=[bx])
                k.dma("sp", lambda h, x_t=x_t: h.dma_start(out=XOUT[i * 128:(i + 1) * 128, :], in_=x_t[:]), reads=[bx], writes=[Bxout])
                k.op("act", lambda h, x_t=x_t: h.activation(out=junk[:], in_=x_t[:], func=AF.Square, accum_out=ss[:]),
                     reads=[bx], writes=[B["junk"], B["ss"]])
                k.op("act", lambda h: h.activation(out=rstd[:], in_=ss[:], func=AF.Sqrt, bias=eps_t[:], scale=1.0 / D),
                     reads=[B["ss"], B["eps"]], writes=[B["rstd"]])
                k.op("dve", lambda h: h.reciprocal(out=rstd[:], in_=rstd[:]), reads=[B["rstd"]], writes=[B["rstd"]])
                k.op("dve", lambda h, x_t=x_t, s2=s2: h.scalar_tensor_tensor(out=hn[s2][:], in0=x_t[:], scalar=rstd[:, 0:1], in1=g_bc[:],
                                                                         op0=ALU.mult, op1=ALU.mult),
                     reads=[bx, B["rstd"], B["g_bc"]], writes=[Bhn[s2]])
                k.op("pool", lambda h, s2=s2: h.tensor_copy(out=hb[s2][:], in_=hn[s2][:]), reads=[Bhn[s2]], writes=[Bhb[s2]])
                k.dma("sp", lambda h, s2=s2: h.dma_start(out=H[i * 128:(i + 1) * 128, :], in_=hb[s2][:]), reads=[Bhb[s2]], writes=[BH])

            def pre_tile_back(i):
                s2 = i % 2
                for kc in range(KC):
                    pp, bn = (pR0, "pR0") if kc < 4 else (pR1, "pR1")
                    k.op("pe", lambda h, kc=kc, pp=pp, s2=s2: h.transpose(out=pp[:, kc % 4, :], in_=hn[s2][:, kc * 128:(kc + 1) * 128], identity=identf[:]),
                         reads=[Bhn[s2], B["identf"]], writes=[B[bn]])
                k.op("act", lambda h: h.copy(out=hnT[:, 0:4, :], in_=pR0[:]), reads=[B["pR0"]], writes=[B["hnT"]])
                k.op("dve", lambda h: h.tensor_copy(out=hnT[:, 4:8, :], in_=pR1[:]), reads=[B["pR1"]], writes=[B["hnT"]])
                for kc in range(KC):
                    k.op("pe", lambda h, kc=kc: h.matmul(out=pL[:], lhsT=hnT[:, kc, :], rhs=wr[:, kc, :], start=(kc == 0), stop=(kc == KC - 1)),
                         reads=[B["hnT"], B["wr"]], writes=[B["pL"]])
                k.op("dve", lambda h: h.tensor_reduce(out=mx[:], in_=pL[:], axis=AX.X, op=ALU.max, negate=True),
                     reads=[B["pL"]], writes=[B["mx"]])
                k.op("act", lambda h: h.activation(out=ex[:], in_=pL[:], func=AF.Exp, bias=mx[:], scale=1.0, accum_out=sm[:]),
                     reads=[B["pL"], B["mx"]], writes=[B["ex"], B["sm"]])
                k.op("dve", lambda h: h.reciprocal(out=sm[:], in_=sm[:]), reads=[B["sm"]], writes=[B["sm"]])
                k.op("dve", lambda h, i=i: h.tensor_scalar(out=AFF[:, :, i], in0=ex[:], scalar1=sm[:, 0:1], scalar2=None, op0=ALU.mult),
                     reads=[B["ex"], B["sm"]], writes=[BA])
            pre_tile(0)
            for i in range(NT):
                if i + 1 < NT:
                    pre_tile(i + 1)
                pre_tile_back(i)
            k.op("dve", lambda h: h.memset(lo[:], 0.0), writes=[B["lo"]])
            k.op("dve", lambda h: h.memset(hi[:], 1.0), writes=[B["hi"]])
            for it in range(N_BISECT):
                wdt = 2.0 ** -(it + 1)
                k.op("dve", lambda h, wdt=wdt: h.tensor_scalar(out=mid[:], in0=lo[:], scalar1=wdt, scalar2=None, op0=ALU.add),
                     reads=[B["lo"]], writes=[B["mid"]])
                k.op("dve", lambda h: h.tensor_tensor(out=cmp_t[:], in0=AFF[:], in1=mid[:, :].unsqueeze(2).to_broadcast([128, NE, NT]), op=ALU.is_ge),
                     reads=[BA, B["mid"]], writes=[B["cmp"]])
                k.op("dve", lambda h: h.tensor_reduce(out=cntp[:], in_=cmp_t[:], axis=AX.X, op=ALU.add), reads=[B["cmp"]], writes=[B["cntp"]])
                k.op("pe", lambda h: h.matmul(out=pCn[:], lhsT=onesf[:], rhs=cntp[:], start=True, stop=True),
                     reads=[B["onesf"], B["cntp"]], writes=[B["pCn"]])
                k.op("dve", lambda h, wdt=wdt: h.tensor_scalar(out=hi[:], in0=pCn[:], scalar1=CAP - 0.5, scalar2=wdt, op0=ALU.is_ge, op1=ALU.mult),
                     reads=[B["pCn"]], writes=[B["hi"]])
                k.op("dve", lambda h: h.tensor_tensor(out=lo[:], in0=lo[:], in1=hi[:], op=ALU.add), reads=[B["lo"], B["hi"]], writes=[B["lo"]])
            k.op("dve", lambda h: h.tensor_tensor(out=selm[:], in0=AFF[:], in1=lo[:, :].unsqueeze(2).to_broadcast([128, NE, NT]), op=ALU.is_ge),
                 reads=[BA, B["lo"]], writes=[Bsel])
            for e in range(NE):
                k.op("dve", lambda h, e=e: h.tensor_tensor_scan(out=incl[:, e, :], data0=ones32[:], data1=selm[:, e, :], initial=0.0,
                                                              op0=ALU.mult, op1=ALU.add),
                     reads=[Bsel, B["ones32"]], writes=[B["incl"]])
            k.op("dve", lambda h: h.tensor_tensor(out=incl[:], in0=incl[:], in1=selm[:], op=ALU.subtract), reads=[B["incl"], Bsel], writes=[B["incl"]])
            k.op("pe", lambda h: h.matmul(out=pP[:], lhsT=ustr[:], rhs=selm[:].rearrange("p e i -> p (e i)"), start=True, stop=False),
                 reads=[B["ustr"], Bsel], writes=[B["pP"]])
            k.op("pe", lambda h: h.matmul(out=pP[:], lhsT=onesf[:], rhs=incl[:].rearrange("p e i -> p (e i)"), start=False, stop=True),
                 reads=[B["onesf"], B["incl"]], writes=[B["pP"]])
            k.op("act", lambda h: h.copy(out=pos[:].rearrange("p e i -> p (e i)"), in_=pP[:]), reads=[B["pP"]], writes=[Bpos])
            k.op("dve", lambda h: h.tensor_copy(out=ahi[:], in_=AFF[:]), reads=[BA], writes=[B["ahi"]])
            k.op("dve", lambda h: h.tensor_copy(out=RH[:, :, :, 2], in_=ahi[:]), reads=[B["ahi"]], writes=[BRH])
            k.op("dve", lambda h: h.tensor_tensor(out=RH[:, :, :, 3], in0=AFF[:], in1=ahi[:], op=ALU.subtract), reads=[BA, B["ahi"]], writes=[BRH])
            if "dbg_aff" in P and layer == 0:
                bd = Buf()
                k.dma("sp", lambda h: h.dma_start(out=P["dbg_aff"], in_=AFF[:].rearrange("p e i -> p (e i)")), reads=[BA], writes=[bd])
                k.dma("sp", lambda h: h.dma_start(out=P["dbg_sel"], in_=selm[:].rearrange("p e i -> p (e i)")), reads=[Bsel], writes=[bd])
                k.dma("sp", lambda h: h.dma_start(out=P["dbg_pos"], in_=pos[:].rearrange("p e i -> p (e i)")), reads=[Bpos], writes=[bd])
                k.dma("sp", lambda h: h.dma_start(out=P["dbg_lo"], in_=lo[:]), reads=[B["lo"]], writes=[bd])
                k.dma("sp", lambda h: h.dma_start(out=P["dbg_hi"], in_=hi[:]), reads=[B["hi"]], writes=[bd])
                P["_dbg_bufs"].append(bd)
        k.barrier()

        with ExitStack() as st:
            sb = lambda shape, dt, name=None: cx.sb(st, shape, dt, name)
            ps = lambda shape, dt, name=None: cx.ps(st, shape, dt, name)
            NCH = 4
            w1c = [sb([128, KC, 512], BF16, "w1c%d" % c) for c in range(NCH)]
            w3c = [sb([128, KC, 512], BF16, "w3c%d" % c) for c in range(NCH)]
            w2c = [sb([128, 4, D], BF16, "w2c%d" % c) for c in range(NCH)]
            Bw1 = [Buf() for _ in range(NCH)]
            Bw3 = [Buf() for _ in range(NCH)]
            Bw2 = [Buf() for _ in range(NCH)]
            Pm = [sb([128, CAP], BF16, "Pm%d" % c) for c in range(4)]
            BPm = [Buf() for _ in range(4)]
            idxf = sb([128, 4], F32, "idxf")
            pIs = sb([128, 4, 4], F32, "pIs")
            BpIs = Buf()
            idxi = [sb([128, 4], I32, "idxi%d" % c) for c in range(2)]
            gt = [sb([128, 4], F32, "gt%d" % c) for c in range(2)]
            Bidxf = Buf()
            Bidx = [Buf() for _ in range(2)]
            Bgt = [Buf() for _ in range(2)]
            xs = [sb([128, D], BF16, "xs%d" % c) for c in range(4)]
            Bxs = [Buf() for _ in range(4)]
            xsT = [sb([128, KC, CAP], BF16, "xsT%d" % c) for c in range(2)]
            BxsT = [Buf() for _ in range(2)]
            actT = sb([128, 16, CAP], BF16, "actT")
            BactT = [Buf() for _ in range(16)]
            sa = [sb([128, CAP], F32, "sa%d" % c) for c in range(2)]
            Bsa = [Buf() for _ in range(2)]
            yt = [sb([128, D], F32, "yt%d" % c) for c in range(2)]
            Byt = [Buf() for _ in range(2)]
            pT = ps([128, KC, 128], BF16, "pT")
            pI = ps([128, 4, 4], F32, "pI")
            pa = [ps([128, CAP], F32, "pa%d" % c) for c in range(2)]
            pu = [ps([128, CAP], F32, "pu%d" % c) for c in range(2)]
            py = [ps([128, 512], F32, "py%d" % c) for c in range(2)]
            BpT, BpI = Buf(), Buf()
            Bpa = [Buf() for _ in range(2)]
            Bpu = [Buf() for _ in range(2)]
            Bpy = [Buf() for _ in range(2)]
            cnt = {"stg": 0, "cast": 0, "y": 0}
            cast_engs = ["act", "dve", "act", "pool"]

            def load_chunk(src_ap, dst, bdst):
                k.dma("pool", lambda h: h.dma_start(out=dst[:], in_=src_ap), writes=[bdst])

            def load_w13(e, c):
                load_chunk(W1[layer, e].rearrange("(kc p) f -> p kc f", p=128)[:, :, c * 512:(c + 1) * 512], w1c[c], Bw1[c])
                load_chunk(W3[layer, e].rearrange("(kc p) f -> p kc f", p=128)[:, :, c * 512:(c + 1) * 512], w3c[c], Bw3[c])

            def load_w2(e, c):
                load_chunk(W2[layer, e].rearrange("(fc p) d -> p fc d", p=128)[:, c * 4:(c + 1) * 4, :], w2c[c], Bw2[c])

            def build_steps(e):
                par = e % 2
                stepsA, stepsB = [], []

                def emit_pm(i):
                    pm, bpm = Pm[i % 4], BPm[i % 4]
                    k.op("dve", lambda h: h.tensor_scalar(out=pm[:], in0=iota[:], scalar1=pos[:, e, i:i + 1], scalar2=selm[:, e, i:i + 1],
                                                          op0=ALU.is_equal, op1=ALU.mult),
                         reads=[Biota, Bpos, Bsel], writes=[bpm])

                def step_i(i):
                    def f():
                        if i == 0:
                            emit_pm(0)
                            emit_pm(1)
                        if i + 2 < NT:
                            emit_pm(i + 2)
                        pm, bpm = Pm[i % 4], BPm[i % 4]
                        for c in range(4):
                            k.op("pe", lambda h, c=c: h.matmul(out=pI[:, c, :], lhsT=pm[:, c * 128:(c + 1) * 128], rhs=RH[:, e, i, :],
                                                               start=(i == 0 and c == 0), stop=(i == NT - 1 and c == 3)),
                                 reads=[bpm, BRH], writes=[BpI])
                    return f

                for i in range(NT):
                    stepsA.append(step_i(i))

                def fin_a():
                    k.op("dve", lambda h: h.tensor_copy(out=pIs[:], in_=pI[:]), reads=[BpI], writes=[BpIs])
                    k.op("dve", lambda h: h.scalar_tensor_tensor(out=idxf[:], in0=pIs[:, :, 1], scalar=128.0, in1=pIs[:, :, 0],
                                                                 op0=ALU.mult, op1=ALU.add),
                         reads=[BpIs], writes=[Bidxf])
                    k.op("dve", lambda h: h.tensor_copy(out=idxi[par][:], in_=idxf[:]), reads=[Bidxf], writes=[Bidx[par]])
                    if "dbg_idx" in P and layer == 0:
                        bd = Buf()
                        k.dma("sp", lambda h: h.dma_start(out=P["dbg_idx"][:, e * 4:(e + 1) * 4], in_=idxf[:]), reads=[Bidxf], writes=[bd])
                        k.dma("sp", lambda h: h.dma_start(out=P["dbg_pis"][:, e * 16:(e + 1) * 16], in_=pIs[:].rearrange("p a b -> p (a b)")), reads=[BpIs], writes=[bd])
                        P["_dbg_bufs"].append(bd)
                    k.op("dve", lambda h: h.tensor_tensor(out=gt[par][:], in0=pIs[:, :, 2], in1=pIs[:, :, 3], op=ALU.add),
                         reads=[BpIs], writes=[Bgt[par]])
                    for c in range(4):
                        k.dma("pool", lambda h, c=c: h.indirect_dma_start(
                            out=xs[c][:], out_offset=None, in_=H, in_offset=bass.IndirectOffsetOnAxis(ap=idxi[par][:, c:c + 1], axis=0)),
                            reads=[BH, Bidx[par]], writes=[Bxs[c]])
                stepsA.append(fin_a)

                def fin_b(c):
                    def f():
                        for kc in range(KC):
                            k.op("pe", lambda h, kc=kc: h.transpose(out=pT[:, kc, :], in_=xs[c][:, kc * 128:(kc + 1) * 128], identity=identb[:]),
                                 reads=[Bxs[c], Bidb], writes=[BpT])
                        k.op("act", lambda h: h.copy(out=xsT[par][:, :, c * 128:(c + 1) * 128], in_=pT[:]),
                             reads=[BpT], writes=[BxsT[par]])
                    return f
                for c in range(4):
                    stepsB.append(fin_b(c))
                return stepsA, stepsB

            def ffn(e, inter, interB):
                par = e % 2
                n_inter = len(inter)
                done = 0
                doneB = 0
                for fc in range(16):
                    c = fc // 4
                    s2 = fc % 2
                    for (wc, bw, pp, bp) in [(w1c[c], Bw1[c], pa[s2], Bpa[s2]), (w3c[c], Bw3[c], pu[s2], Bpu[s2])]:
                        for kc in range(KC):
                            k.op("pe", lambda h, kc=kc, wc=wc, pp=pp, fc=fc: h.matmul(
                                out=pp[:], lhsT=wc[:, kc, (fc % 4) * 128:(fc % 4 + 1) * 128], rhs=xsT[par][:, kc, :],
                                start=(kc == 0), stop=(kc == KC - 1)),
                                reads=[bw, BxsT[par]], writes=[bp])
                    k.op("act", lambda h, s2=s2: h.activation(out=sa[s2][:], in_=pa[s2][:], func=AF.Silu), reads=[Bpa[s2]], writes=[Bsa[s2]])
                    k.op("dve", lambda h, s2=s2, fc=fc: h.tensor_tensor(out=actT[:, fc, :], in0=sa[s2][:], in1=pu[s2][:], op=ALU.mult),
                         reads=[Bsa[s2], Bpu[s2]], writes=[BactT[fc]])
                    if fc % 4 == 3 and e + 1 < NE:
                        load_w13(e + 1, c)
                    target = (n_inter * (fc + 1)) // 16
                    while done < target:
                        inter[done]()
                        done += 1
                for c in range(4):
                    for dc in range(2):
                        q = cnt["y"] % 2
                        cnt["y"] += 1
                        for fc in range(16):
                            k.op("pe", lambda h, fc=fc, c=c, dc=dc, q=q: h.matmul(
                                out=py[q][:], lhsT=actT[:, fc, c * 128:(c + 1) * 128], rhs=w2c[fc // 4][:, fc % 4, dc * 512:(dc + 1) * 512],
                                start=(fc == 0), stop=(fc == 15)),
                                reads=[BactT[fc], Bw2[fc // 4]], writes=[Bpy[q]])
                        eng = "act" if dc == 0 else "dve"
                        if eng == "act":
                            k.op("act", lambda h, c=c, dc=dc, q=q: h.activation(out=yt[c % 2][:, dc * 512:(dc + 1) * 512], in_=py[q][:], func=AF.Copy,
                                                                                  scale=gt[par][:, c:c + 1]),
                                 reads=[Bpy[q], Bgt[par]], writes=[Byt[c % 2]])
                        else:
                            k.op("dve", lambda h, c=c, dc=dc, q=q: h.tensor_scalar(out=yt[c % 2][:, dc * 512:(dc + 1) * 512], in0=py[q][:],
                                                                                    scalar1=gt[par][:, c:c + 1], scalar2=None, op0=ALU.mult),
                                 reads=[Bpy[q], Bgt[par]], writes=[Byt[c % 2]])
                    if doneB < len(interB):
                        interB[doneB]()
                        doneB += 1
                    k.dma("pool", lambda h, c=c: h.indirect_dma_start(
                        out=XOUT, out_offset=bass.IndirectOffsetOnAxis(ap=idxi[par][:, c:c + 1], axis=0),
                        in_=yt[c % 2][:], in_offset=None, compute_op=ALU.add),
                        reads=[Byt[c % 2], Bidx[par]], writes=[Bxout])
                    if c == 3 and e + 1 < NE:
                        for cc in range(NCH):
                            load_w2(e + 1, cc)

            for c in range(NCH):
                load_w13(0, c)
            for c in range(NCH):
                load_w2(0, c)
            sA, sB = build_steps(0)
            for f in sA + sB:
                f()
            for e in range(NE):
                sA, sB = build_steps(e + 1) if e + 1 < NE else ([], [])
                ffn(e, sA, sB)
        k.barrier()


NH = 8
MIN = 3104
VW = 132


def stage_mlstm(cx, XIN, XOUT, Bxin, Bxout, P):
    nc, k = cx.nc, cx.k
    W = P["mlstm_w_in"]
    KS, VS, OGS = P["KS"], P["VS"], P["OGS"]
    HF = XOUT
    BKS, BVS, BOGS, BHF = Buf("KS"), Buf("VS"), Buf("OGS"), Buf("HF")
    with ExitStack() as st0:
        sb0 = lambda shape, dt, name=None: cx.sb(st0, shape, dt, name)
        QT = sb0([128, 4, S], BF16, "QT")
        KT = sb0([128, 4, S], BF16, "KT")
        G = sb0([128, NT, 32], F32, "G")
        identb = sb0([128, 128], BF16, "identb")
        identf = sb0([128, 128], F32, "identf")
        eps_t = sb0([128, 1], F32, "eps")
        one_t = sb0([128, 1], F32, "one")
        g_bc = sb0([128, D], F32, "g_bc")
        BQT = [Buf() for _ in range(NT)]
        BKT = [Buf() for _ in range(NT)]
        BG = [Buf() for _ in range(NT)]
        Bidb, Bidf, Beps, Bone, Bg = Buf(), Buf(), Buf(), Buf(), Buf()
        k.dma("pool", lambda h: h.dma_start(out=identb[:], in_=P["ident"]), writes=[Bidb])
        k.dma("sp", lambda h: h.dma_start(out=identf[:], in_=P["ident"]), writes=[Bidf])
        k.dma("sp", lambda h: h.dma_start(out=g_bc[:], in_=P["mlstm_norm_g"].to_broadcast([128, D])), writes=[Bg])
        k.op("dve", lambda h: h.memset(eps_t[:], EPS), writes=[Beps])
        k.op("dve", lambda h: h.memset(one_t[:], 1.0), writes=[Bone])

        with ExitStack() as st:
            sb = lambda shape, dt, name=None: cx.sb(st, shape, dt, name)
            ps = lambda shape, dt, name=None: cx.ps(st, shape, dt, name)
            w_in = sb([128, KC, MIN], BF16, "w_in")
            bgate = sb([128, 32], F32, "bgate")
            xt = [sb([128, D], F32, "xt%d" % i) for i in range(2)]
            junk = sb([128, D], BF16, "junk")
            xn = sb([128, D], BF16, "xn")
            xnT2 = [sb([128, KC, 128], BF16, "xnT%d" % i) for i in range(2)]
            BxnT2 = [Buf() for _ in range(2)]
            ss = sb([128, 1], F32, "ss")
            rstd = sb([128, 1], F32, "rstd")
            ktok = [sb([128, 512], BF16, "ktok%d" % i) for i in range(2)]
            vtile = [sb([128, NH, VW], BF16, "vt%d" % i) for i in range(2)]
            ogt = [sb([128, D], BF16, "ogt%d" % i) for i in range(2)]
            zt = sb([128, 32], F32, "zt")
            et = sb([128, 32], F32, "et")
            pA = ps([128, KC, 128], BF16, "pA")
            pQ = ps([128, 4, 128], F32, "pQ")
            pK = ps([128, 4, 128], F32, "pK")
            pTk = ps([128, 512], F32, "pTk")
            pV0 = ps([128, 512], F32, "pV0")
            pV1 = ps([128, 512], F32, "pV1")
            pGf = ps([128, 512], F32, "pG")
            pG = pGf[:, 0:32]
            B = {n: Buf(n) for n in ["w_in", "bgate", "junk", "xn", "xnT", "ss", "rstd", "zt", "et",
                                     "pA", "pQ", "pK", "pTk", "pV0", "pV1", "pG"]}
            Bxt = [Buf() for _ in range(2)]
            Bkt = [Buf() for _ in range(2)]
            Bvt = [Buf() for _ in range(2)]
            Bog = [Buf() for _ in range(2)]
            wv = W.rearrange("(kc p) n -> p kc n", p=128)
            for c0 in range(0, MIN, 776):
                k.dma("pool", lambda h, c0=c0: h.dma_start(out=w_in[:, :, c0:c0 + 776], in_=wv[:, :, c0:c0 + 776]), writes=[B["w_in"]])
            k.dma("sp", lambda h: h.dma_start(out=bgate[:], in_=P["mlstm_gate_bias"].to_broadcast([128, 32])), writes=[B["bgate"]])
            for i2 in range(2):
                k.op("pool", lambda h, i2=i2: h.memset(vtile[i2][:], 0.0), writes=[Bvt[i2]])
                k.op("pool", lambda h, i2=i2: h.memset(vtile[i2][:, :, 128:129], 1.0), writes=[Bvt[i2]])

            def p0_tile(i):
                s2 = i % 2
                x_t, bx = xt[s2], Bxt[s2]
                k.dma("sp", lambda h: h.dma_start(out=x_t[:], in_=XIN[i * 128:(i + 1) * 128, :]), reads=[Bxin], writes=[bx])
                k.op("act", lambda h: h.activation(out=junk[:], in_=x_t[:], func=AF.Square, accum_out=ss[:]),
                     reads=[bx], writes=[B["junk"], B["ss"]])
                k.op("act", lambda h: h.activation(out=rstd[:], in_=ss[:], func=AF.Sqrt, bias=eps_t[:], scale=1.0 / D),
                     reads=[B["ss"], Beps], writes=[B["rstd"]])
                k.op("dve", lambda h: h.reciprocal(out=rstd[:], in_=rstd[:]), reads=[B["rstd"]], writes=[B["rstd"]])
                k.op("dve", lambda h: h.scalar_tensor_tensor(out=xn[:], in0=x_t[:], scalar=rstd[:, 0:1], in1=g_bc[:],
                                                             op0=ALU.mult, op1=ALU.mult),
                     reads=[bx, B["rstd"], Bg], writes=[B["xn"]])
                for kc in range(KC):
                    k.op("pe", lambda h, kc=kc: h.transpose(out=pA[:, kc, :], in_=xn[:, kc * 128:(kc + 1) * 128], identity=identb[:]),
                         reads=[B["xn"], Bidb], writes=[B["pA"]])
                k.op("act", lambda h: h.copy(out=xnT2[s2][:], in_=pA[:]), reads=[B["pA"]], writes=[BxnT2[s2]])

            def p0_back(i):
                s2 = i % 2
                xnT = xnT2[s2]
                B["xnT"] = BxnT2[s2]
                for (pp, bn, col0) in [(pQ, "pQ", 0), (pK, "pK", 512)]:
                    for j in range(4):
                        for kc in range(KC):
                            k.op("pe", lambda h, kc=kc, j=j, pp=pp, col0=col0: h.matmul(
                                out=pp[:, j, :], lhsT=w_in[:, kc, col0 + j * 128:col0 + (j + 1) * 128], rhs=xnT[:, kc, :],
                                start=(kc == 0), stop=(kc == KC - 1)),
                                reads=[B["w_in"], B["xnT"]], writes=[B[bn]])
                k.op("act", lambda h: h.copy(out=QT[:, :, i * 128:(i + 1) * 128], in_=pQ[:]), reads=[B["pQ"]], writes=[BQT[i]])
                k.op("act", lambda h: h.activation(out=KT[:, :, i * 128:(i + 1) * 128], in_=pK[:], func=AF.Copy, scale=0.125),
                     reads=[B["pK"]], writes=[BKT[i]])
                for kc in range(KC):
                    k.op("pe", lambda h, kc=kc: h.matmul(out=pTk[:], lhsT=xnT[:, kc, :], rhs=w_in[:, kc, 512:1024],
                                                         start=(kc == 0), stop=(kc == KC - 1)),
                         reads=[B["w_in"], B["xnT"]], writes=[B["pTk"]])
                k.op("dve", lambda h: h.tensor_scalar(out=ktok[s2][:], in0=pTk[:], scalar1=0.125, scalar2=None, op0=ALU.mult),
                     reads=[B["pTk"]], writes=[Bkt[s2]])
                k.dma("sp", lambda h: h.dma_start(out=KS[i * 128:(i + 1) * 128, :], in_=ktok[s2][:]), reads=[Bkt[s2]], writes=[BKS])
                for half, (pp, bn) in enumerate([(pV0, "pV0"), (pV1, "pV1")]):
                    for kc in range(KC):
                        k.op("pe", lambda h, kc=kc, pp=pp, half=half: h.matmul(
                            out=pp[:], lhsT=xnT[:, kc, :], rhs=w_in[:, kc, 1024 + half * 512:1024 + (half + 1) * 512],
                            start=(kc == 0), stop=(kc == KC - 1)),
                            reads=[B["w_in"], B["xnT"]], writes=[B[bn]])
                    eng = "act" if half == 0 else "dve"
                    if eng == "act":
                        k.op("act", lambda h, pp=pp, half=half: h.copy(out=vtile[s2][:, half * 4:(half + 1) * 4, 0:128],
                                                                       in_=pp[:].rearrange("p (h d) -> p h d", d=128)),
                             reads=[B[bn]], writes=[Bvt[s2]])
                    else:
                        k.op("dve", lambda h, pp=pp, half=half: h.tensor_copy(out=vtile[s2][:, half * 4:(half + 1) * 4, 0:128],
                                                                              in_=pp[:].rearrange("p (h d) -> p h d", d=128)),
                             reads=[B[bn]], writes=[Bvt[s2]])
                k.dma("sp", lambda h: h.dma_start(out=VS[i * 128:(i + 1) * 128, :, :], in_=vtile[s2][:]), reads=[Bvt[s2]], writes=[BVS])
                for half, (pp, bn) in enumerate([(pV0, "pV0"), (pV1, "pV1")]):
                    for kc in range(KC):
                        k.op("pe", lambda h, kc=kc, pp=pp, half=half: h.matmul(
                            out=pp[:], lhsT=xnT[:, kc, :], rhs=w_in[:, kc, 2048 + half * 512:2048 + (half + 1) * 512],
                            start=(kc == 0), stop=(kc == KC - 1)),
                            reads=[B["w_in"], B["xnT"]], writes=[B[bn]])
                    k.op("act", lambda h, pp=pp, half=half: h.activation(out=ogt[s2][:, half * 512:(half + 1) * 512], in_=pp[:], func=AF.Sigmoid),
                         reads=[B[bn]], writes=[Bog[s2]])
                k.dma("sp", lambda h: h.dma_start(out=OGS[i * 128:(i + 1) * 128, :], in_=ogt[s2][:]), reads=[Bog[s2]], writes=[BOGS])
                for kc in range(KC):
                    k.op("pe", lambda h, kc=kc: h.matmul(out=pG, lhsT=xnT[:, kc, :], rhs=w_in[:, kc, 3072:3104],
                                                         start=(kc == 0), stop=(kc == KC - 1)),
                         reads=[B["w_in"], B["xnT"]], writes=[B["pG"]])
                k.op("dve", lambda h: h.tensor_tensor(out=zt[:], in0=pG, in1=bgate[:], op=ALU.add), reads=[B["pG"], B["bgate"]], writes=[B["zt"]])
                k.op("act", lambda h: h.activation(out=et[:], in_=zt[:], func=AF.Exp, scale=-1.0), reads=[B["zt"]], writes=[B["et"]])
                k.op("act", lambda h: h.activation(out=et[:], in_=et[:], func=AF.Ln, bias=one_t[:], scale=1.0), reads=[B["et"], Bone], writes=[B["et"]])
                zv = zt[:].rearrange("p (a b) -> p a b", b=8)
                ev = et[:].rearrange("p (a b) -> p a b", b=8)
                gv = G[:, i, :].rearrange("p (a b) -> p a b", b=8)
                k.op("dve", lambda h: h.tensor_copy(out=gv[:, 0:4:2, :], in_=zv[:, 0:4:2, :]), reads=[B["zt"]], writes=[BG[i]])
                k.op("dve", lambda h: h.tensor_scalar(out=gv[:, 1:4:2, :], in0=ev[:, 1:4:2, :], scalar1=-1.0, scalar2=None, op0=ALU.mult),
                     reads=[B["et"]], writes=[BG[i]])

            p0_tile(0)
            for i in range(NT):
                if i + 1 < NT:
                    p0_tile(i + 1)
                p0_back(i)
        k.barrier()
        if ML_STOP == "p0":
            return

        with ExitStack() as st:
            sb = lambda shape, dt, name=None: cx.sb(st, shape, dt, name)
            ps = lambda shape, dt, name=None: cx.ps(st, shape, dt, name)
            tri = [sb([128, 128], F32, "tri%d" % d_) for d_ in range(2)]
            negm = [sb([128, 128], F32, "negm%d" % d_) for d_ in range(2)]
            ones_c = sb([128, 1], BF16, "ones_c")
            w_out = sb([128, KC, D], BF16, "w_out")
            og_bc = sb([128, D], F32, "og_bc")
            Cst = sb([128, NH, VW], F32, "Cst")
            Cbf = sb([128, NH, VW], BF16, "Cbf")
            LFB = sb([128, NH, 128], F32, "LFB")
            bias_s = sb([128, NH], F32, "bias_s")
            eb = sb([128, NH], F32, "eb")
            ebL = sb([128, NH], F32, "ebL")
            DT = sb([128, NH, 128], F32, "DT")
            SwT = sb([128, NH, 128], BF16, "SwT")
            tmpn = sb([128, 4, 128], F32, "tmpn")
            num = sb([128, 4, 128], F32, "num")
            dsm = sb([128, 8], F32, "dsm")
            dtm = sb([128, 4], F32, "dtm")
            den = sb([128, 4], F32, "den")
            hdir = [sb([128, NH, 128], F32, "hdir%d" % i) for i in range(2)]
            kw = sb([128, NH, 64], BF16, "kw")
            ktl = [sb([128, 512], BF16, "ktl%d" % i) for i in range(2)]
            vtl = [sb([128, NH, VW], BF16, "vtl%d" % i) for i in range(2)]
            hfl = [sb([128, D], F32, "hfl")] * 2
            ogl = [sb([128, D], BF16, "ogl")] * 2
            xl = [sb([128, D], F32, "xl")] * 2
            ktm = [sb([128, 4, 128], BF16, "ktm%d" % i) for i in range(2)]
            ssh = sb([128, NH], F32, "ssh")
            yb = sb([128, D], BF16, "yb")
            ybT = sb([128, KC, 128], BF16, "ybT")
            xo = [sb([128, D], F32, "xo")] * 2
            sqh = LFB[:].rearrange("p h d -> p (h d)")
            pSs = [ps([128, 4, 128], F32, "pS%d" % i) for i in range(2)]
            pBMs = [ps([128, 4, 128], F32, "pBM%d" % i) for i in range(2)]
            pNi = ps([128, 4, 128], F32, "pNi")
            pNe = ps([128, 4, 128], F32, "pNe")
            pSmCu = ps([128, 512], F32, "pSmCu")
            pCu = pSmCu[:, 0:3 * VW].rearrange("p (a b) -> p a b", b=VW)
            pSm = pSmCu[:, 400:416]
            pA = ps([128, KC, 128], BF16, "pA")
            pY = pNi[:].rearrange("p a b -> p (a b)")
            B = {n: Buf(n) for n in ["tri", "negm", "ones_c", "w_out", "og_bc", "Cst", "Cbf", "LFB", "bias_s", "eb", "ebL", "DT", "SwT",
                                     "tmpn", "num", "dsm", "dtm", "den", "kw", "sqh", "ssh", "yb", "ybT",
                                     "pS", "pBM", "pNi", "pNe", "pSm", "pCu", "pA", "pY"]}
            B["pCu"] = B["pSm"]
            B["pY"] = B["pNi"]
            BpS = [Buf() for _ in range(2)]
            BpBM = [Buf() for _ in range(2)]
            BDT = [Buf() for _ in range(2)]
            BSwT = [Buf() for _ in range(2)]
            BebL = [Buf() for _ in range(2)]
            Bhd = [Buf() for _ in range(2)]
            Bktl = [Buf() for _ in range(2)]
            Bvtl = [Buf() for _ in range(2)]
            Bhfl = [Buf()] * 2
            Bogl = [Buf()] * 2
            Bxl = [Buf()] * 2
            Bxo = [Buf()] * 2
            k.dma("sp", lambda h: h.dma_start(out=tri[0][:], in_=P["tri_f"]), writes=[B["tri"]])
            k.dma("sp", lambda h: h.dma_start(out=tri[1][:], in_=P["tri_b"]), writes=[B["tri"]])
            k.dma("sp", lambda h: h.dma_start(out=negm[0][:], in_=P["negm_f"]), writes=[B["negm"]])
            k.dma("sp", lambda h: h.dma_start(out=negm[1][:], in_=P["negm_b"]), writes=[B["negm"]])
            k.dma("pool", lambda h: h.dma_start(out=w_out[:], in_=P["mlstm_w_out"].rearrange("(kc p) n -> p kc n", p=128)), writes=[B["w_out"]])
            k.dma("sp", lambda h: h.dma_start(out=og_bc[:], in_=P["mlstm_out_norm_g"].to_broadcast([128, D])), writes=[B["og_bc"]])
            k.op("pool", lambda h: h.memset(ones_c[:], 1.0), writes=[B["ones_c"]])
            B["ktm"] = Buf("ktm")
            k.op("pool", lambda h: h.memset(ktm[0][:], 0.0), writes=[B["ktm"]])
            k.op("pool", lambda h: h.memset(ktm[1][:], 0.0), writes=[B["ktm"]])

            def chunk(dr, c, step):
                s2 = step % 2
                l_last = 127 if dr == 0 else 0
                lf = G[:, c, 8 + 16 * dr:16 + 16 * dr]
                ig = G[:, c, 16 * dr:8 + 16 * dr]
                kt_, bkt = ktl[s2], Bktl[s2]
                vt_, bvt = vtl[s2], Bvtl[s2]
                hd, bhd = hdir[s2], Bhd[s2]
                csl = slice(c * 128, (c + 1) * 128)
                k.dma("sp", lambda h: h.dma_start(out=kt_[:], in_=KS[csl, :]), reads=[BKS], writes=[bkt])
                k.dma("sp", lambda h: h.dma_start(out=vt_[:], in_=VS[csl, :, :]), reads=[BVS], writes=[bvt])
                if dr == 1 and "ld2" not in ML_SKIP:
                    if "no_hfl" not in ML_SKIP:
                        k.dma("sp", lambda h: h.dma_start(out=hfl[s2][:], in_=HF[csl, :]), reads=[BHF], writes=[Bhfl[s2]])
                    if "no_ogl" not in ML_SKIP:
                        k.dma("sp", lambda h: h.dma_start(out=ogl[s2][:], in_=OGS[csl, :]), reads=[BOGS], writes=[Bogl[s2]])
                    if "no_xl" not in ML_SKIP:
                        k.dma("sp", lambda h: h.dma_start(out=xl[s2][:], in_=XIN[csl, :]), reads=[Bxin], writes=[Bxl[s2]])
                k.op("act", lambda h: h.copy(out=ktm[0][0:64, :, :], in_=KT[0:64, :, csl]), reads=[BKT[c]], writes=[B["ktm"]])
                k.op("pool", lambda h: h.tensor_copy(out=ktm[1][64:128, :, :], in_=KT[64:128, :, csl]), reads=[BKT[c]], writes=[B["ktm"]])
                k.op("pe", lambda h: h.matmul(out=pSm[:, 0:8], lhsT=tri[dr][:], rhs=lf, start=True, stop=True),
                     reads=[B["tri"], BG[c]], writes=[B["pSm"]])
                k.op("dve", lambda h: h.tensor_copy(out=LFB[:], in_=lf.unsqueeze(2).to_broadcast([128, NH, 128])), reads=[BG[c]], writes=[B["LFB"]])
                k.op("dve", lambda h: h.tensor_tensor(out=bias_s[:], in0=ig, in1=pSm[:, 0:8], op=ALU.subtract),
                     reads=[BG[c], B["pSm"]], writes=[B["bias_s"]])
                k.op("act", lambda h: h.activation(out=eb[:], in_=pSm[:, 0:8], func=AF.Exp), reads=[B["pSm"]], writes=[B["eb"]])
                def half_front(h0, hf):
                    pBM, pS = pBMs[hf], pSs[hf]
                    for hh in range(4):
                        k.op("pe", lambda h, hh=hh: h.matmul(out=pBM[:, hh, :], lhsT=LFB[:, h0 + hh, :], rhs=tri[dr][:],
                                                            start=(hh == 0), stop=False),
                             reads=[B["LFB"], B["tri"]], writes=[BpBM[hf]])
                        k.op("pe", lambda h, hh=hh: h.matmul(out=pBM[:, hh, :], lhsT=identf[:], rhs=negm[dr][:],
                                                            start=False, stop=(hh == 3)),
                             reads=[Bidf, B["negm"]], writes=[BpBM[hf]])
                    for hh in range(4):
                        hd_ = h0 + hh
                        j, r = hd_ // 2, hd_ % 2
                        k.op("pe", lambda h, hh=hh, j=j, r=r: h.matmul(
                            out=pS[:, hh, :], lhsT=ktm[r][:, j, :], rhs=QT[:, j, csl],
                            start=(hh == 0), stop=(hh == 3)),
                            reads=[B["ktm"], BQT[c]], writes=[BpS[hf]])
                    for hh in range(4):
                        k.op("act", lambda h, hh=hh: h.activation(out=DT[:, h0 + hh, :], in_=pBM[:, hh, :], func=AF.Exp,
                                                                  bias=bias_s[:, h0 + hh:h0 + hh + 1], scale=1.0),
                             reads=[BpBM[hf], B["bias_s"]], writes=[BDT[hf]])
                    k.op("act", lambda h: h.activation(out=ebL[:, h0:h0 + 4], in_=pBM[:, :, l_last], func=AF.Exp),
                         reads=[BpBM[hf]], writes=[BebL[hf]])
                    k.op("dve", lambda h: h.tensor_tensor(out=SwT[:, h0:h0 + 4, :], in0=pS[:], in1=DT[:, h0:h0 + 4, :], op=ALU.mult),
                         reads=[BpS[hf], BDT[hf]], writes=[BSwT[hf]])

                def half_back(h0, hf):
                    for hh in range(4):
                        hd_ = h0 + hh
                        j, r = hd_ // 2, hd_ % 2
                        k.op("pe", lambda h, hh=hh, hd_=hd_: h.matmul(out=pNi[:, hh, :], lhsT=SwT[:, hd_, :], rhs=vt_[:, hd_, 0:128],
                                                                     start=(hh == 0), stop=(hh == 3)),
                             reads=[BSwT[hf], bvt], writes=[B["pNi"]])
                        k.op("pe", lambda h, hh=hh, hd_=hd_, j=j, r=r: h.matmul(
                            out=pNe[:, hh, :], lhsT=QT[:, j, csl], rhs=Cbf[:, hd_, 0:128],
                            start=(hh == 0), stop=(hh == 3)),
                            reads=[BQT[c], B["Cbf"]], writes=[B["pNe"]])
                        k.op("pe", lambda h, hh=hh, hd_=hd_: h.matmul(out=pSm[:, 8 + hh:9 + hh], lhsT=SwT[:, hd_, :], rhs=ones_c[:],
                                                                     start=True, stop=True),
                             reads=[BSwT[hf], B["ones_c"]], writes=[B["pSm"]])
                        k.op("pe", lambda h, hh=hh, hd_=hd_, j=j, r=r: h.matmul(
                            out=pSm[:, 12 + hh:13 + hh], lhsT=QT[:, j, csl], rhs=Cbf[:, hd_, 128:129],
                            start=True, stop=True),
                            reads=[BQT[c], B["Cbf"]], writes=[B["pSm"]])
                    ebh = eb[:, h0:h0 + 4]
                    k.op("dve", lambda h: h.tensor_tensor(out=tmpn[:], in0=pNe[:], in1=ebh.unsqueeze(2).to_broadcast([128, 4, 128]), op=ALU.mult),
                         reads=[B["pNe"], B["eb"]], writes=[B["tmpn"]])
                    k.op("dve", lambda h: h.tensor_tensor(out=num[:], in0=pNi[:], in1=tmpn[:], op=ALU.add),
                         reads=[B["pNi"], B["tmpn"]], writes=[B["num"]])
                    k.op("dve", lambda h: h.tensor_copy(out=dsm[:], in_=pSm[:, 8:16]), reads=[B["pSm"]], writes=[B["dsm"]])
                    k.op("dve", lambda h: h.tensor_tensor(out=dtm[:], in0=dsm[:, 4:8], in1=ebh, op=ALU.mult), reads=[B["dsm"], B["eb"]], writes=[B["dtm"]])
                    k.op("dve", lambda h: h.tensor_tensor(out=den[:], in0=dtm[:], in1=dsm[:, 0:4], op=ALU.add), reads=[B["dtm"], B["dsm"]], writes=[B["den"]])
                    k.op("dve", lambda h: h.scalar_tensor_tensor(out=dtm[:], in0=den[:], scalar=-1.0, in1=den[:], op0=ALU.mult, op1=ALU.max),
                         reads=[B["den"]], writes=[B["dtm"]])
                    k.op("dve", lambda h: h.tensor_scalar(out=den[:], in0=dtm[:], scalar1=1.0, scalar2=None, op0=ALU.max), reads=[B["dtm"]], writes=[B["den"]])
                    k.op("dve", lambda h: h.reciprocal(out=den[:], in_=den[:]), reads=[B["den"]], writes=[B["den"]])
                    k.op("dve", lambda h: h.tensor_tensor(out=hd[:, h0:h0 + 4, :], in0=num[:], in1=den[:, :].unsqueeze(2).to_broadcast([128, 4, 128]), op=ALU.mult),
                         reads=[B["num"], B["den"]], writes=[bhd])
                half_front(0, 0)
                half_front(4, 1)
                half_back(0, 0)
                half_back(4, 1)
                if "tail" in ML_SKIP:
                    return
                if "kw" not in ML_SKIP:
                  k.op("dve", lambda h: h.tensor_tensor(out=kw[:], in0=kt_[:].rearrange("p (h d) -> p h d", d=64),
                                                      in1=DT[:, :, l_last:l_last + 1].to_broadcast([128, NH, 64]), op=ALU.mult),
                     reads=[bkt, BDT[0], BDT[1]], writes=[B["kw"]])
                kwp = kw[:].rearrange("p (j r) d -> p j (r d)", r=2)
                for hd_ in range(NH if "upd" not in ML_SKIP else 0):
                    j, r = hd_ // 2, hd_ % 2
                    slot = hd_ % 3
                    k.op("pe", lambda h, hd_=hd_, j=j, slot=slot: h.matmul(out=pCu[:, slot, 0:129], lhsT=kwp[:, j, :], rhs=vt_[:, hd_, 0:129],
                                                                        start=True, stop=True),
                         reads=[B["kw"], bvt], writes=[B["pCu"]])
                    rs = slice(r * 64, (r + 1) * 64)
                    k.op("dve", lambda h, hd_=hd_, slot=slot, rs=rs: h.scalar_tensor_tensor(
                        out=Cst[rs, hd_, 0:129], in0=Cst[rs, hd_, 0:129], scalar=ebL[rs, hd_:hd_ + 1], in1=pCu[rs, slot, 0:129],
                        op0=ALU.mult, op1=ALU.add),
                        reads=[B["Cst"], BebL[0], BebL[1], B["pCu"]], writes=[B["Cst"]])
                k.op("act", lambda h: h.copy(out=Cbf[:], in_=Cst[:]), reads=[B["Cst"]], writes=[B["Cbf"]])
                if dr == 0:
                    k.dma("sp", lambda h: h.dma_start(out=HF[csl, :], in_=hd[:].rearrange("p h d -> p (h d)")), reads=[bhd], writes=[BHF])
                    if "dbg_hf" in P:
                        bd = Buf(); P["_dbg_bufs"].append(bd)
                        k.dma("sp", lambda h: h.dma_start(out=P["dbg_hf"][csl, :], in_=hd[:].rearrange("p h d -> p (h d)")), reads=[bhd], writes=[bd])
                    return
                if "dbg_hs" in P:
                    bd = Buf(); P["_dbg_bufs"].append(bd)
                    k.dma("sp", lambda h: h.dma_start(out=P["dbg_hs"][csl, :], in_=hd[:].rearrange("p h d -> p (h d)")), reads=[bhd], writes=[bd])
                if "epi" in ML_SKIP:
                    return
                hs = hd[:].rearrange("p h d -> p (h d)")
                k.op("dve", lambda h: h.tensor_tensor(out=hs, in0=hs, in1=hfl[s2][:], op=ALU.add), reads=[bhd, Bhfl[s2]], writes=[bhd])
                k.op("act", lambda h: h.activation(out=sqh, in_=hs, func=AF.Square), reads=[bhd], writes=[B["LFB"]])
                k.op("dve", lambda h: h.tensor_reduce(out=ssh[:], in_=LFB[:], axis=AX.X, op=ALU.add),
                     reads=[B["LFB"]], writes=[B["ssh"]])
                k.op("act", lambda h: h.activation(out=ssh[:], in_=ssh[:], func=AF.Sqrt, bias=eps_t[:], scale=1.0 / 128),
                     reads=[B["ssh"], Beps], writes=[B["ssh"]])
                k.op("dve", lambda h: h.reciprocal(out=ssh[:], in_=ssh[:]), reads=[B["ssh"]], writes=[B["ssh"]])
                k.op("dve", lambda h: h.tensor_tensor(out=hd[:], in0=hd[:], in1=ssh[:, :].unsqueeze(2).to_broadcast([128, NH, 128]), op=ALU.mult),
                     reads=[bhd, B["ssh"]], writes=[bhd])
                k.op("dve", lambda h: h.tensor_tensor(out=hs, in0=hs, in1=og_bc[:], op=ALU.mult), reads=[bhd, B["og_bc"]], writes=[bhd])
                k.op("dve", lambda h: h.tensor_tensor(out=yb[:], in0=hs, in1=ogl[s2][:], op=ALU.mult), reads=[bhd, Bogl[s2]], writes=[B["yb"]])
                for kc in range(KC):
                    k.op("pe", lambda h, kc=kc: h.transpose(out=pA[:, kc, :], in_=yb[:, kc * 128:(kc + 1) * 128], identity=identb[:]),
                         reads=[B["yb"], Bidb], writes=[B["pA"]])
                k.op("act", lambda h: h.copy(out=ybT[:], in_=pA[:]), reads=[B["pA"]], writes=[B["ybT"]])
                for half in range(2):
                    for kc in range(KC):
                        k.op("pe", lambda h, kc=kc, half=half: h.matmul(out=pY, lhsT=ybT[:, kc, :], rhs=w_out[:, kc, half * 512:(half + 1) * 512],
                                                                        start=(kc == 0), stop=(kc == KC - 1)),
                             reads=[B["ybT"], B["w_out"]], writes=[B["pY"]])
                    k.op("dve", lambda h, half=half: h.tensor_tensor(out=xo[s2][:, half * 512:(half + 1) * 512], in0=pY,
                                                                     in1=xl[s2][:, half * 512:(half + 1) * 512], op=ALU.add),
                         reads=[B["pY"], Bxl[s2]], writes=[Bxo[s2]])
                k.dma("sp", lambda h: h.dma_start(out=XOUT[csl, :], in_=xo[s2][:]), reads=[Bxo[s2]], writes=[Bxout])

            step = 0
            for dr in range(2):
                if ML_STOP == "p1" and dr == 1:
                    break
                if ML_STOP == "p2only" and dr == 0:
                    continue
                k.op("dve", lambda h: h.memset(Cst[:], 0.0), writes=[B["Cst"]])
                k.op("pool", lambda h: h.memset(Cbf[:], 0.0), writes=[B["Cbf"]])
                order = range(NT) if dr == 0 else range(NT - 1, -1, -1)
                for c in order:
                    chunk(dr, c, step)
                    step += 1
        k.barrier()


def _t5_bucket(rel):
    nb = 16
    ret = (rel > 0).astype(np.int32) * nb
    n = np.abs(rel)
    max_exact = nb // 2
    nf = np.maximum(n, 1).astype(np.float32)
    large = max_exact + (np.log(nf / max_exact) / math.log(128 / max_exact) * (nb - max_exact)).astype(np.int32)
    large = np.minimum(large, nb - 1)
    return ret + np.where(n < max_exact, n, large)


def _attn_tables(rel_bias):
    p = np.arange(128)[:, None, None]
    j = np.arange(3)[None, :, None]
    q = np.arange(128)[None, None, :]
    rel = j * 128 + p - 128 - q
    bucket = _t5_bucket(rel)
    bias = rel_bias[bucket]
    bias = np.ascontiguousarray(bias.transpose(0, 1, 3, 2)).astype(np.float32)
    mask = np.where(np.abs(rel) <= 128, 0.0, NEG).astype(np.float32)
    mask = np.ascontiguousarray(np.broadcast_to(mask[:, :, None, :], (128, 3, 16, 128)))
    return bias, mask


N_CORES = 4
N_STAGES = 4
DEBUG = False
ML_SKIP = set()
ML_STOP = None
CHAIN = None
LAST = {}


def build_program(n_stages=None):
    n_stages = N_STAGES if n_stages is None else n_stages
    nc = bass.Bass("TRN2", target_bir_lowering=False)
    P = {}

    def inp(name, shape, dt=F32):
        P[name] = nc.dram_tensor(name, list(shape), dt, kind="ExternalInput").ap()

    inp("x", [S, D])
    inp("attn_norm_g", [1, D])
    inp("attn_w_in", [D, 1536])
    inp("attn_q_norm_g", [1, 64])
    inp("attn_k_norm_g", [1, 64])
    inp("attn_sink", [1, 16])
    inp("attn_w_out", [D, D])
    inp("attn_bias", [128, 3, 16, 128])
    inp("attn_mask", [128, 3, 16, 128])
    inp("ident", [128, 128])
    inp("ustrict", [128, 128])
    inp("iota512", [128, CAP])
    inp("rh_const", [128, NE * NT * 4])
    inp("mlstm_norm_g", [1, D])
    inp("mlstm_w_in", [D, MIN])
    inp("mlstm_gate_bias", [1, 32])
    inp("mlstm_out_norm_g", [1, D])
    inp("mlstm_w_out", [D, D])
    inp("tri_f", [128, 128])
    inp("tri_b", [128, 128])
    inp("negm_f", [128, 128])
    inp("negm_b", [128, 128])
    inp("ffn_norm_g", [2, D])
    inp("router_w", [2, D, NE])
    inp("expert_w1", [2, NE, D, 2 * D])
    inp("expert_w3", [2, NE, D, 2 * D])
    inp("expert_w2", [2, NE, 2 * D, D])
    out = nc.dram_tensor("out", [S, D], F32, kind="ExternalOutput").ap()
    if DEBUG is True:
        for n, shp in [("dbg_aff", [128, 512]), ("dbg_sel", [128, 512]), ("dbg_pos", [128, 512]), ("dbg_lo", [128, 16]),
                       ("dbg_hi", [128, 16]), ("dbg_idx", [128, 64]), ("dbg_pis", [128, 256])]:
            P[n] = nc.dram_tensor(n, shp, F32, kind="ExternalOutput").ap()
    P["_dbg_bufs"] = []
    if DEBUG == "ml":
        for n in ["dbg_hf", "dbg_hs"]:
            P[n] = nc.dram_tensor(n, [S, D], F32, kind="ExternalOutput").ap()
    scr = {}
    for n in ["XA", "XB"]:
        scr[n] = nc.dram_tensor(n, [S, D], F32, kind="Internal").ap()
    scr["XC"] = scr["XA"]
    H = nc.dram_tensor("Hs", [S, D], BF16, kind="Internal").ap()
    P["KS"] = nc.dram_tensor("KS", [S, 512], BF16, kind="Internal").ap()
    P["VS"] = nc.dram_tensor("VS", [S, NH, VW], BF16, kind="Internal").ap()
    P["OGS"] = nc.dram_tensor("OGS", [S, D], BF16, kind="Internal").ap()
    with ExitStack() as st:
        cx = Ctx(nc, st)
        k = cx.k
        Bx = Buf("x")
        chain = [("attn", P["x"], scr["XA"]), ("moe0", scr["XA"], scr["XB"]), ("mlstm", scr["XB"], scr["XC"]),
                 ("moe1", scr["XC"], out)][:n_stages]
        if CHAIN is not None:
            chain = [(CHAIN[0], P["x"], out)]
        chain[-1] = (chain[-1][0], chain[-1][1], out)
        bin_ = Bx
        BH = Buf("H")
        for (name, src, dst) in chain:
            bout = Buf(name + "_out")
            if name == "attn":
                stage_attn(cx, src, dst, bin_, bout, P)
                k.barrier()
            elif name == "moe0":
                stage_moe(cx, src, dst, H, bin_, bout, BH, P, 0)
            elif name == "moe1":
                stage_moe(cx, src, dst, H, bin_, bout, BH, P, 1)
            elif name == "mlstm":
                stage_mlstm(cx, src, dst, bin_, bout, P)
            bin_ = bout
        k.wait_bufs("sp", [bin_] + P["_dbg_bufs"])
        k.emit()
    print("program: insts=%d waits=%d per-engine=%s" % (k.n_inst, k.n_wait, {e: len(k.prog[e]) for e in ENGS}))
    return nc


def _consts():
    ident = np.eye(128, dtype=np.float32)
    ustrict = np.triu(np.ones((128, 128), dtype=np.float32), 1)
    iota512 = np.ascontiguousarray(np.broadcast_to(np.arange(CAP, dtype=np.float32), (128, CAP)))
    rh = np.zeros((128, NE, NT, 4), dtype=np.float32)
    rh[:, :, :, 0] = np.arange(128, dtype=np.float32)[:, None, None]
    rh[:, :, :, 1] = np.arange(NT, dtype=np.float32)[None, None, :]
    tri_f = np.triu(np.ones((128, 128), dtype=np.float32), 0)
    tri_b = np.tril(np.ones((128, 128), dtype=np.float32), 0)
    negm_f = np.where(np.arange(128)[:, None] > np.arange(128)[None, :], NEG, 0.0).astype(np.float32)
    negm_b = np.where(np.arange(128)[:, None] < np.arange(128)[None, :], NEG, 0.0).astype(np.float32)
    return {"ident": ident, "ustrict": ustrict, "iota512": iota512, "rh_const": rh.reshape(128, -1),
            "tri_f": tri_f, "tri_b": tri_b, "negm_f": negm_f, "negm_b": negm_b}


def kernel(**inputs):
    inputs = {k_: np.asarray(v) for k_, v in inputs.items()}
    x = inputs["x"].astype(np.float32, copy=False)
    bias, mask = _attn_tables(inputs["rel_bias"].astype(np.float32))
    consts = _consts()
    bi, bf = inputs["mlstm_b_i"][0].astype(np.float32), inputs["mlstm_b_f"][0].astype(np.float32)
    gate_bias = np.ascontiguousarray(np.concatenate([bi[0], bf[0], bi[1], bf[1]])[None, :])
    nc = build_program()
    in_maps = []
    for c in range(N_CORES):
        m = {
            "x": np.ascontiguousarray(x[c]),
            "attn_norm_g": np.ascontiguousarray(inputs["attn_norm_g"][0:1]),
            "attn_w_in": np.ascontiguousarray(inputs["attn_w_in"][0]),
            "attn_q_norm_g": np.ascontiguousarray(inputs["attn_q_norm_g"][0:1]),
            "attn_k_norm_g": np.ascontiguousarray(inputs["attn_k_norm_g"][0:1]),
            "attn_sink": np.ascontiguousarray(inputs["attn_sink"][0:1]),
            "attn_w_out": np.ascontiguousarray(inputs["attn_w_out"][0]),
            "attn_bias": bias, "attn_mask": mask,
            "mlstm_norm_g": np.ascontiguousarray(inputs["mlstm_norm_g"][0:1]),
            "mlstm_w_in": np.ascontiguousarray(inputs["mlstm_w_in"][0]),
            "mlstm_gate_bias": gate_bias,
            "mlstm_out_norm_g": np.ascontiguousarray(inputs["mlstm_out_norm_g"][0:1]),
            "mlstm_w_out": np.ascontiguousarray(inputs["mlstm_w_out"][0]),
            "ffn_norm_g": inputs["ffn_norm_g"], "router_w": inputs["router_w"],
            "expert_w1": inputs["expert_w1"], "expert_w3": inputs["expert_w3"], "expert_w2": inputs["expert_w2"],
        }
        m.update(consts)
        in_maps.append(m)
    res = run_bass_kernel_spmd(nc, in_maps, core_ids=list(range(N_CORES)))
    LAST["res"] = res.results
    return np.stack([r["out"] for r in res.results], axis=0)
```

```python
import math
from contextlib import ExitStack

import numpy as np
import concourse.bass as bass
import concourse.mybir as mybir
from concourse.bass_utils import run_bass_kernel_spmd

F32 = mybir.dt.float32
BF16 = mybir.dt.bfloat16
I32 = mybir.dt.int32
ALU = mybir.AluOpType
AF = mybir.ActivationFunctionType
AX = mybir.AxisListType

ENGS = ["pe", "dve", "act", "pool", "sp"]

S = 4096
D = 1024
NT = S // 128
KC = D // 128
NEG = -30000.0
EPS = 1e-6


class Buf:
    __slots__ = ("name", "w", "r")

    def __init__(self, name=""):
        self.name = name
        self.w = None
        self.r = []


class KB:
    N_DMA_SEMS = 56
    N_HW_SEMS = 40

    def __init__(self, nc, stack):
        self.nc = nc
        self.stack = stack
        self.handles = {"pe": nc.tensor, "dve": nc.vector, "act": nc.scalar,
                        "pool": nc.gpsimd, "sp": nc.sync}
        self.prog = {e: [] for e in ENGS}
        self.epoch = 0
        self.sems = {}
        self.count = {}
        self._new_epoch_sems()
        self.dma_sems = [stack.enter_context(nc.semaphore("d%d" % i)) for i in range(self.N_DMA_SEMS)]
        self.dma_val = [0] * self.N_DMA_SEMS
        self.dma_next = 0
        self.dma_next_sw = 0
        self.seen = {e: {} for e in ENGS}
        self.n_inst = 0
        self.n_wait = 0
        self._rec = None

    def _new_epoch_sems(self):
        for e in ["pe", "dve", "act", "pool"]:
            self.sems[(e, self.epoch)] = self.stack.enter_context(self.nc.semaphore("s_%s_%d" % (e, self.epoch)))
            self.count[e] = 0

    def _sem_of(self, key):
        return self.sems[key] if isinstance(key, tuple) else self.dma_sems[key]

    def _wait(self, eng, toks):
        need = {}
        for t in toks:
            if t is None:
                continue
            k, v = t
            if isinstance(k, tuple):
                if k[1] < self.epoch:
                    continue
                if k[0] == "pe" and eng == "pe":
                    continue
            if need.get(k, 0) < v:
                need[k] = v
        for k, v in need.items():
            if self.seen[eng].get(k, 0) >= v:
                continue
            self.seen[eng][k] = v
            self.prog[eng].append(("wait", self._sem_of(k), v))
            self.n_wait += 1

    def _deps(self, eng, reads, writes):
        toks = []
        for b in reads:
            toks.append(b.w)
        for b in writes:
            toks.append(b.w)
            for t in b.r:
                if isinstance(t[0], tuple) and t[0][0] == eng:
                    continue
                toks.append(t)
        return toks

    def _commit(self, tok, reads, writes):
        for b in reads:
            b.r.append(tok)
            if len(b.r) > 64:
                best = {}
                for k, v in b.r:
                    if best.get(k, 0) < v:
                        best[k] = v
                b.r = list(best.items())
        for b in writes:
            b.w = tok
            b.r = []
        self.n_inst += 1

    def begin_record(self):
        self._rec = [[]]

    def step(self):
        if self._rec is not None and self._rec[-1]:
            self._rec.append([])

    def end_record(self):
        r, self._rec = self._rec, None
        return [st_ for st_ in r if st_]

    def replay(self, step_):
        for (kind, eng, fn, reads, writes) in step_:
            (self.op if kind == "op" else self.dma)(eng, fn, reads, writes)

    def replay_merged(self, a, b):
        ia = ib = 0
        while ia < len(a) or ib < len(b):
            if ib >= len(b) or (ia < len(a) and ia * len(b) <= ib * len(a)):
                self.replay(a[ia]); ia += 1
            else:
                self.replay(b[ib]); ib += 1

    def op(self, eng, fn, reads=(), writes=()):
        if self._rec is not None:
            self._rec[-1].append(("op", eng, fn, list(reads), list(writes)))
            return None
        self._wait(eng, self._deps(eng, reads, writes))
        self.count[eng] += 1
        key = (eng, self.epoch)
        tok = (key, self.count[eng])
        self.prog[eng].append(("op", fn, self.sems[key], 1))
        self._commit(tok, reads, writes)
        return tok

    def dma(self, eng, fn, reads=(), writes=()):
        if self._rec is not None:
            self._rec[-1].append(("dma", eng, fn, list(reads), list(writes)))
            return None
        if eng == "pool":
            j = self.N_HW_SEMS + self.dma_next_sw
            self.dma_next_sw = (self.dma_next_sw + 1) % (self.N_DMA_SEMS - self.N_HW_SEMS)
        else:
            j = self.dma_next
            self.dma_next = (j + 1) % self.N_HW_SEMS
        toks = self._deps("dma", reads, writes)
        if self.dma_val[j] > 0:
            toks.append((j, self.dma_val[j]))
        self._wait(eng, toks)
        self.dma_val[j] += 16
        tok = (j, self.dma_val[j])
        self.prog[eng].append(("op", fn, self.dma_sems[j], 16))
        self._commit(tok, reads, writes)
        return tok

    def wait_bufs(self, eng, bufs):
        toks = []
        for b in bufs:
            toks.append(b.w)
            toks.extend(b.r)
        self._wait(eng, toks)

    def barrier(self):
        toks = [((e, self.epoch), self.count[e]) for e in ["pe", "dve", "act", "pool"] if self.count[e] > 0]
        toks += [(j, v) for j, v in enumerate(self.dma_val) if v > 0]
        for e in ENGS:
            self._wait(e, [t for t in toks if not (isinstance(t[0], tuple) and t[0][0] == e)])
        self.epoch += 1
        self._new_epoch_sems()

    def emit(self):
        nc = self.nc
        prog = self.prog

        def run(e):
            def body(h):
                for item in prog[e]:
                    if item[0] == "wait":
                        h.wait_ge(item[1], item[2])
                    else:
                        item[1](h).then_inc(item[2], item[3])
            return body

        with nc.Block() as block:
            block.tensor(run("pe"))
            block.vector(run("dve"))
            block.scalar(run("act"))
            block.gpsimd(run("pool"))
            block.sync(run("sp"))


class Ctx:
    def __init__(self, nc, stack):
        self.nc = nc
        self.k = KB(nc, stack)
        self.uid = 0

    def sb(self, st, shape, dt, name=None):
        self.uid += 1
        t = st.enter_context(self.nc.sbuf_tensor("%s_%d" % (name or "t", self.uid), list(shape), dt))
        return t

    def ps(self, st, shape, dt, name=None):
        self.uid += 1
        t = st.enter_context(self.nc.psum_tensor("%s_%d" % (name or "p", self.uid), list(shape), dt))
        return t


def stage_attn(cx, XIN, XOUT, Bxin, Bxout, P):
    nc, k = cx.nc, cx.k
    with ExitStack() as st:
        sb = lambda shape, dt, name=None: cx.sb(st, shape, dt, name)
        ps = lambda shape, dt, name=None: cx.ps(st, shape, dt, name)
        w_in = sb([128, KC, 1536], BF16, "w_in")
        w_out = sb([128, KC, 1024], BF16, "w_out")
        g_bc = sb([128, D], F32, "g_bc")
        ident = sb([128, 128], BF16, "ident")
        BM = sb([128, 3, 16, 128], F32, "BM")
        gqk = sb([128, 20, 64], F32, "gqk")
        esink = sb([128, 16], F32, "esink")
        eps_t = sb([128, 1], F32, "eps")
        V_all = sb([128, NT, 4, 65], BF16, "V_all")
        kT_all = sb([64, 4, S], BF16, "kT_all")
        xt = [sb([128, D], F32, "xt%d" % i) for i in range(3)]
        sq = sb([128, 1280], F32, "sq")
        junk = sb([128, D], BF16, "junk")
        xn = sb([128, D], BF16, "xn")
        xnT = sb([128, KC, 128], BF16, "xnT")
        tmpq = sb([128, 20, 64], F32, "tmpq")
        qkn = sb([128, 20, 64], BF16, "qkn")
        qT = [sb([64, 16, 128], BF16, "qT%d" % i) for i in range(3)]
        tS = [sb([128, 512], F32, "tS%d" % i) for i in range(2)]
        PT = [sb([128, 512], BF16, "PT%d" % i) for i in range(2)]
        ot = sb([128, 16, 64], BF16, "ot")
        otT = sb([128, KC, 128], BF16, "otT")
        x1t = [sb([128, D], F32, "x1t%d" % i) for i in range(2)]
        ss = sb([128, 1], F32, "ss")
        rstd = sb([128, 1], F32, "rstd")
        ssq = sb([128, 20], F32, "ssq")
        rqk = sb([128, 20], F32, "rqk")
        den = sb([128, 4], F32, "den")
        rden = sb([128, 4], F32, "rden")
        pA = ps([128, KC, 128], BF16, "pA")
        pB = ps([128, 512], F32, "pB")
        pC = ps([128, 512], F32, "pC")
        pD = ps([128, 512], F32, "pD")
        pE = ps([64, 8, 128], BF16, "pE")
        pGs = [ps([128, 512], F32, "pG%d" % i) for i in range(2)]
        pH = ps([128, 4, 65], F32, "pH")

        B = {}
        for n in ["w_in", "w_out", "g_bc", "ident", "BM", "gqk", "esink", "eps", "V1", "sq", "junk",
                  "xn", "xnT", "tmpq", "qkn", "ot", "otT", "ss", "rstd", "ssq", "rqk", "den", "rden",
                  "pA", "pB", "pC", "pD", "pE", "pH", "gq_s", "gk_s", "mask_s", "sink_s"]:
            B[n] = Buf(n)
        Bxt = [Buf("xt%d" % i) for i in range(3)]
        BqT = [Buf("qT%d" % i) for i in range(3)]
        BtS = [Buf() for _ in range(2)]
        BpG = [Buf() for _ in range(2)]
        BPT = [Buf() for _ in range(2)]
        Bx1 = [Buf() for _ in range(2)]
        BV = [Buf("V%d" % i) for i in range(NT)]
        BkT = [Buf("kT%d" % i) for i in range(NT)]

        k.dma("pool", lambda h: h.dma_start(out=w_in[:], in_=P["attn_w_in"].rearrange("(kc p) n -> p kc n", p=128)),
              writes=[B["w_in"]])
        k.dma("pool", lambda h: h.dma_start(out=w_out[:], in_=P["attn_w_out"].rearrange("(kc p) n -> p kc n", p=128)),
              writes=[B["w_out"]])
        k.dma("pool", lambda h: h.dma_start(out=ident[:], in_=P["ident"]), writes=[B["ident"]])
        k.dma("sp", lambda h: h.dma_start(out=g_bc[:], in_=P["attn_norm_g"].to_broadcast([128, D])), writes=[B["g_bc"]])
        k.dma("sp", lambda h: h.dma_start(out=BM[:], in_=P["attn_bias"]), writes=[B["BM"]])
        mstage = [xt[0], xt[1], x1t[0], x1t[1], xt[2], sq]
        for j in range(3):
            for hh in range(2):
                buf = mstage[j * 2 + hh]
                bb = Buf()
                k.dma("sp", lambda h, j=j, hh=hh, buf=buf: h.dma_start(
                    out=buf[:, 0:1024], in_=P["attn_mask"][:, j, hh * 8:(hh + 1) * 8, :].rearrange("p h q -> p (h q)")),
                    writes=[bb])
                k.op("dve", lambda h, j=j, hh=hh, buf=buf: h.tensor_tensor(
                    out=BM[:, j, hh * 8:(hh + 1) * 8, :], in0=BM[:, j, hh * 8:(hh + 1) * 8, :],
                    in1=buf[:, 0:1024].rearrange("p (h q) -> p h q", q=128), op=ALU.add),
                    reads=[bb], writes=[B["BM"]])
                for tb in (Bxt + Bx1 + [B["sq"]]):
                    pass
        for tb in Bxt + Bx1 + [B["sq"]]:
            tb.r.append(B["BM"].w)
        gq_s = sb([128, 64], F32, "gq_s")
        gk_s = sb([128, 64], F32, "gk_s")
        k.dma("sp", lambda h: h.dma_start(out=gq_s[:], in_=P["attn_q_norm_g"].to_broadcast([128, 64])), writes=[B["gq_s"]])
        k.dma("sp", lambda h: h.dma_start(out=gk_s[:], in_=P["attn_k_norm_g"].to_broadcast([128, 64])), writes=[B["gk_s"]])
        k.op("dve", lambda h: h.tensor_scalar(out=gqk[:, 0:16, :], in0=gq_s[:, :].unsqueeze(1).to_broadcast([128, 16, 64]),
                                              scalar1=0.125, scalar2=None, op0=ALU.mult),
             reads=[B["gq_s"]], writes=[B["gqk"]])
        k.op("dve", lambda h: h.tensor_copy(out=gqk[:, 16:20, :], in_=gk_s[:, :].unsqueeze(1).to_broadcast([128, 4, 64])),
             reads=[B["gk_s"]], writes=[B["gqk"]])
        k.dma("sp", lambda h: h.dma_start(out=esink[:], in_=P["attn_sink"].to_broadcast([128, 16])), writes=[B["esink"]])
        k.op("act", lambda h: h.activation(out=esink[:], in_=esink[:], func=AF.Exp), reads=[B["esink"]], writes=[B["esink"]])
        k.op("dve", lambda h: h.memset(eps_t[:], EPS), writes=[B["eps"]])
        k.op("pool", lambda h: h.memset(V_all[:, :, :, 64:65], 1.0), writes=[B["V1"]])

        def phase1(i):
            s3 = i % 3
            x_t, bx = xt[s3], Bxt[s3]
            k.dma("sp", lambda h: h.dma_start(out=x_t[:], in_=XIN[i * 128:(i + 1) * 128, :]), reads=[Bxin], writes=[bx])
            k.op("act", lambda h: h.activation(out=junk[:], in_=x_t[:], func=AF.Square, accum_out=ss[:]),
                 reads=[bx], writes=[B["junk"], B["ss"]])
            k.op("act", lambda h: h.activation(out=rstd[:], in_=ss[:], func=AF.Sqrt, bias=eps_t[:], scale=1.0 / D),
                 reads=[B["ss"], B["eps"]], writes=[B["rstd"]])
            k.op("dve", lambda h: h.reciprocal(out=rstd[:], in_=rstd[:]), reads=[B["rstd"]], writes=[B["rstd"]])
            k.op("dve", lambda h: h.scalar_tensor_tensor(out=xn[:], in0=x_t[:], scalar=rstd[:, 0:1], in1=g_bc[:],
                                                         op0=ALU.mult, op1=ALU.mult),
                 reads=[bx, B["rstd"], B["g_bc"]], writes=[B["xn"]])
            k.step()
            for kc in range(KC):
                k.op("pe", lambda h, kc=kc: h.transpose(out=pA[:, kc, :], in_=xn[:, kc * 128:(kc + 1) * 128], identity=ident[:]),
                     reads=[B["xn"], B["ident"]], writes=[B["pA"]])
            k.op("act", lambda h: h.copy(out=xnT[:], in_=pA[:]), reads=[B["pA"]], writes=[B["xnT"]])
            k.step()
            for c, (pp, bn) in enumerate([(pB, "pB"), (pC, "pC"), (pD, "pD")]):
                for kc in range(KC):
                    k.op("pe", lambda h, kc=kc, c=c, pp=pp: h.matmul(
                        out=pp[:], lhsT=xnT[:, kc, :], rhs=w_in[:, kc, c * 512:(c + 1) * 512],
                        start=(kc == 0), stop=(kc == KC - 1)),
                        reads=[B["xnT"], B["w_in"]], writes=[B[bn]])
            k.op("act", lambda h: h.activation(out=sq[:, 0:512], in_=pB[:], func=AF.Square), reads=[B["pB"]], writes=[B["sq"]])
            k.op("act", lambda h: h.activation(out=sq[:, 512:1024], in_=pC[:], func=AF.Square), reads=[B["pC"]], writes=[B["sq"]])
            k.op("act", lambda h: h.activation(out=sq[:, 1024:1280], in_=pD[:, 0:256], func=AF.Square), reads=[B["pD"]], writes=[B["sq"]])
            k.op("dve", lambda h: h.tensor_reduce(out=ssq[:], in_=sq[:].rearrange("p (h d) -> p h d", d=64), axis=AX.X, op=ALU.add),
                 reads=[B["sq"]], writes=[B["ssq"]])
            k.op("act", lambda h: h.activation(out=rqk[:], in_=ssq[:], func=AF.Sqrt, bias=eps_t[:], scale=1.0 / 64),
                 reads=[B["ssq"], B["eps"]], writes=[B["rqk"]])
            k.op("dve", lambda h: h.reciprocal(out=rqk[:], in_=rqk[:]), reads=[B["rqk"]], writes=[B["rqk"]])
            for (pp, bn, h0, nh) in [(pB, "pB", 0, 8), (pC, "pC", 8, 8), (pD, "pD", 16, 4)]:
                k.op("dve", lambda h, pp=pp, h0=h0, nh=nh: h.tensor_tensor(
                    out=tmpq[:, h0:h0 + nh, :], in0=pp[:, 0:nh * 64].rearrange("p (h d) -> p h d", d=64),
                    in1=rqk[:, h0:h0 + nh].unsqueeze(2).to_broadcast([128, nh, 64]), op=ALU.mult),
                    reads=[B[bn], B["rqk"]], writes=[B["tmpq"]])
            k.op("act", lambda h: h.copy(out=V_all[:, i, :, 0:64], in_=pD[:, 256:512].rearrange("p (g d) -> p g d", d=64)),
                 reads=[B["pD"]], writes=[BV[i]])
            k.step()
            k.op("pool", lambda h: h.tensor_tensor(out=qkn[:], in0=tmpq[:], in1=gqk[:], op=ALU.mult),
                 reads=[B["tmpq"], B["gqk"]], writes=[B["qkn"]])
            q_t, bq = qT[i % 3], BqT[i % 3]
            for half in range(2):
                k.step()
                for hh in range(8):
                    k.op("pe", lambda h, half=half, hh=hh: h.transpose(out=pE[:, hh, :], in_=qkn[:, half * 8 + hh, :], identity=ident[:]),
                         reads=[B["qkn"], B["ident"]], writes=[B["pE"]])
                k.op("act", lambda h, half=half: h.copy(out=q_t[:, half * 8:(half + 1) * 8, :], in_=pE[:]),
                     reads=[B["pE"]], writes=[bq])
            k.step()
            for g in range(4):
                k.op("pe", lambda h, g=g: h.transpose(out=pE[:, g, :], in_=qkn[:, 16 + g, :], identity=ident[:]),
                     reads=[B["qkn"], B["ident"]], writes=[B["pE"]])
            k.op("dve", lambda h: h.tensor_copy(out=kT_all[:, :, i * 128:(i + 1) * 128], in_=pE[:, 0:4, :]),
                 reads=[B["pE"]], writes=[BkT[i]])

        cnt = [0]

        def phase2(i):
            q_t, bq = qT[i % 3], BqT[i % 3]
            blocks = [j for j in (i - 1, i, i + 1) if 0 <= j < NT]
            items = [(g, bi, j) for g in range(4) for bi, j in enumerate(blocks)]
            slots = []

            def emit_S(n):
                g, bi, j = items[n]
                c2 = cnt[0] % 2
                cnt[0] += 1
                slots.append(c2)
                k.op("pe", lambda h: h.matmul(
                    out=pGs[c2][:], lhsT=kT_all[:, g, j * 128:(j + 1) * 128],
                    rhs=q_t[:, 4 * g:4 * g + 4, :].rearrange("p h q -> p (h q)"), start=True, stop=True),
                    reads=[BkT[j], bq], writes=[BpG[c2]])
                k.op("dve", lambda h: h.tensor_tensor(
                    out=tS[c2][:], in0=pGs[c2][:], in1=BM[:, j - i + 1, 4 * g:4 * g + 4, :].rearrange("p h q -> p (h q)"), op=ALU.add),
                    reads=[BpG[c2], B["BM"]], writes=[BtS[c2]])
                k.op("act", lambda h: h.activation(out=PT[c2][:], in_=tS[c2][:], func=AF.Exp),
                     reads=[BtS[c2]], writes=[BPT[c2]])

            def emit_O(n):
                g, bi, j = items[n]
                c2 = slots[n]
                for hh in range(4):
                    k.op("pe", lambda h, hh=hh: h.matmul(
                        out=pH[:, hh, :], lhsT=PT[c2][:, hh * 128:(hh + 1) * 128], rhs=V_all[:, j, g, :],
                        start=(bi == 0 and hh == 0), stop=(bi == len(blocks) - 1 and hh == 3)),
                        reads=[BPT[c2], BV[j], B["V1"]], writes=[B["pH"]])
                if bi == len(blocks) - 1:
                    k.op("dve", lambda h: h.tensor_tensor(out=den[:], in0=pH[:, :, 64], in1=esink[:, 4 * g:4 * g + 4], op=ALU.add),
                         reads=[B["pH"], B["esink"]], writes=[B["den"]])
                    k.op("dve", lambda h: h.reciprocal(out=rden[:], in_=den[:]), reads=[B["den"]], writes=[B["rden"]])
                    k.op("dve", lambda h: h.tensor_tensor(
                        out=ot[:, 4 * g:4 * g + 4, :], in0=pH[:, :, 0:64],
                        in1=rden[:, :].unsqueeze(2).to_broadcast([128, 4, 64]), op=ALU.mult),
                        reads=[B["pH"], B["rden"]], writes=[B["ot"]])

            emit_S(0)
            for n in range(len(items)):
                k.step()
                if n + 1 < len(items):
                    emit_S(n + 1)
                emit_O(n)
            otf = ot[:].rearrange("p h d -> p (h d)")
            k.step()
            for kc in range(KC):
                k.op("pe", lambda h, kc=kc: h.transpose(out=pA[:, kc, :], in_=otf[:, kc * 128:(kc + 1) * 128], identity=ident[:]),
                     reads=[B["ot"], B["ident"]], writes=[B["pA"]])
            k.op("act", lambda h: h.copy(out=otT[:], in_=pA[:]), reads=[B["pA"]], writes=[B["otT"]])
            x_t, bx = xt[i % 3], Bxt[i % 3]
            xo, bxo = x1t[i % 2], Bx1[i % 2]
            for c, (pp, bn) in enumerate([(pB, "pB"), (pC, "pC")]):
                k.step()
                for kc in range(KC):
                    k.op("pe", lambda h, kc=kc, c=c, pp=pp: h.matmul(
                        out=pp[:], lhsT=otT[:, kc, :], rhs=w_out[:, kc, c * 512:(c + 1) * 512],
                        start=(kc == 0), stop=(kc == KC - 1)),
                        reads=[B["otT"], B["w_out"]], writes=[B[bn]])
                k.op("dve", lambda h, c=c, pp=pp: h.tensor_tensor(out=xo[:, c * 512:(c + 1) * 512], in0=pp[:],
                                                                  in1=x_t[:, c * 512:(c + 1) * 512], op=ALU.add),
                     reads=[B[bn], bx], writes=[bxo])
            k.dma("sp", lambda h: h.dma_start(out=XOUT[i * 128:(i + 1) * 128, :], in_=xo[:]), reads=[bxo], writes=[Bxout])

        for i in range(NT + 2):
            sa, sb_ = [], []
            if i < NT:
                k.begin_record()
                phase1(i)
                sa = k.end_record()
            if i >= 2:
                k.begin_record()
                phase2(i - 2)
                sb_ = k.end_record()
            k.replay_merged(sa, sb_)
        allb = []
        return allb


NE = 16
CAP = 512
N_BISECT = 32


def stage_moe(cx, XIN, XOUT, H, Bxin, Bxout, BH, P, layer):
    nc, k = cx.nc, cx.k
    W1, W3, W2 = P["expert_w1"], P["expert_w3"], P["expert_w2"]
    with ExitStack() as st0:
        sb0 = lambda shape, dt, name=None: cx.sb(st0, shape, dt, name)
        AFF = sb0([128, NE, NT], F32, "AFF")
        selm = sb0([128, NE, NT], F32, "selm")
        pos = sb0([128, NE, NT], F32, "pos")
        RH = sb0([128, NE, NT, 4], BF16, "RH")
        iota = sb0([128, CAP], F32, "iota")
        identb = sb0([128, 128], BF16, "identb")
        BA, Bsel, Bpos, BRH, Biota, Bidb = Buf("AFF"), Buf("selm"), Buf("pos"), Buf("RH"), Buf("iota"), Buf("identb")
        k.dma("sp", lambda h: h.dma_start(out=iota[:], in_=P["iota512"]), writes=[Biota])
        k.dma("pool", lambda h: h.dma_start(out=identb[:], in_=P["ident"]), writes=[Bidb])
        k.dma("pool", lambda h: h.dma_start(out=RH[:].rearrange("p e i c -> p (e i c)"), in_=P["rh_const"]), writes=[BRH])

        with ExitStack() as st:
            sb = lambda shape, dt, name=None: cx.sb(st, shape, dt, name)
            ps = lambda shape, dt, name=None: cx.ps(st, shape, dt, name)
            g_bc = sb([128, D], F32, "g_bc")
            wr = sb([128, KC, NE], F32, "wr")
            identf = sb([128, 128], F32, "identf")
            onesf = sb([128, 128], F32, "onesf")
            ustr = sb([128, 128], F32, "ustr")
            eps_t = sb([128, 1], F32, "eps")
            ones32 = sb([128, NT], F32, "ones32")
            xt = [sb([128, D], F32, "xt%d" % i) for i in range(2)]
            hn = [sb([128, D], F32, "hn%d" % i) for i in range(2)]
            hb = [sb([128, D], BF16, "hb%d" % i) for i in range(2)]
            hnT = sb([128, KC, 128], F32, "hnT")
            junk = sb([128, D], BF16, "junk")
            ss = sb([128, 1], F32, "ss")
            rstd = sb([128, 1], F32, "rstd")
            mx = sb([128, 1], F32, "mx")
            ex = sb([128, NE], F32, "ex")
            sm = sb([128, 1], F32, "sm")
            lo = sb([128, NE], F32, "lo")
            hi = sb([128, NE], F32, "hi")
            mid = sb([128, NE], F32, "mid")
            cmp_t = sb([128, NE, NT], F32, "cmp")
            cntp = sb([128, NE], F32, "cntp")
            mge = sb([128, NE], mybir.dt.uint32, "mge")
            mlt = sb([128, NE], mybir.dt.uint32, "mlt")
            incl = sb([128, NE, NT], F32, "incl")
            ahi = sb([128, NE, NT], BF16, "ahi")
            pR0 = ps([128, 4, 128], F32, "pR0")
            pR1 = ps([128, 4, 128], F32, "pR1")
            pL_full = ps([128, 512], F32, "pL")
            pCn_full = ps([128, 512], F32, "pCn")
            pL = pL_full[:, 0:NE]
            pCn = pCn_full[:, 0:NE]
            pP = ps([128, NE * NT], F32, "pP")
            B = {n: Buf(n) for n in ["g_bc", "wr", "identf", "onesf", "ustr", "eps", "ones32", "hnT", "junk", "ss", "rstd",
                                     "mx", "ex", "sm", "lo", "hi", "mid", "cmp", "cntp", "mge", "mlt", "incl", "ahi",
                                     "pR0", "pR1", "pL", "pCn", "pP"]}
            Bxt = [Buf() for _ in range(2)]
            Bhn = [Buf() for _ in range(2)]
            Bhb = [Buf() for _ in range(2)]
            k.dma("sp", lambda h: h.dma_start(out=g_bc[:], in_=P["ffn_norm_g"][layer:layer + 1, :].to_broadcast([128, D])), writes=[B["g_bc"]])
            k.dma("sp", lambda h: h.dma_start(out=wr[:], in_=P["router_w"][layer].rearrange("(kc p) e -> p kc e", p=128)), writes=[B["wr"]])
            k.dma("sp", lambda h: h.dma_start(out=identf[:], in_=P["ident"]), writes=[B["identf"]])
            k.dma("sp", lambda h: h.dma_start(out=ustr[:], in_=P["ustrict"]), writes=[B["ustr"]])
            k.op("dve", lambda h: h.memset(onesf[:], 1.0), writes=[B["onesf"]])
            k.op("dve", lambda h: h.memset(ones32[:], 1.0), writes=[B["ones32"]])
            k.op("dve", lambda h: h.memset(eps_t[:], EPS), writes=[B["eps"]])
            def pre_tile(i):
                s2 = i % 2
                x_t, bx = xt[s2], Bxt[s2]
                k.dma("sp", lambda h, x_t=x_t: h.dma_start(out=x_t[:], in_=XIN[i * 128:(i + 1) * 128, :]), reads=[Bxin], writes=[bx])
                k.dma("sp", lambda h, x_t=x_t: h.dma_start(out=XOUT[i * 128:(i + 1) * 128, :], in_=x_t[:]), reads=[bx], writes=[Bxout])
                k.op("act", lambda h, x_t=x_t: h.activation(out=junk[:], in_=x_t[:], func=AF.Square, accum_out=ss[:]),
                     reads=[bx], writes=[B["junk"], B["ss"]])
                k.op("act", lambda h: h.activation(out=rstd[:], in_=ss[:], func=AF.Sqrt, bias=eps_t[:], scale=1.0 / D),
                     reads=[B["ss"], B["eps"]], writes=[B["rstd"]])
                k.op("dve", lambda h: h.reciprocal(out=rstd[:], in_=rstd[:]), reads=[B["rstd"]], writes=[B["rstd"]])
                k.op("dve", lambda h, x_t=x_t, s2=s2: h.scalar_tensor_tensor(out=hn[s2][:], in0=x_t[:], scalar=rstd[:, 0:1], in1=g_bc[:],
                                                                         op0=ALU.mult, op1=ALU.mult),
                     reads=[bx, B["rstd"], B["g_bc"]], writes=[Bhn[s2]])
                k.op("pool", lambda h, s2=s2: h.tensor_copy(out=hb[s2][:], in_=hn[s2][:]), reads=[Bhn[s2]], writes=[Bhb[s2]])
                k.dma("sp", lambda h, s2=s2: h.dma_start(out=H[i * 128:(i + 1) * 128, :], in_=hb[s2][:]), reads=[Bhb[s2]], writes=[BH])

            def pre_tile_back(i):
                s2 = i % 2
                for kc in range(KC):
                    pp, bn = (pR0, "pR0") if kc < 4 else (pR1, "pR1")
                    k.op("pe", lambda h, kc=kc, pp=pp, s2=s2: h.transpose(out=pp[:, kc % 4, :], in_=hn[s2][:, kc * 128:(kc + 1) * 128], identity=identf[:]),
                         reads=[Bhn[s2], B["identf"]], writes=[B[bn]])
                k.op("act", lambda h: h.copy(out=hnT[:, 0:4, :], in_=pR0[:]), reads=[B["pR0"]], writes=[B["hnT"]])
                k.op("dve", lambda h: h.tensor_copy(out=hnT[:, 4:8, :], in_=pR1[:]), reads=[B["pR1"]], writes=[B["hnT"]])
                for kc in range(KC):
                    k.op("pe", lambda h, kc=kc: h.matmul(out=pL[:], lhsT=hnT[:, kc, :], rhs=wr[:, kc, :], start=(kc == 0), stop=(kc == KC - 1)),
                         reads=[B["hnT"], B["wr"]], writes=[B["pL"]])
                k.op("dve", lambda h: h.tensor_reduce(out=mx[:], in_=pL[:], axis=AX.X, op=ALU.max, negate=True),
                     reads=[B["pL"]], writes=[B["mx"]])
                k.op("act", lambda h: h.activation(out=ex[:], in_=pL[:], func=AF.Exp, bias=mx[:], scale=1.0, accum_out=sm[:]),
                     reads=[B["pL"], B["mx"]], writes=[B["ex"], B["sm"]])
                k.op("dve", lambda h: h.reciprocal(out=sm[:], in_=sm[:]), reads=[B["sm"]], writes=[B["sm"]])
                k.op("dve", lambda h, i=i: h.tensor_scalar(out=AFF[:, :, i], in0=ex[:], scalar1=sm[:, 0:1], scalar2=None, op0=ALU.mult),
                     reads=[B["ex"], B["sm"]], writes=[BA])
            pre_tile(0)
            for i in range(NT):
                if i + 1 < NT:
                    pre_tile(i + 1)
                pre_tile_back(i)
            k.op("dve", lambda h: h.memset(lo[:], 0.0), writes=[B["lo"]])
            k.op("dve", lambda h: h.memset(hi[:], 1.0), writes=[B["hi"]])
            for it in range(N_BISECT):
                wdt = 2.0 ** -(it + 1)
                k.op("dve", lambda h, wdt=wdt: h.tensor_scalar(out=mid[:], in0=lo[:], scalar1=wdt, scalar2=None, op0=ALU.add),
                     reads=[B["lo"]], writes=[B["mid"]])
                k.op("dve", lambda h: h.tensor_tensor(out=cmp_t[:], in0=AFF[:], in1=mid[:, :].unsqueeze(2).to_broadcast([128, NE, NT]), op=ALU.is_ge),
                     reads=[BA, B["mid"]], writes=[B["cmp"]])
                k.op("dve", lambda h: h.tensor_reduce(out=cntp[:], in_=cmp_t[:], axis=AX.X, op=ALU.add), reads=[B["cmp"]], writes=[B["cntp"]])
                k.op("pe", lambda h: h.matmul(out=pCn[:], lhsT=onesf[:], rhs=cntp[:], start=True, stop=True),
                     reads=[B["onesf"], B["cntp"]], writes=[B["pCn"]])
                k.op("dve", lambda h, wdt=wdt: h.tensor_scalar(out=hi[:], in0=pCn[:], scalar1=CAP - 0.5, scalar2=wdt, op0=ALU.is_ge, op1=ALU.mult),
                     reads=[B["pCn"]], writes=[B["hi"]])
                k.op("dve", lambda h: h.tensor_tensor(out=lo[:], in0=lo[:], in1=hi[:], op=ALU.add), reads=[B["lo"], B["hi"]], writes=[B["lo"]])
            k.op("dve", lambda h: h.tensor_tensor(out=selm[:], in0=AFF[:], in1=lo[:, :].unsqueeze(2).to_broadcast([128, NE, NT]), op=ALU.is_ge),
                 reads=[BA, B["lo"]], writes=[Bsel])
            for e in range(NE):
                k.op("dve", lambda h, e=e: h.tensor_tensor_scan(out=incl[:, e, :], data0=ones32[:], data1=selm[:, e, :], initial=0.0,
                                                              op0=ALU.mult, op1=ALU.add),
                     reads=[Bsel, B["ones32"]], writes=[B["incl"]])
            k.op("dve", lambda h: h.tensor_tensor(out=incl[:], in0=incl[:], in1=selm[:], op=ALU.subtract), reads=[B["incl"], Bsel], writes=[B["incl"]])
            k.op("pe", lambda h: h.matmul(out=pP[:], lhsT=ustr[:], rhs=selm[:].rearrange("p e i -> p (e i)"), start=True, stop=False),
                 reads=[B["ustr"], Bsel], writes=[B["pP"]])
            k.op("pe", lambda h: h.matmul(out=pP[:], lhsT=onesf[:], rhs=incl[:].rearrange("p e i -> p (e i)"), start=False, stop=True),
                 reads=[B["onesf"], B["incl"]], writes=[B["pP"]])
            k.op("act", lambda h: h.copy(out=pos[:].rearrange("p e i -> p (e i)"), in_=pP[:]), reads=[B["pP"]], writes=[Bpos])
            k.op("dve", lambda h: h.tensor_copy(out=ahi[:], in_=AFF[:]), reads=[BA], writes=[B["ahi"]])
            k.op("dve", lambda h: h.tensor_copy(out=RH[:, :, :, 2], in_=ahi[:]), reads=[B["ahi"]], writes=[BRH])
            k.op("dve", lambda h: h.tensor_tensor(out=RH[:, :, :, 3], in0=AFF[:], in1=ahi[:], op=ALU.subtract), reads=[BA, B["ahi"]], writes=[BRH])
            if "dbg_aff" in P and layer == 0:
                bd = Buf()
                k.dma("sp", lambda h: h.dma_start(out=P["dbg_aff"], in_=AFF[:].rearrange("p e i -> p (e i)")), reads=[BA], writes=[bd])
                k.dma("sp", lambda h: h.dma_start(out=P["dbg_sel"], in_=selm[:].rearrange("p e i -> p (e i)")), reads=[Bsel], writes=[bd])
                k.dma("sp", lambda h: h.dma_start(out=P["dbg_pos"], in_=pos[:].rearrange("p e i -> p (e i)")), reads=[Bpos], writes=[bd])
                k.dma("sp", lambda h: h.dma_start(out=P["dbg_lo"], in_=lo[:]), reads=[B["lo"]], writes=[bd])
                k.dma("sp", lambda h: h.dma_start(out=P["dbg_hi"], in_=hi[:]), reads=[B["hi"]], writes=[bd])
                P["_dbg_bufs"].append(bd)
        k.barrier()

        with ExitStack() as st:
            sb = lambda shape, dt, name=None: cx.sb(st, shape, dt, name)
            ps = lambda shape, dt, name=None: cx.ps(st, shape, dt, name)
            NCH = 4
            w1c = [sb([128, KC, 512], BF16, "w1c%d" % c) for c in range(NCH)]
            w3c = [sb([128, KC, 512], BF16, "w3c%d" % c) for c in range(NCH)]
            w2c = [sb([128, 4, D], BF16, "w2c%d" % c) for c in range(NCH)]
            Bw1 = [Buf() for _ in range(NCH)]
            Bw3 = [Buf() for _ in range(NCH)]
            Bw2 = [Buf() for _ in range(NCH)]
            Pm = [sb([128, CAP], BF16, "Pm%d" % c) for c in range(4)]
            BPm = [Buf() for _ in range(4)]
            idxf = sb([128, 4], F32, "idxf")
            pIs = sb([128, 4, 4], F32, "pIs")
            BpIs = Buf()
            idxi = [sb([128, 4], I32, "idxi%d" % c) for c in range(2)]
            gt = [sb([128, 4], F32, "gt%d" % c) for c in range(2)]
            Bidxf = Buf()
            Bidx = [Buf() for _ in range(2)]
            Bgt = [Buf() for _ in range(2)]
            xs = [sb([128, D], BF16, "xs%d" % c) for c in range(4)]
            Bxs = [Buf() for _ in range(4)]
            xsT = [sb([128, KC, CAP], BF16, "xsT%d" % c) for c in range(2)]
            BxsT = [Buf() for _ in range(2)]
            actT = sb([128, 16, CAP], BF16, "actT")
            BactT = [Buf() for _ in range(16)]
            sa = [sb([128, CAP], F32, "sa%d" % c) for c in range(2)]
            Bsa = [Buf() for _ in range(2)]
            yt = [sb([128, D], F32, "yt%d" % c) for c in range(2)]
            Byt = [Buf() for _ in range(2)]
            pT = ps([128, KC, 128], BF16, "pT")
            pI = ps([128, 4, 4], F32, "pI")
            pa = [ps([128, CAP], F32, "pa%d" % c) for c in range(2)]
            pu = [ps([128, CAP], F32, "pu%d" % c) for c in range(2)]
            py = [ps([128, 512], F32, "py%d" % c) for c in range(2)]
            BpT, BpI = Buf(), Buf()
            Bpa = [Buf() for _ in range(2)]
            Bpu = [Buf() for _ in range(2)]
            Bpy = [Buf() for _ in range(2)]
            cnt = {"stg": 0, "cast": 0, "y": 0}
            cast_engs = ["act", "dve", "act", "pool"]

            def load_chunk(src_ap, dst, bdst):
                k.dma("pool", lambda h: h.dma_start(out=dst[:], in_=src_ap), writes=[bdst])

            def load_w13(e, c):
                load_chunk(W1[layer, e].rearrange("(kc p) f -> p kc f", p=128)[:, :, c * 512:(c + 1) * 512], w1c[c], Bw1[c])
                load_chunk(W3[layer, e].rearrange("(kc p) f -> p kc f", p=128)[:, :, c * 512:(c + 1) * 512], w3c[c], Bw3[c])

            def load_w2(e, c):
                load_chunk(W2[layer, e].rearrange("(fc p) d -> p fc d", p=128)[:, c * 4:(c + 1) * 4, :], w2c[c], Bw2[c])

            def build_steps(e):
                par = e % 2
                stepsA, stepsB = [], []

                def emit_pm(i):
                    pm, bpm = Pm[i % 4], BPm[i % 4]
                    k.op("dve", lambda h: h.tensor_scalar(out=pm[:], in0=iota[:], scalar1=pos[:, e, i:i + 1], scalar2=selm[:, e, i:i + 1],
                                                          op0=ALU.is_equal, op1=ALU.mult),
                         reads=[Biota, Bpos, Bsel], writes=[bpm])

                def step_i(i):
                    def f():
                        if i == 0:
                            emit_pm(0)
                            emit_pm(1)
                        if i + 2 < NT:
                            emit_pm(i + 2)
                        pm, bpm = Pm[i % 4], BPm[i % 4]
                        for c in range(4):
                            k.op("pe", lambda h, c=c: h.matmul(out=pI[:, c, :], lhsT=pm[:, c * 128:(c + 1) * 128], rhs=RH[:, e, i, :],
                                                               start=(i == 0 and c == 0), stop=(i == NT - 1 and c == 3)),
                                 reads=[bpm, BRH], writes=[BpI])
                    return f

                for i in range(NT):
                    stepsA.append(step_i(i))

                def fin_a():
                    k.op("dve", lambda h: h.tensor_copy(out=pIs[:], in_=pI[:]), reads=[BpI], writes=[BpIs])
                    k.op("dve", lambda h: h.scalar_tensor_tensor(out=idxf[:], in0=pIs[:, :, 1], scalar=128.0, in1=pIs[:, :, 0],
                                                                 op0=ALU.mult, op1=ALU.add),
                         reads=[BpIs], writes=[Bidxf])
                    k.op("dve", lambda h: h.tensor_copy(out=idxi[par][:], in_=idxf[:]), reads=[Bidxf], writes=[Bidx[par]])
                    if "dbg_idx" in P and layer == 0:
                        bd = Buf()
                        k.dma("sp", lambda h: h.dma_start(out=P["dbg_idx"][:, e * 4:(e + 1) * 4], in_=idxf[:]), reads=[Bidxf], writes=[bd])
                        k.dma("sp", lambda h: h.dma_start(out=P["dbg_pis"][:, e * 16:(e + 1) * 16], in_=pIs[:].rearrange("p a b -> p (a b)")), reads=[BpIs], writes=[bd])
                        P["_dbg_bufs"].append(bd)
                    k.op("dve", lambda h: h.tensor_tensor(out=gt[par][:], in0=pIs[:, :, 2], in1=pIs[:, :, 3], op=ALU.add),
                         reads=[BpIs], writes=[Bgt[par]])
                    for c in range(4):
                        k.dma("pool", lambda h, c=c: h.indirect_dma_start(
                            out=xs[c][:], out_offset=None, in_=H, in_offset=bass.IndirectOffsetOnAxis(ap=idxi[par][:, c:c + 1], axis=0)),
                            reads=[BH, Bidx[par]], writes=[Bxs[c]])
                stepsA.append(fin_a)

                def fin_b(c):
                    def f():
                        for kc in range(KC):
                            k.op("pe", lambda h, kc=kc: h.transpose(out=pT[:, kc, :], in_=xs[c][:, kc * 128:(kc + 1) * 128], identity=identb[:]),
                                 reads=[Bxs[c], Bidb], writes=[BpT])
                        k.op("act", lambda h: h.copy(out=xsT[par][:, :, c * 128:(c + 1) * 128], in_=pT[:]),
                             reads=[BpT], writes=[BxsT[par]])
                    return f
                for c in range(4):
                    stepsB.append(fin_b(c))
                return stepsA, stepsB

            def ffn(e, inter, interB):
                par = e % 2
                n_inter = len(inter)
                done = 0
                doneB = 0
                for fc in range(16):
                    c = fc // 4
                    s2 = fc % 2
                    for (wc, bw, pp, bp) in [(w1c[c], Bw1[c], pa[s2], Bpa[s2]), (w3c[c], Bw3[c], pu[s2], Bpu[s2])]:
                        for kc in range(KC):
                            k.op("pe", lambda h, kc=kc, wc=wc, pp=pp, fc=fc: h.matmul(
                                out=pp[:], lhsT=wc[:, kc, (fc % 4) * 128:(fc % 4 + 1) * 128], rhs=xsT[par][:, kc, :],
                                start=(kc == 0), stop=(kc == KC - 1)),
                                reads=[bw, BxsT[par]], writes=[bp])
                    k.op("act", lambda h, s2=s2: h.activation(out=sa[s2][:], in_=pa[s2][:], func=AF.Silu), reads=[Bpa[s2]], writes=[Bsa[s2]])
                    k.op("dve", lambda h, s2=s2, fc=fc: h.tensor_tensor(out=actT[:, fc, :], in0=sa[s2][:], in1=pu[s2][:], op=ALU.mult),
                         reads=[Bsa[s2], Bpu[s2]], writes=[BactT[fc]])
                    if fc % 4 == 3 and e + 1 < NE:
                        load_w13(e + 1, c)
                    target = (n_inter * (fc + 1)) // 16
                    while done < target:
                        inter[done]()
                        done += 1
                for c in range(4):
                    for dc in range(2):
                        q = cnt["y"] % 2
                        cnt["y"] += 1
                        for fc in range(16):
                            k.op("pe", lambda h, fc=fc, c=c, dc=dc, q=q: h.matmul(
                                out=py[q][:], lhsT=actT[:, fc, c * 128:(c + 1) * 128], rhs=w2c[fc // 4][:, fc % 4, dc * 512:(dc + 1) * 512],
                                start=(fc == 0), stop=(fc == 15)),
                                reads=[BactT[fc], Bw2[fc // 4]], writes=[Bpy[q]])
                        eng = "act" if dc == 0 else "dve"
                        if eng == "act":
                            k.op("act", lambda h, c=c, dc=dc, q=q: h.activation(out=yt[c % 2][:, dc * 512:(dc + 1) * 512], in_=py[q][:], func=AF.Copy,
                                                                                  scale=gt[par][:, c:c + 1]),
                                 reads=[Bpy[q], Bgt[par]], writes=[Byt[c % 2]])
                        else:
                            k.op("dve", lambda h, c=c, dc=dc, q=q: h.tensor_scalar(out=yt[c % 2][:, dc * 512:(dc + 1) * 512], in0=py[q][:],
                                                                                    scalar1=gt[par][:, c:c + 1], scalar2=None, op0=ALU.mult),
                                 reads=[Bpy[q], Bgt[par]], writes=[Byt[c % 2]])
                    if doneB < len(interB):
                        interB[doneB]()
                        doneB += 1
                    k.dma("pool", lambda h, c=c: h.indirect_dma_start(
                        out=XOUT, out_offset=bass.IndirectOffsetOnAxis(ap=idxi[par][:, c:c + 1], axis=0),
                        in_=yt[c % 2][:], in_offset=None, compute_op=ALU.add),
                        reads=[Byt[c % 2], Bidx[par]], writes=[Bxout])
                    if c == 3 and e + 1 < NE:
                        for cc in range(NCH):
                            load_w2(e + 1, cc)

            for c in range(NCH):
                load_w13(0, c)
            for c in range(NCH):
                load_w2(0, c)
            sA, sB = build_steps(0)
            for f in sA + sB:
                f()
            for e in range(NE):
                sA, sB = build_steps(e + 1) if e + 1 < NE else ([], [])
                ffn(e, sA, sB)
        k.barrier()


NH = 8
MIN = 3104
VW = 132


def stage_mlstm(cx, XIN, XOUT, Bxin, Bxout, P):
    nc, k = cx.nc, cx.k
    W = P["mlstm_w_in"]
    KS, VS, OGS = P["KS"], P["VS"], P["OGS"]
    HF = XOUT
    BKS, BVS, BOGS, BHF = Buf("KS"), Buf("VS"), Buf("OGS"), Buf("HF")
    with ExitStack() as st0:
        sb0 = lambda shape, dt, name=None: cx.sb(st0, shape, dt, name)
        QT = sb0([128, 4, S], BF16, "QT")
        KT = sb0([128, 4, S], BF16, "KT")
        G = sb0([128, NT, 32], F32, "G")
        identb = sb0([128, 128], BF16, "identb")
        identf = sb0([128, 128], F32, "identf")
        eps_t = sb0([128, 1], F32, "eps")
        one_t = sb0([128, 1], F32, "one")
        g_bc = sb0([128, D], F32, "g_bc")
        BQT = [Buf() for _ in range(NT)]
        BKT = [Buf() for _ in range(NT)]
        BG = [Buf() for _ in range(NT)]
        Bidb, Bidf, Beps, Bone, Bg = Buf(), Buf(), Buf(), Buf(), Buf()
        k.dma("pool", lambda h: h.dma_start(out=identb[:], in_=P["ident"]), writes=[Bidb])
        k.dma("sp", lambda h: h.dma_start(out=identf[:], in_=P["ident"]), writes=[Bidf])
        k.dma("sp", lambda h: h.dma_start(out=g_bc[:], in_=P["mlstm_norm_g"].to_broadcast([128, D])), writes=[Bg])
        k.op("dve", lambda h: h.memset(eps_t[:], EPS), writes=[Beps])
        k.op("dve", lambda h: h.memset(one_t[:], 1.0), writes=[Bone])

        with ExitStack() as st:
            sb = lambda shape, dt, name=None: cx.sb(st, shape, dt, name)
            ps = lambda shape, dt, name=None: cx.ps(st, shape, dt, name)
            w_in = sb([128, KC, MIN], BF16, "w_in")
            bgate = sb([128, 32], F32, "bgate")
            xt = [sb([128, D], F32, "xt%d" % i) for i in range(2)]
            junk = sb([128, D], BF16, "junk")
            xn = sb([128, D], BF16, "xn")
            xnT2 = [sb([128, KC, 128], BF16, "xnT%d" % i) for i in range(2)]
            BxnT2 = [Buf() for _ in range(2)]
            ss = sb([128, 1], F32, "ss")
            rstd = sb([128, 1], F32, "rstd")
            ktok = [sb([128, 512], BF16, "ktok%d" % i) for i in range(2)]
            vtile = [sb([128, NH, VW], BF16, "vt%d" % i) for i in range(2)]
            ogt = [sb([128, D], BF16, "ogt%d" % i) for i in range(2)]
            zt = sb([128, 32], F32, "zt")
            et = sb([128, 32], F32, "et")
            pA = ps([128, KC, 128], BF16, "pA")
            pQ = ps([128, 4, 128], F32, "pQ")
            pK = ps([128, 4, 128], F32, "pK")
            pTk = ps([128, 512], F32, "pTk")
            pV0 = ps([128, 512], F32, "pV0")
            pV1 = ps([128, 512], F32, "pV1")
            pGf = ps([128, 512], F32, "pG")
            pG = pGf[:, 0:32]
            B = {n: Buf(n) for n in ["w_in", "bgate", "junk", "xn", "xnT", "ss", "rstd", "zt", "et",
                                     "pA", "pQ", "pK", "pTk", "pV0", "pV1", "pG"]}
            Bxt = [Buf() for _ in range(2)]
            Bkt = [Buf() for _ in range(2)]
            Bvt = [Buf() for _ in range(2)]
            Bog = [Buf() for _ in range(2)]
            wv = W.rearrange("(kc p) n -> p kc n", p=128)
            for c0 in range(0, MIN, 776):
                k.dma("pool", lambda h, c0=c0: h.dma_start(out=w_in[:, :, c0:c0 + 776], in_=wv[:, :, c0:c0 + 776]), writes=[B["w_in"]])
            k.dma("sp", lambda h: h.dma_start(out=bgate[:], in_=P["mlstm_gate_bias"].to_broadcast([128, 32])), writes=[B["bgate"]])
            for i2 in range(2):
                k.op("pool", lambda h, i2=i2: h.memset(vtile[i2][:], 0.0), writes=[Bvt[i2]])
                k.op("pool", lambda h, i2=i2: h.memset(vtile[i2][:, :, 128:129], 1.0), writes=[Bvt[i2]])

            def p0_tile(i):
                s2 = i % 2
                x_t, bx = xt[s2], Bxt[s2]
                k.dma("sp", lambda h: h.dma_start(out=x_t[:], in_=XIN[i * 128:(i + 1) * 128, :]), reads=[Bxin], writes=[bx])
                k.op("act", lambda h: h.activation(out=junk[:], in_=x_t[:], func=AF.Square, accum_out=ss[:]),
                     reads=[bx], writes=[B["junk"], B["ss"]])
                k.op("act", lambda h: h.activation(out=rstd[:], in_=ss[:], func=AF.Sqrt, bias=eps_t[:], scale=1.0 / D),
                     reads=[B["ss"], Beps], writes=[B["rstd"]])
                k.op("dve", lambda h: h.reciprocal(out=rstd[:], in_=rstd[:]), reads=[B["rstd"]], writes=[B["rstd"]])
                k.op("dve", lambda h: h.scalar_tensor_tensor(out=xn[:], in0=x_t[:], scalar=rstd[:, 0:1], in1=g_bc[:],
                                                             op0=ALU.mult, op1=ALU.mult),
                     reads=[bx, B["rstd"], Bg], writes=[B["xn"]])
                for kc in range(KC):
                    k.op("pe", lambda h, kc=kc: h.transpose(out=pA[:, kc, :], in_=xn[:, kc * 128:(kc + 1) * 128], identity=identb[:]),
                         reads=[B["xn"], Bidb], writes=[B["pA"]])
                k.op("act", lambda h: h.copy(out=xnT2[s2][:], in_=pA[:]), reads=[B["pA"]], writes=[BxnT2[s2]])

            def p0_back(i):
                s2 = i % 2
                xnT = xnT2[s2]
                B["xnT"] = BxnT2[s2]
                for (pp, bn, col0) in [(pQ, "pQ", 0), (pK, "pK", 512)]:
                    for j in range(4):
                        for kc in range(KC):
                            k.op("pe", lambda h, kc=kc, j=j, pp=pp, col0=col0: h.matmul(
                                out=pp[:, j, :], lhsT=w_in[:, kc, col0 + j * 128:col0 + (j + 1) * 128], rhs=xnT[:, kc, :],
                                start=(kc == 0), stop=(kc == KC - 1)),
                                reads=[B["w_in"], B["xnT"]], writes=[B[bn]])
                k.op("act", lambda h: h.copy(out=QT[:, :, i * 128:(i + 1) * 128], in_=pQ[:]), reads=[B["pQ"]], writes=[BQT[i]])
                k.op("act", lambda h: h.activation(out=KT[:, :, i * 128:(i + 1) * 128], in_=pK[:], func=AF.Copy, scale=0.125),
                     reads=[B["pK"]], writes=[BKT[i]])
                for kc in range(KC):
                    k.op("pe", lambda h, kc=kc: h.matmul(out=pTk[:], lhsT=xnT[:, kc, :], rhs=w_in[:, kc, 512:1024],
                                                         start=(kc == 0), stop=(kc == KC - 1)),
                         reads=[B["w_in"], B["xnT"]], writes=[B["pTk"]])
                k.op("dve", lambda h: h.tensor_scalar(out=ktok[s2][:], in0=pTk[:], scalar1=0.125, scalar2=None, op0=ALU.mult),
                     reads=[B["pTk"]], writes=[Bkt[s2]])
                k.dma("sp", lambda h: h.dma_start(out=KS[i * 128:(i + 1) * 128, :], in_=ktok[s2][:]), reads=[Bkt[s2]], writes=[BKS])
                for half, (pp, bn) in enumerate([(pV0, "pV0"), (pV1, "pV1")]):
                    for kc in range(KC):
                        k.op("pe", lambda h, kc=kc, pp=pp, half=half: h.matmul(
                            out=pp[:], lhsT=xnT[:, kc, :], rhs=w_in[:, kc, 1024 + half * 512:1024 + (half + 1) * 512],
                            start=(kc == 0), stop=(kc == KC - 1)),
                            reads=[B["w_in"], B["xnT"]], writes=[B[bn]])
                    eng = "act" if half == 0 else "dve"
                    if eng == "act":
                        k.op("act", lambda h, pp=pp, half=half: h.copy(out=vtile[s2][:, half * 4:(half + 1) * 4, 0:128],
                                                                       in_=pp[:].rearrange("p (h d) -> p h d", d=128)),
                             reads=[B[bn]], writes=[Bvt[s2]])
                    else:
                        k.op("dve", lambda h, pp=pp, half=half: h.tensor_copy(out=vtile[s2][:, half * 4:(half + 1) * 4, 0:128],
                                                                              in_=pp[:].rearrange("p (h d) -> p h d", d=128)),
                             reads=[B[bn]], writes=[Bvt[s2]])
                k.dma("sp", lambda h: h.dma_start(out=VS[i * 128:(i + 1) * 128, :, :], in_=vtile[s2][:]), reads=[Bvt[s2]], writes=[BVS])
                for half, (pp, bn) in enumerate([(pV0, "pV0"), (pV1, "pV1")]):
                    for kc in range(KC):
                        k.op("pe", lambda h, kc=kc, pp=pp, half=half: h.matmul(
                            out=pp[:], lhsT=xnT[:, kc, :], rhs=w_in[:, kc, 2048 + half * 512:2048 + (half + 1) * 512],
                            start=(kc == 0), stop=(kc == KC - 1)),
                            reads=[B["w_in"], B["xnT"]], writes=[B[bn]])
                    k.op("act", lambda h, pp=pp, half=half: h.activation(out=ogt[s2][:, half * 512:(half + 1) * 512], in_=pp[:], func=AF.Sigmoid),
                         reads=[B[bn]], writes=[Bog[s2]])
                k.dma("sp", lambda h: h.dma_start(out=OGS[i * 128:(i + 1) * 128, :], in_=ogt[s2][:]), reads=[Bog[s2]], writes=[BOGS])
                for kc in range(KC):
                    k.op("pe", lambda h, kc=kc: h.matmul(out=pG, lhsT=xnT[:, kc, :], rhs=w_in[:, kc, 3072:3104],
                                                         start=(kc == 0), stop=(kc == KC - 1)),
                         reads=[B["w_in"], B["xnT"]], writes=[B["pG"]])
                k.op("dve", lambda h: h.tensor_tensor(out=zt[:], in0=pG, in1=bgate[:], op=ALU.add), reads=[B["pG"], B["bgate"]], writes=[B["zt"]])
                k.op("act", lambda h: h.activation(out=et[:], in_=zt[:], func=AF.Exp, scale=-1.0), reads=[B["zt"]], writes=[B["et"]])
                k.op("act", lambda h: h.activation(out=et[:], in_=et[:], func=AF.Ln, bias=one_t[:], scale=1.0), reads=[B["et"], Bone], writes=[B["et"]])
                zv = zt[:].rearrange("p (a b) -> p a b", b=8)
                ev = et[:].rearrange("p (a b) -> p a b", b=8)
                gv = G[:, i, :].rearrange("p (a b) -> p a b", b=8)
                k.op("dve", lambda h: h.tensor_copy(out=gv[:, 0:4:2, :], in_=zv[:, 0:4:2, :]), reads=[B["zt"]], writes=[BG[i]])
                k.op("dve", lambda h: h.tensor_scalar(out=gv[:, 1:4:2, :], in0=ev[:, 1:4:2, :], scalar1=-1.0, scalar2=None, op0=ALU.mult),
                     reads=[B["et"]], writes=[BG[i]])

            p0_tile(0)
            for i in range(NT):
                if i + 1 < NT:
                    p0_tile(i + 1)
                p0_back(i)
        k.barrier()
        if ML_STOP == "p0":
            return

        with ExitStack() as st:
            sb = lambda shape, dt, name=None: cx.sb(st, shape, dt, name)
            ps = lambda shape, dt, name=None: cx.ps(st, shape, dt, name)
            HB = P["HB"]
            BHB = Buf("HB")
            tri = [sb([128, 128], F32, "tri%d" % d_) for d_ in range(2)]
            negm = [sb([128, 128], F32, "negm%d" % d_) for d_ in range(2)]
            ones_c = sb([128, 1], BF16, "ones_c")
            w_out = sb([128, KC, D], BF16, "w_out")
            og_bc = sb([128, D], F32, "og_bc")
            tmpn = sb([128, 4, 128], F32, "tmpn")
            dsm = sb([128, 16], F32, "dsm")
            dtm = sb([128, NH], F32, "dtm")
            den = sb([128, NH], F32, "den")
            kw = sb([128, NH, 64], BF16, "kw")
            ssh = sb([128, NH], F32, "ssh")
            yb = sb([128, D], BF16, "yb")
            ybT = sb([128, KC, 128], BF16, "ybT")
            xo = sb([128, D], F32, "xo")
            def per_dir(shape, dt, name):
                return [sb(shape, dt, "%s_%d" % (name, d_)) for d_ in range(2)]
            Cst_d = per_dir([128, NH, VW], F32, "Cst")
            Cbf_d = per_dir([128, NH, VW], BF16, "Cbf")
            LFB_d = per_dir([128, NH, 128], F32, "LFB")
            bias_d = per_dir([128, NH], F32, "bias_s")
            eb_d = per_dir([128, NH], F32, "eb")
            ebL_d = per_dir([128, NH], F32, "ebL")
            DT_d = per_dir([128, NH, 128], F32, "DT")
            SwT_d = per_dir([128, NH, 128], BF16, "SwT")
            hd_d = [[sb([128, NH, 128], F32, "hdir%d%d" % (d_, i)) for i in range(2)] for d_ in range(2)]
            ktl_d = [[sb([128, 512], BF16, "ktl%d%d" % (d_, i)) for i in range(2)] for d_ in range(2)]
            vtl_d = [[sb([128, NH, VW], BF16, "vtl%d%d" % (d_, i)) for i in range(2)] for d_ in range(2)]
            hfl_s = sb([128, D], F32, "hfl")
            ogl_s = sb([128, D], BF16, "ogl")
            xl_s = sb([128, D], F32, "xl")
            ktm_d = [[sb([128, 4, 128], BF16, "ktm%d%d" % (d_, i)) for i in range(2)] for d_ in range(2)]
            pSs = [ps([128, 4, 128], F32, "pS%d" % i) for i in range(2)]
            pBMs = [ps([128, 4, 128], F32, "pBM%d" % i) for i in range(2)]
            pNi = ps([128, 4, 128], F32, "pNi")
            pNe = ps([128, 4, 128], F32, "pNe")
            pSmCu = ps([128, 512], F32, "pSmCu")
            pCu = pSmCu[:, 0:3 * VW].rearrange("p (a b) -> p a b", b=VW)
            pSm_d = [pSmCu[:, 400 + 24 * d_:424 + 24 * d_] for d_ in range(2)]
            pA = ps([128, KC, 128], BF16, "pA")
            pY = pNi[:].rearrange("p a b -> p (a b)")
            Bsh = {n: Buf(n) for n in ["tri", "negm", "ones_c", "w_out", "og_bc", "tmpn", "dsm", "dtm", "den", "kw", "ssh",
                                       "yb", "ybT", "xo", "pNi", "pNe", "pSm", "pA", "hfl", "ogl", "xl"]}
            Bsh["pCu"] = Bsh["pSm"]
            Bsh["pY"] = Bsh["pNi"]
            Bd = []
            for d_ in range(2):
                bb = dict(Bsh)
                for n in ["Cst", "Cbf", "LFB", "bias_s", "eb", "ktm", "pS", "pBM"]:
                    bb[n] = Buf(n + str(d_))
                bb["hd"] = [Buf(), Buf()]
                bb["Csth"] = [Buf() for _ in range(NH)]
                bb["DT"] = [Buf(), Buf()]
                bb["SwT"] = [Buf(), Buf()]
                bb["ebL"] = [Buf(), Buf()]
                bb["ktl"] = [Buf(), Buf()]
                bb["vtl"] = [Buf(), Buf()]
                Bd.append(bb)
            k.dma("sp", lambda h: h.dma_start(out=tri[0][:], in_=P["tri_f"]), writes=[Bsh["tri"]])
            k.dma("sp", lambda h: h.dma_start(out=tri[1][:], in_=P["tri_b"]), writes=[Bsh["tri"]])
            k.dma("sp", lambda h: h.dma_start(out=negm[0][:], in_=P["negm_f"]), writes=[Bsh["negm"]])
            k.dma("sp", lambda h: h.dma_start(out=negm[1][:], in_=P["negm_b"]), writes=[Bsh["negm"]])
            k.dma("pool", lambda h: h.dma_start(out=w_out[:], in_=P["mlstm_w_out"].rearrange("(kc p) n -> p kc n", p=128)), writes=[Bsh["w_out"]])
            k.dma("sp", lambda h: h.dma_start(out=og_bc[:], in_=P["mlstm_out_norm_g"].to_broadcast([128, D])), writes=[Bsh["og_bc"]])
            k.op("pool", lambda h: h.memset(ones_c[:], 1.0), writes=[Bsh["ones_c"]])
            for d_ in range(2):
                k.op("pool", lambda h, d_=d_: h.memset(ktm_d[d_][0][:], 0.0), writes=[Bd[d_]["ktm"]])
                k.op("pool", lambda h, d_=d_: h.memset(ktm_d[d_][1][:], 0.0), writes=[Bd[d_]["ktm"]])
                k.op("dve", lambda h, d_=d_: h.memset(Cst_d[d_][:], 0.0), writes=Bd[d_]["Csth"])
                k.op("pool", lambda h, d_=d_: h.memset(Cbf_d[d_][:], 0.0), writes=[Bd[d_]["Cbf"]])

            def chunk(dr, c, step):
                B = Bd[dr]
                s2 = step % 2
                late = step >= NT // 2
                l_last = 127 if dr == 0 else 0
                Cst, Cbf, LFB, bias_s, eb, ebL = Cst_d[dr], Cbf_d[dr], LFB_d[dr], bias_d[dr], eb_d[dr], ebL_d[dr]
                DT, SwT, hd, ktm = DT_d[dr], SwT_d[dr], hd_d[dr][s2], ktm_d[dr]
                hfl, ogl, xl = hfl_s, ogl_s, xl_s
                pS, pBM = pSs[dr], pBMs[dr]
                pSm = pSm_d[dr]
                lf = G[:, c, 8 + 16 * dr:16 + 16 * dr]
                ig = G[:, c, 16 * dr:8 + 16 * dr]
                kt_, bkt = ktl_d[dr][s2], B["ktl"][s2]
                vt_, bvt = vtl_d[dr][s2], B["vtl"][s2]
                bhd = B["hd"][s2]
                csl = slice(c * 128, (c + 1) * 128)
                k.dma("sp", lambda h: h.dma_start(out=kt_[:], in_=KS[csl, :]), reads=[BKS], writes=[bkt])
                k.dma("sp", lambda h: h.dma_start(out=vt_[:], in_=VS[csl, :, :]), reads=[BVS], writes=[bvt])
                k.op("act", lambda h: h.copy(out=ktm[0][0:64, :, :], in_=KT[0:64, :, csl]), reads=[BKT[c]], writes=[B["ktm"]])
                k.op("pool", lambda h: h.tensor_copy(out=ktm[1][64:128, :, :], in_=KT[64:128, :, csl]), reads=[BKT[c]], writes=[B["ktm"]])
                k.op("pe", lambda h: h.matmul(out=pSm[:, 0:8], lhsT=tri[dr][:], rhs=lf, start=True, stop=True),
                     reads=[B["tri"], BG[c]], writes=[B["pSm"]])
                k.op("dve", lambda h: h.tensor_copy(out=LFB[:], in_=lf.unsqueeze(2).to_broadcast([128, NH, 128])), reads=[BG[c]], writes=[B["LFB"]])
                k.op("dve", lambda h: h.tensor_tensor(out=bias_s[:], in0=ig, in1=pSm[:, 0:8], op=ALU.subtract),
                     reads=[BG[c], B["pSm"]], writes=[B["bias_s"]])
                k.op("act", lambda h: h.activation(out=eb[:], in_=pSm[:, 0:8], func=AF.Exp), reads=[B["pSm"]], writes=[B["eb"]])

                def half_front(h0, hf):
                    k.step()
                    for hh in range(4):
                        k.op("pe", lambda h, hh=hh: h.matmul(out=pBM[:, hh, :], lhsT=LFB[:, h0 + hh, :], rhs=tri[dr][:],
                                                            start=(hh == 0), stop=False),
                             reads=[B["LFB"], B["tri"]], writes=[B["pBM"]])
                        k.op("pe", lambda h, hh=hh: h.matmul(out=pBM[:, hh, :], lhsT=identf[:], rhs=negm[dr][:],
                                                            start=False, stop=(hh == 3)),
                             reads=[Bidf, B["negm"]], writes=[B["pBM"]])
                    for hh in range(4):
                        hd_ = h0 + hh
                        j, r = hd_ // 2, hd_ % 2
                        k.op("pe", lambda h, hh=hh, j=j, r=r: h.matmul(
                            out=pS[:, hh, :], lhsT=ktm[r][:, j, :], rhs=QT[:, j, csl],
                            start=(hh == 0), stop=(hh == 3)),
                            reads=[B["ktm"], BQT[c]], writes=[B["pS"]])
                    for hh in range(4):
                        k.op("act", lambda h, hh=hh: h.activation(out=DT[:, h0 + hh, :], in_=pBM[:, hh, :], func=AF.Exp,
                                                                  bias=bias_s[:, h0 + hh:h0 + hh + 1], scale=1.0),
                             reads=[B["pBM"], B["bias_s"]], writes=[B["DT"][hf]])
                    k.op("act", lambda h: h.activation(out=ebL[:, h0:h0 + 4], in_=pBM[:, :, l_last], func=AF.Exp),
                         reads=[B["pBM"]], writes=[B["ebL"][hf]])
                    k.op("dve", lambda h: h.tensor_tensor(out=SwT[:, h0:h0 + 4, :], in0=pS[:], in1=DT[:, h0:h0 + 4, :], op=ALU.mult),
                         reads=[B["pS"], B["DT"][hf]], writes=[B["SwT"][hf]])

                def half_back(h0, hf):
                    k.step()
                    for hh in range(4):
                        hd_ = h0 + hh
                        j, r = hd_ // 2, hd_ % 2
                        k.op("pe", lambda h, hh=hh, hd_=hd_: h.matmul(out=pNi[:, hh, :], lhsT=SwT[:, hd_, :], rhs=vt_[:, hd_, 0:128],
                                                                     start=(hh == 0), stop=(hh == 3)),
                             reads=[B["SwT"][hf], bvt], writes=[B["pNi"]])
                        k.op("pe", lambda h, hh=hh, hd_=hd_, j=j, r=r: h.matmul(
                            out=pNe[:, hh, :], lhsT=QT[:, j, csl], rhs=Cbf[:, hd_, 0:128],
                            start=(hh == 0), stop=(hh == 3)),
                            reads=[BQT[c], B["Cbf"]], writes=[B["pNe"]])
                        k.op("pe", lambda h, hh=hh, hd_=hd_: h.matmul(out=pSm[:, 8 + hd_:9 + hd_], lhsT=SwT[:, hd_, :], rhs=ones_c[:],
                                                                     start=True, stop=True),
                             reads=[B["SwT"][hf], B["ones_c"]], writes=[B["pSm"]])
                        k.op("pe", lambda h, hh=hh, hd_=hd_, j=j, r=r: h.matmul(
                            out=pSm[:, 16 + hd_:17 + hd_], lhsT=QT[:, j, csl], rhs=Cbf[:, hd_, 128:129],
                            start=True, stop=True),
                            reads=[BQT[c], B["Cbf"]], writes=[B["pSm"]])
                    ebh = eb[:, h0:h0 + 4]
                    k.op("dve", lambda h: h.tensor_tensor(out=tmpn[:], in0=pNe[:], in1=ebh.unsqueeze(2).to_broadcast([128, 4, 128]), op=ALU.mult),
                         reads=[B["pNe"], B["eb"]], writes=[B["tmpn"]])
                    k.op("dve", lambda h: h.tensor_tensor(out=hd[:, h0:h0 + 4, :], in0=pNi[:], in1=tmpn[:], op=ALU.add),
                         reads=[B["pNi"], B["tmpn"]], writes=[bhd])

                def finish_den():
                    k.op("dve", lambda h: h.tensor_copy(out=dsm[:], in_=pSm[:, 8:24]), reads=[B["pSm"]], writes=[B["dsm"]])
                    k.op("dve", lambda h: h.tensor_tensor(out=dtm[:], in0=dsm[:, 8:16], in1=eb[:], op=ALU.mult), reads=[B["dsm"], B["eb"]], writes=[B["dtm"]])
                    k.op("dve", lambda h: h.tensor_tensor(out=den[:], in0=dtm[:], in1=dsm[:, 0:8], op=ALU.add), reads=[B["dtm"], B["dsm"]], writes=[B["den"]])
                    k.op("dve", lambda h: h.scalar_tensor_tensor(out=dtm[:], in0=den[:], scalar=-1.0, in1=den[:], op0=ALU.mult, op1=ALU.max),
                         reads=[B["den"]], writes=[B["dtm"]])
                    k.op("dve", lambda h: h.tensor_scalar(out=den[:], in0=dtm[:], scalar1=1.0, scalar2=None, op0=ALU.max), reads=[B["dtm"]], writes=[B["den"]])
                    k.op("dve", lambda h: h.reciprocal(out=den[:], in_=den[:]), reads=[B["den"]], writes=[B["den"]])
                    k.op("dve", lambda h: h.tensor_tensor(out=hd[:], in0=hd[:], in1=den[:, :].unsqueeze(2).to_broadcast([128, NH, 128]), op=ALU.mult),
                         reads=[bhd, B["den"]], writes=[bhd])

                half_front(0, 0)
                half_back(0, 0)
                half_front(4, 1)
                half_back(4, 1)
                finish_den()
                k.step()
                k.op("dve", lambda h: h.tensor_tensor(out=kw[:], in0=kt_[:].rearrange("p (h d) -> p h d", d=64),
                                                     in1=DT[:, :, l_last:l_last + 1].to_broadcast([128, NH, 64]), op=ALU.mult),
                     reads=[bkt, B["DT"][0], B["DT"][1]], writes=[B["kw"]])
                kwp = kw[:].rearrange("p (j r) d -> p j (r d)", r=2)
                cu_banks = [(pCu, B["pCu"]),
                            (pS[:].rearrange("p a b -> p (a b)")[:, 0:3 * VW].rearrange("p (a b) -> p a b", b=VW), B["pS"]),
                            (pBM[:].rearrange("p a b -> p (a b)")[:, 0:3 * VW].rearrange("p (a b) -> p a b", b=VW), B["pBM"])]
                for hd_ in range(NH):
                    j = hd_ // 2
                    cu, bcu = cu_banks[hd_ // 3]
                    slot = hd_ % 3
                    k.op("pe", lambda h, hd_=hd_, j=j, slot=slot, cu=cu: h.matmul(out=cu[:, slot, 0:129], lhsT=kwp[:, j, :], rhs=vt_[:, hd_, 0:129],
                                                                               start=True, stop=True),
                         reads=[B["kw"], bvt], writes=[bcu])
                for hd_ in range(NH):
                    r = hd_ % 2
                    cu, bcu = cu_banks[hd_ // 3]
                    slot = hd_ % 3
                    rs = slice(r * 64, (r + 1) * 64)
                    k.op("dve", lambda h, hd_=hd_, slot=slot, rs=rs, cu=cu: h.scalar_tensor_tensor(
                        out=Cst[rs, hd_, 0:129], in0=Cst[rs, hd_, 0:129], scalar=ebL[rs, hd_:hd_ + 1], in1=cu[rs, slot, 0:129],
                        op0=ALU.mult, op1=ALU.add),
                        reads=[B["Csth"][hd_], B["ebL"][hd_ // 4], bcu], writes=[B["Csth"][hd_]])
                k.op("act", lambda h: h.copy(out=Cbf[:], in_=Cst[:]), reads=B["Csth"], writes=[B["Cbf"]])
                k.step()
                hs = hd[:].rearrange("p h d -> p (h d)")
                if not late:
                    park, bpark = (HF, BHF) if dr == 0 else (HB, BHB)
                    k.dma("sp", lambda h: h.dma_start(out=park[csl, :], in_=hs), reads=[bhd], writes=[bpark])
                    return
                pending.append((dr, c, s2))

            def epilogue(dr, c, s2):
                B = Bd[dr]
                hd, bhd = hd_d[dr][s2], B["hd"][s2]
                hfl, ogl, xl = hfl_s, ogl_s, xl_s
                csl = slice(c * 128, (c + 1) * 128)
                hs = hd[:].rearrange("p h d -> p (h d)")
                sqh = xo[:]
                other, bother = (HB, BHB) if dr == 0 else (HF, BHF)
                k.dma("sp", lambda h: h.dma_start(out=hfl[:], in_=other[csl, :]), reads=[bother], writes=[B["hfl"]])
                k.dma("sp", lambda h: h.dma_start(out=ogl[:], in_=OGS[csl, :]), reads=[BOGS], writes=[B["ogl"]])
                k.dma("sp", lambda h: h.dma_start(out=xl[:], in_=XIN[csl, :]), reads=[Bxin], writes=[B["xl"]])
                k.step()
                k.op("dve", lambda h: h.tensor_tensor(out=hs, in0=hs, in1=hfl[:], op=ALU.add), reads=[bhd, B["hfl"]], writes=[bhd])
                k.op("act", lambda h: h.activation(out=sqh, in_=hs, func=AF.Square), reads=[bhd], writes=[B["xo"]])
                k.op("dve", lambda h: h.tensor_reduce(out=ssh[:], in_=xo[:].rearrange("p (h d) -> p h d", d=128), axis=AX.X, op=ALU.add),
                     reads=[B["xo"]], writes=[B["ssh"]])
                k.op("act", lambda h: h.activation(out=ssh[:], in_=ssh[:], func=AF.Sqrt, bias=eps_t[:], scale=1.0 / 128),
                     reads=[B["ssh"], Beps], writes=[B["ssh"]])
                k.op("dve", lambda h: h.reciprocal(out=ssh[:], in_=ssh[:]), reads=[B["ssh"]], writes=[B["ssh"]])
                k.step()
                k.op("dve", lambda h: h.tensor_tensor(out=hd[:], in0=hd[:], in1=ssh[:, :].unsqueeze(2).to_broadcast([128, NH, 128]), op=ALU.mult),
                     reads=[bhd, B["ssh"]], writes=[bhd])
                k.op("dve", lambda h: h.tensor_tensor(out=hs, in0=hs, in1=og_bc[:], op=ALU.mult), reads=[bhd, B["og_bc"]], writes=[bhd])
                k.op("dve", lambda h: h.tensor_tensor(out=yb[:], in0=hs, in1=ogl[:], op=ALU.mult), reads=[bhd, B["ogl"]], writes=[B["yb"]])
                k.step()
                for kc in range(KC):
                    k.op("pe", lambda h, kc=kc: h.transpose(out=pA[:, kc, :], in_=yb[:, kc * 128:(kc + 1) * 128], identity=identb[:]),
                         reads=[B["yb"], Bidb], writes=[B["pA"]])
                k.op("act", lambda h: h.copy(out=ybT[:], in_=pA[:]), reads=[B["pA"]], writes=[B["ybT"]])
                for half in range(2):
                    k.step()
                    for kc in range(KC):
                        k.op("pe", lambda h, kc=kc, half=half: h.matmul(out=pY, lhsT=ybT[:, kc, :], rhs=w_out[:, kc, half * 512:(half + 1) * 512],
                                                                        start=(kc == 0), stop=(kc == KC - 1)),
                             reads=[B["ybT"], B["w_out"]], writes=[B["pY"]])
                    k.op("dve", lambda h, half=half: h.tensor_tensor(out=xo[:, half * 512:(half + 1) * 512], in0=pY,
                                                                     in1=xl[:, half * 512:(half + 1) * 512], op=ALU.add),
                         reads=[B["pY"], B["xl"]], writes=[B["xo"]])
                k.step()
                k.dma("sp", lambda h: h.dma_start(out=XOUT[csl, :], in_=xo[:]), reads=[B["xo"]], writes=[Bxout])

            pending = []

            def merge3(a, b, e):
                lists = [l for l in (a, b, e) if l]
                pos = [0] * len(lists)
                while any(p < len(l) for p, l in zip(pos, lists)):
                    best = min((i for i in range(len(lists)) if pos[i] < len(lists[i])),
                               key=lambda i: (pos[i] / len(lists[i]), i))
                    k.replay(lists[best][pos[best]])
                    pos[best] += 1

            for step in range(NT + 1):
                todo, pending = pending, []
                sa = sb_ = []
                if step < NT:
                    k.begin_record()
                    chunk(0, step, step)
                    sa = k.end_record()
                    k.begin_record()
                    chunk(1, NT - 1 - step, step)
                    sb_ = k.end_record()
                k.begin_record()
                for (dr_, c_, s2_) in todo:
                    epilogue(dr_, c_, s2_)
                    k.step()
                se = k.end_record()
                merge3(sa, sb_, se)
        k.barrier()


def _t5_bucket(rel):
    nb = 16
    ret = (rel > 0).astype(np.int32) * nb
    n = np.abs(rel)
    max_exact = nb // 2
    nf = np.maximum(n, 1).astype(np.float32)
    large = max_exact + (np.log(nf / max_exact) / math.log(128 / max_exact) * (nb - max_exact)).astype(np.int32)
    large = np.minimum(large, nb - 1)
    return ret + np.where(n < max_exact, n, large)


def _attn_tables(rel_bias):
    p = np.arange(128)[:, None, None]
    j = np.arange(3)[None, :, None]
    q = np.arange(128)[None, None, :]
    rel = j * 128 + p - 128 - q
    bucket = _t5_bucket(rel)
    bias = rel_bias[bucket]
    bias = np.ascontiguousarray(bias.transpose(0, 1, 3, 2)).astype(np.float32)
    mask = np.where(np.abs(rel) <= 128, 0.0, NEG).astype(np.float32)
    mask = np.ascontiguousarray(np.broadcast_to(mask[:, :, None, :], (128, 3, 16, 128)))
    return bias, mask


N_CORES = 4
N_STAGES = 4
DEBUG = False
ML_SKIP = set()
ML_STOP = None
CHAIN = None
LAST = {}


def build_program(n_stages=None):
    n_stages = N_STAGES if n_stages is None else n_stages
    nc = bass.Bass("TRN2", target_bir_lowering=False)
    P = {}

    def inp(name, shape, dt=F32):
        P[name] = nc.dram_tensor(name, list(shape), dt, kind="ExternalInput").ap()

    inp("x", [S, D])
    inp("attn_norm_g", [1, D])
    inp("attn_w_in", [D, 1536])
    inp("attn_q_norm_g", [1, 64])
    inp("attn_k_norm_g", [1, 64])
    inp("attn_sink", [1, 16])
    inp("attn_w_out", [D, D])
    inp("attn_bias", [128, 3, 16, 128])
    inp("attn_mask", [128, 3, 16, 128])
    inp("ident", [128, 128])
    inp("ustrict", [128, 128])
    inp("iota512", [128, CAP])
    inp("rh_const", [128, NE * NT * 4])
    inp("mlstm_norm_g", [1, D])
    inp("mlstm_w_in", [D, MIN])
    inp("mlstm_gate_bias", [1, 32])
    inp("mlstm_out_norm_g", [1, D])
    inp("mlstm_w_out", [D, D])
    inp("tri_f", [128, 128])
    inp("tri_b", [128, 128])
    inp("negm_f", [128, 128])
    inp("negm_b", [128, 128])
    inp("ffn_norm_g", [2, D])
    inp("router_w", [2, D, NE])
    inp("expert_w1", [2, NE, D, 2 * D])
    inp("expert_w3", [2, NE, D, 2 * D])
    inp("expert_w2", [2, NE, 2 * D, D])
    out = nc.dram_tensor("out", [S, D], F32, kind="ExternalOutput").ap()
    if DEBUG is True:
        for n, shp in [("dbg_aff", [128, 512]), ("dbg_sel", [128, 512]), ("dbg_pos", [128, 512]), ("dbg_lo", [128, 16]),
                       ("dbg_hi", [128, 16]), ("dbg_idx", [128, 64]), ("dbg_pis", [128, 256])]:
            P[n] = nc.dram_tensor(n, shp, F32, kind="ExternalOutput").ap()
    P["_dbg_bufs"] = []
    if DEBUG == "ml":
        for n in ["dbg_hf", "dbg_hs"]:
            P[n] = nc.dram_tensor(n, [S, D], F32, kind="ExternalOutput").ap()
    scr = {}
    for n in ["XA", "XB"]:
        scr[n] = nc.dram_tensor(n, [S, D], F32, kind="Internal").ap()
    scr["XC"] = scr["XA"]
    H = nc.dram_tensor("Hs", [S, D], BF16, kind="Internal").ap()
    P["KS"] = nc.dram_tensor("KS", [S, 512], BF16, kind="Internal").ap()
    P["VS"] = nc.dram_tensor("VS", [S, NH, VW], BF16, kind="Internal").ap()
    P["OGS"] = nc.dram_tensor("OGS", [S, D], BF16, kind="Internal").ap()
    with ExitStack() as st:
        cx = Ctx(nc, st)
        k = cx.k
        Bx = Buf("x")
        chain = [("attn", P["x"], scr["XA"]), ("moe0", scr["XA"], scr["XB"]), ("mlstm", scr["XB"], scr["XC"]),
                 ("moe1", scr["XC"], out)][:n_stages]
        if CHAIN is not None:
            chain = [(CHAIN[0], P["x"], out)]
        chain[-1] = (chain[-1][0], chain[-1][1], out)
        bin_ = Bx
        BH = Buf("H")
        for (name, src, dst) in chain:
            bout = Buf(name + "_out")
            if name == "attn":
                stage_attn(cx, src, dst, bin_, bout, P)
                k.barrier()
            elif name == "moe0":
                stage_moe(cx, src, dst, H, bin_, bout, BH, P, 0)
            elif name == "moe1":
                stage_moe(cx, src, dst, H, bin_, bout, BH, P, 1)
            elif name == "mlstm":
                P["HB"] = out if dst is not out else scr["XB"]
                stage_mlstm(cx, src, dst, bin_, bout, P)
            bin_ = bout
        k.wait_bufs("sp", [bin_] + P["_dbg_bufs"])
        k.emit()
    print("program: insts=%d waits=%d per-engine=%s" % (k.n_inst, k.n_wait, {e: len(k.prog[e]) for e in ENGS}))
    return nc


def _consts():
    ident = np.eye(128, dtype=np.float32)
    ustrict = np.triu(np.ones((128, 128), dtype=np.float32), 1)
    iota512 = np.ascontiguousarray(np.broadcast_to(np.arange(CAP, dtype=np.float32), (128, CAP)))
    rh = np.zeros((128, NE, NT, 4), dtype=np.float32)
    rh[:, :, :, 0] = np.arange(128, dtype=np.float32)[:, None, None]
    rh[:, :, :, 1] = np.arange(NT, dtype=np.float32)[None, None, :]
    tri_f = np.triu(np.ones((128, 128), dtype=np.float32), 0)
    tri_b = np.tril(np.ones((128, 128), dtype=np.float32), 0)
    negm_f = np.where(np.arange(128)[:, None] > np.arange(128)[None, :], NEG, 0.0).astype(np.float32)
    negm_b = np.where(np.arange(128)[:, None] < np.arange(128)[None, :], NEG, 0.0).astype(np.float32)
    return {"ident": ident, "ustrict": ustrict, "iota512": iota512, "rh_const": rh.reshape(128, -1),
            "tri_f": tri_f, "tri_b": tri_b, "negm_f": negm_f, "negm_b": negm_b}


def kernel(**inputs):
    inputs = {k_: np.asarray(v) for k_, v in inputs.items()}
    x = inputs["x"].astype(np.float32, copy=False)
    bias, mask = _attn_tables(inputs["rel_bias"].astype(np.float32))
    consts = _consts()
    bi, bf = inputs["mlstm_b_i"][0].astype(np.float32), inputs["mlstm_b_f"][0].astype(np.float32)
    gate_bias = np.ascontiguousarray(np.concatenate([bi[0], bf[0], bi[1], bf[1]])[None, :])
    nc = build_program()
    in_maps = []
    for c in range(N_CORES):
        m = {
            "x": np.ascontiguousarray(x[c]),
            "attn_norm_g": np.ascontiguousarray(inputs["attn_norm_g"][0:1]),
            "attn_w_in": np.ascontiguousarray(inputs["attn_w_in"][0]),
            "attn_q_norm_g": np.ascontiguousarray(inputs["attn_q_norm_g"][0:1]),
            "attn_k_norm_g": np.ascontiguousarray(inputs["attn_k_norm_g"][0:1]),
            "attn_sink": np.ascontiguousarray(inputs["attn_sink"][0:1]),
            "attn_w_out": np.ascontiguousarray(inputs["attn_w_out"][0]),
            "attn_bias": bias, "attn_mask": mask,
            "mlstm_norm_g": np.ascontiguousarray(inputs["mlstm_norm_g"][0:1]),
            "mlstm_w_in": np.ascontiguousarray(inputs["mlstm_w_in"][0]),
            "mlstm_gate_bias": gate_bias,
            "mlstm_out_norm_g": np.ascontiguousarray(inputs["mlstm_out_norm_g"][0:1]),
            "mlstm_w_out": np.ascontiguousarray(inputs["mlstm_w_out"][0]),
            "ffn_norm_g": inputs["ffn_norm_g"], "router_w": inputs["router_w"],
            "expert_w1": inputs["expert_w1"], "expert_w3": inputs["expert_w3"], "expert_w2": inputs["expert_w2"],
        }
        m.update(consts)
        in_maps.append(m)
    res = run_bass_kernel_spmd(nc, in_maps, core_ids=list(range(N_CORES)))
    LAST["res"] = res.results
    return np.stack([r["out"] for r in res.results], axis=0)
```

```python
import math
from contextlib import ExitStack

import numpy as np
import concourse.bass as bass
import concourse.mybir as mybir
from concourse.bass_utils import run_bass_kernel_spmd

F32 = mybir.dt.float32
BF16 = mybir.dt.bfloat16
I32 = mybir.dt.int32
ALU = mybir.AluOpType
AF = mybir.ActivationFunctionType
AX = mybir.AxisListType

ENGS = ["pe", "dve", "act", "pool", "sp"]

S = 4096
D = 1024
NT = S // 128
KC = D // 128
NEG = -30000.0
EPS = 1e-6


class Buf:
    __slots__ = ("name", "w", "r")

    def __init__(self, name=""):
        self.name = name
        self.w = None
        self.r = []


class KB:
    N_DMA_SEMS = 56
    N_HW_SEMS = 40

    def __init__(self, nc, stack):
        self.nc = nc
        self.stack = stack
        self.handles = {"pe": nc.tensor, "dve": nc.vector, "act": nc.scalar,
                        "pool": nc.gpsimd, "sp": nc.sync}
        self.prog = {e: [] for e in ENGS}
        self.epoch = 0
        self.sems = {}
        self.count = {}
        self._new_epoch_sems()
        self.dma_sems = [stack.enter_context(nc.semaphore("d%d" % i)) for i in range(self.N_DMA_SEMS)]
        self.dma_val = [0] * self.N_DMA_SEMS
        self.dma_next = 0
        self.dma_next_sw = 0
        self.seen = {e: {} for e in ENGS}
        self.n_inst = 0
        self.n_wait = 0
        self._rec = None

    def _new_epoch_sems(self):
        for e in ["pe", "dve", "act", "pool"]:
            self.sems[(e, self.epoch)] = self.stack.enter_context(self.nc.semaphore("s_%s_%d" % (e, self.epoch)))
            self.count[e] = 0

    def _sem_of(self, key):
        return self.sems[key] if isinstance(key, tuple) else self.dma_sems[key]

    def _wait(self, eng, toks):
        need = {}
        for t in toks:
            if t is None:
                continue
            k, v = t
            if isinstance(k, tuple):
                if k[1] < self.epoch:
                    continue
                if k[0] == "pe" and eng == "pe":
                    continue
            if need.get(k, 0) < v:
                need[k] = v
        for k, v in need.items():
            if self.seen[eng].get(k, 0) >= v:
                continue
            self.seen[eng][k] = v
            self.prog[eng].append(("wait", self._sem_of(k), v))
            self.n_wait += 1

    def _deps(self, eng, reads, writes):
        toks = []
        for b in reads:
            toks.append(b.w)
        for b in writes:
            toks.append(b.w)
            for t in b.r:
                if isinstance(t[0], tuple) and t[0][0] == eng:
                    continue
                toks.append(t)
        return toks

    def _commit(self, tok, reads, writes):
        for b in reads:
            b.r.append(tok)
            if len(b.r) > 64:
                best = {}
                for k, v in b.r:
                    if best.get(k, 0) < v:
                        best[k] = v
                b.r = list(best.items())
        for b in writes:
            b.w = tok
            b.r = []
        self.n_inst += 1

    def begin_record(self):
        self._rec = [[]]

    def step(self):
        if self._rec is not None and self._rec[-1]:
            self._rec.append([])

    def end_record(self):
        r, self._rec = self._rec, None
        return [st_ for st_ in r if st_]

    def replay(self, step_):
        for (kind, eng, fn, reads, writes) in step_:
            (self.op if kind == "op" else self.dma)(eng, fn, reads, writes)

    def replay_merged(self, a, b):
        ia = ib = 0
        while ia < len(a) or ib < len(b):
            if ib >= len(b) or (ia < len(a) and ia * len(b) <= ib * len(a)):
                self.replay(a[ia]); ia += 1
            else:
                self.replay(b[ib]); ib += 1

    def op(self, eng, fn, reads=(), writes=()):
        if self._rec is not None:
            self._rec[-1].append(("op", eng, fn, list(reads), list(writes)))
            return None
        self._wait(eng, self._deps(eng, reads, writes))
        self.count[eng] += 1
        key = (eng, self.epoch)
        tok = (key, self.count[eng])
        self.prog[eng].append(("op", fn, self.sems[key], 1))
        self._commit(tok, reads, writes)
        return tok

    def dma(self, eng, fn, reads=(), writes=()):
        if self._rec is not None:
            self._rec[-1].append(("dma", eng, fn, list(reads), list(writes)))
            return None
        if eng == "pool":
            j = self.N_HW_SEMS + self.dma_next_sw
            self.dma_next_sw = (self.dma_next_sw + 1) % (self.N_DMA_SEMS - self.N_HW_SEMS)
        else:
            j = self.dma_next
            self.dma_next = (j + 1) % self.N_HW_SEMS
        toks = self._deps("dma", reads, writes)
        if self.dma_val[j] > 0:
            toks.append((j, self.dma_val[j]))
        self._wait(eng, toks)
        self.dma_val[j] += 16
        tok = (j, self.dma_val[j])
        self.prog[eng].append(("op", fn, self.dma_sems[j], 16))
        self._commit(tok, reads, writes)
        return tok

    def wait_bufs(self, eng, bufs):
        toks = []
        for b in bufs:
            toks.append(b.w)
            toks.extend(b.r)
        self._wait(eng, toks)

    def barrier(self):
        toks = [((e, self.epoch), self.count[e]) for e in ["pe", "dve", "act", "pool"] if self.count[e] > 0]
        toks += [(j, v) for j, v in enumerate(self.dma_val) if v > 0]
        for e in ENGS:
            self._wait(e, [t for t in toks if not (isinstance(t[0], tuple) and t[0][0] == e)])
        self.epoch += 1
        self._new_epoch_sems()

    def emit(self):
        nc = self.nc
        prog = self.prog

        def run(e):
            def body(h):
                for item in prog[e]:
                    if item[0] == "wait":
                        h.wait_ge(item[1], item[2])
                    else:
                        item[1](h).then_inc(item[2], item[3])
            return body

        with nc.Block() as block:
            block.tensor(run("pe"))
            block.vector(run("dve"))
            block.scalar(run("act"))
            block.gpsimd(run("pool"))
            block.sync(run("sp"))


class Ctx:
    def __init__(self, nc, stack):
        self.nc = nc
        self.k = KB(nc, stack)
        self.uid = 0

    def sb(self, st, shape, dt, name=None):
        self.uid += 1
        t = st.enter_context(self.nc.sbuf_tensor("%s_%d" % (name or "t", self.uid), list(shape), dt))
        return t

    def ps(self, st, shape, dt, name=None):
        self.uid += 1
        t = st.enter_context(self.nc.psum_tensor("%s_%d" % (name or "p", self.uid), list(shape), dt))
        return t


def stage_attn(cx, XIN, XOUT, Bxin, Bxout, P):
    nc, k = cx.nc, cx.k
    with ExitStack() as st:
        sb = lambda shape, dt, name=None: cx.sb(st, shape, dt, name)
        ps = lambda shape, dt, name=None: cx.ps(st, shape, dt, name)
        w_in = sb([128, KC, 1536], BF16, "w_in")
        w_out = sb([128, KC, 1024], BF16, "w_out")
        g_bc = sb([128, D], F32, "g_bc")
        ident = sb([128, 128], BF16, "ident")
        BM = sb([128, 3, 16, 128], F32, "BM")
        gqk = sb([128, 20, 64], F32, "gqk")
        esink = sb([128, 16], F32, "esink")
        eps_t = sb([128, 1], F32, "eps")
        V_all = sb([128, NT, 4, 65], BF16, "V_all")
        kT_all = sb([64, 4, S], BF16, "kT_all")
        xt = [sb([128, D], F32, "xt%d" % i) for i in range(3)]
        sq = sb([128, 1280], F32, "sq")
        junk = sb([128, D], BF16, "junk")
        xn = sb([128, D], BF16, "xn")
        xnT = sb([128, KC, 128], BF16, "xnT")
        tmpq = sb([128, 20, 64], F32, "tmpq")
        qkn = sb([128, 20, 64], BF16, "qkn")
        qT = [sb([64, 16, 128], BF16, "qT%d" % i) for i in range(3)]
        tS = [sb([128, 512], F32, "tS%d" % i) for i in range(2)]
        PT = [sb([128, 512], BF16, "PT%d" % i) for i in range(2)]
        ot = sb([128, 16, 64], BF16, "ot")
        otT = sb([128, KC, 128], BF16, "otT")
        x1t = [sb([128, D], F32, "x1t%d" % i) for i in range(2)]
        ss = sb([128, 1], F32, "ss")
        rstd = sb([128, 1], F32, "rstd")
        ssq = sb([128, 20], F32, "ssq")
        rqk = sb([128, 20], F32, "rqk")
        den = sb([128, 4], F32, "den")
        rden = sb([128, 4], F32, "rden")
        pA = ps([128, KC, 128], BF16, "pA")
        pB = ps([128, 512], F32, "pB")
        pC = ps([128, 512], F32, "pC")
        pD = ps([128, 512], F32, "pD")
        pE = ps([64, 8, 128], BF16, "pE")
        pGs = [ps([128, 512], F32, "pG%d" % i) for i in range(2)]
        pH = ps([128, 4, 65], F32, "pH")

        B = {}
        for n in ["w_in", "w_out", "g_bc", "ident", "BM", "gqk", "esink", "eps", "V1", "sq", "junk",
                  "xn", "xnT", "tmpq", "qkn", "ot", "otT", "ss", "rstd", "ssq", "rqk", "den", "rden",
                  "pA", "pB", "pC", "pD", "pE", "pH", "gq_s", "gk_s", "mask_s", "sink_s"]:
            B[n] = Buf(n)
        Bxt = [Buf("xt%d" % i) for i in range(3)]
        BqT = [Buf("qT%d" % i) for i in range(3)]
        BtS = [Buf() for _ in range(2)]
        BpG = [Buf() for _ in range(2)]
        BPT = [Buf() for _ in range(2)]
        Bx1 = [Buf() for _ in range(2)]
        BV = [Buf("V%d" % i) for i in range(NT)]
        BkT = [Buf("kT%d" % i) for i in range(NT)]

        k.dma("pool", lambda h: h.dma_start(out=w_in[:], in_=P["attn_w_in"].rearrange("(kc p) n -> p kc n", p=128)),
              writes=[B["w_in"]])
        k.dma("pool", lambda h: h.dma_start(out=w_out[:], in_=P["attn_w_out"].rearrange("(kc p) n -> p kc n", p=128)),
              writes=[B["w_out"]])
        k.dma("pool", lambda h: h.dma_start(out=ident[:], in_=P["ident"]), writes=[B["ident"]])
        k.dma("sp", lambda h: h.dma_start(out=g_bc[:], in_=P["attn_norm_g"].to_broadcast([128, D])), writes=[B["g_bc"]])
        k.dma("sp", lambda h: h.dma_start(out=BM[:], in_=P["attn_bias"]), writes=[B["BM"]])
        mstage = [xt[0], xt[1], x1t[0], x1t[1], xt[2], sq]
        for j in range(3):
            for hh in range(2):
                buf = mstage[j * 2 + hh]
                bb = Buf()
                k.dma("sp", lambda h, j=j, hh=hh, buf=buf: h.dma_start(
                    out=buf[:, 0:1024], in_=P["attn_mask"][:, j, hh * 8:(hh + 1) * 8, :].rearrange("p h q -> p (h q)")),
                    writes=[bb])
                k.op("dve", lambda h, j=j, hh=hh, buf=buf: h.tensor_tensor(
                    out=BM[:, j, hh * 8:(hh + 1) * 8, :], in0=BM[:, j, hh * 8:(hh + 1) * 8, :],
                    in1=buf[:, 0:1024].rearrange("p (h q) -> p h q", q=128), op=ALU.add),
                    reads=[bb], writes=[B["BM"]])
                for tb in (Bxt + Bx1 + [B["sq"]]):
                    pass
        for tb in Bxt + Bx1 + [B["sq"]]:
            tb.r.append(B["BM"].w)
        gq_s = sb([128, 64], F32, "gq_s")
        gk_s = sb([128, 64], F32, "gk_s")
        k.dma("sp", lambda h: h.dma_start(out=gq_s[:], in_=P["attn_q_norm_g"].to_broadcast([128, 64])), writes=[B["gq_s"]])
        k.dma("sp", lambda h: h.dma_start(out=gk_s[:], in_=P["attn_k_norm_g"].to_broadcast([128, 64])), writes=[B["gk_s"]])
        k.op("dve", lambda h: h.tensor_scalar(out=gqk[:, 0:16, :], in0=gq_s[:, :].unsqueeze(1).to_broadcast([128, 16, 64]),
                                              scalar1=0.125, scalar2=None, op0=ALU.mult),
             reads=[B["gq_s"]], writes=[B["gqk"]])
        k.op("dve", lambda h: h.tensor_copy(out=gqk[:, 16:20, :], in_=gk_s[:, :].unsqueeze(1).to_broadcast([128, 4, 64])),
             reads=[B["gk_s"]], writes=[B["gqk"]])
        gq_col = sb([64, 1], F32, "gq_col")
        gk_col = sb([64, 1], F32, "gk_col")
        B["gq_col"], B["gk_col"] = Buf(), Buf()
        k.dma("sp", lambda h: h.dma_start(out=gq_col[:], in_=P["attn_q_norm_g"].rearrange("o d -> d o")), writes=[B["gq_col"]])
        k.dma("sp", lambda h: h.dma_start(out=gk_col[:], in_=P["attn_k_norm_g"].rearrange("o d -> d o")), writes=[B["gk_col"]])
        k.op("dve", lambda h: h.tensor_scalar(out=gq_col[:], in0=gq_col[:], scalar1=0.125, scalar2=None, op0=ALU.mult),
             reads=[B["gq_col"]], writes=[B["gq_col"]])
        k.dma("sp", lambda h: h.dma_start(out=esink[:], in_=P["attn_sink"].to_broadcast([128, 16])), writes=[B["esink"]])
        k.op("act", lambda h: h.activation(out=esink[:], in_=esink[:], func=AF.Exp), reads=[B["esink"]], writes=[B["esink"]])
        k.op("dve", lambda h: h.memset(eps_t[:], EPS), writes=[B["eps"]])
        k.op("pool", lambda h: h.memset(V_all[:, :, :, 64:65], 1.0), writes=[B["V1"]])

        def phase1(i):
            s3 = i % 3
            x_t, bx = xt[s3], Bxt[s3]
            k.dma("sp", lambda h: h.dma_start(out=x_t[:], in_=XIN[i * 128:(i + 1) * 128, :]), reads=[Bxin], writes=[bx])
            k.op("act", lambda h: h.activation(out=junk[:], in_=x_t[:], func=AF.Square, accum_out=ss[:]),
                 reads=[bx], writes=[B["junk"], B["ss"]])
            k.op("act", lambda h: h.activation(out=rstd[:], in_=ss[:], func=AF.Ln, bias=eps_t[:], scale=1.0 / D),
                 reads=[B["ss"], B["eps"]], writes=[B["rstd"]])
            k.op("act", lambda h: h.activation(out=rstd[:], in_=rstd[:], func=AF.Exp, scale=-0.5), reads=[B["rstd"]], writes=[B["rstd"]])
            k.op("dve", lambda h: h.scalar_tensor_tensor(out=xn[:], in0=x_t[:], scalar=rstd[:, 0:1], in1=g_bc[:],
                                                         op0=ALU.mult, op1=ALU.mult),
                 reads=[bx, B["rstd"], B["g_bc"]], writes=[B["xn"]])
            k.step()
            for kc in range(KC):
                k.op("pe", lambda h, kc=kc: h.transpose(out=pA[:, kc, :], in_=xn[:, kc * 128:(kc + 1) * 128], identity=ident[:]),
                     reads=[B["xn"], B["ident"]], writes=[B["pA"]])
            k.op("act", lambda h: h.copy(out=xnT[:], in_=pA[:]), reads=[B["pA"]], writes=[B["xnT"]])
            k.step()
            for c, (pp, bn) in enumerate([(pB, "pB"), (pC, "pC"), (pD, "pD")]):
                for kc in range(KC):
                    k.op("pe", lambda h, kc=kc, c=c, pp=pp: h.matmul(
                        out=pp[:], lhsT=xnT[:, kc, :], rhs=w_in[:, kc, c * 512:(c + 1) * 512],
                        start=(kc == 0), stop=(kc == KC - 1)),
                        reads=[B["xnT"], B["w_in"]], writes=[B[bn]])
            k.op("act", lambda h: h.activation(out=sq[:, 0:512], in_=pB[:], func=AF.Square), reads=[B["pB"]], writes=[B["sq"]])
            k.op("act", lambda h: h.activation(out=sq[:, 512:1024], in_=pC[:], func=AF.Square), reads=[B["pC"]], writes=[B["sq"]])
            k.op("act", lambda h: h.activation(out=sq[:, 1024:1280], in_=pD[:, 0:256], func=AF.Square), reads=[B["pD"]], writes=[B["sq"]])
            k.op("dve", lambda h: h.tensor_reduce(out=ssq[:], in_=sq[:].rearrange("p (h d) -> p h d", d=64), axis=AX.X, op=ALU.add),
                 reads=[B["sq"]], writes=[B["ssq"]])
            k.op("act", lambda h: h.activation(out=rqk[:], in_=ssq[:], func=AF.Ln, bias=eps_t[:], scale=1.0 / 64),
                 reads=[B["ssq"], B["eps"]], writes=[B["rqk"]])
            k.op("act", lambda h: h.activation(out=rqk[:], in_=rqk[:], func=AF.Exp, scale=-0.5), reads=[B["rqk"]], writes=[B["rqk"]])
            for (pp, bn, h0, nh) in [(pB, "pB", 0, 8), (pC, "pC", 8, 8), (pD, "pD", 16, 4)]:
                k.op("dve", lambda h, pp=pp, h0=h0, nh=nh: h.tensor_tensor(
                    out=qkn[:, h0:h0 + nh, :], in0=pp[:, 0:nh * 64].rearrange("p (h d) -> p h d", d=64),
                    in1=rqk[:, h0:h0 + nh].unsqueeze(2).to_broadcast([128, nh, 64]), op=ALU.mult),
                    reads=[B[bn], B["rqk"]], writes=[B["qkn"]])
            k.op("act", lambda h: h.copy(out=V_all[:, i, :, 0:64], in_=pD[:, 256:512].rearrange("p (g d) -> p g d", d=64)),
                 reads=[B["pD"]], writes=[BV[i]])

            q_t, bq = qT[i % 3], BqT[i % 3]
            for half in range(2):
                k.step()
                for hh in range(8):
                    k.op("pe", lambda h, half=half, hh=hh: h.transpose(out=pE[:, hh, :], in_=qkn[:, half * 8 + hh, :], identity=ident[:]),
                         reads=[B["qkn"], B["ident"]], writes=[B["pE"]])
                k.op("act", lambda h, half=half: h.activation(out=q_t[:, half * 8:(half + 1) * 8, :], in_=pE[:], func=AF.Copy,
                                                               scale=gq_col[:, 0:1]),
                     reads=[B["pE"], B["gq_col"]], writes=[bq])
            k.step()
            for g in range(4):
                k.op("pe", lambda h, g=g: h.transpose(out=pE[:, g, :], in_=qkn[:, 16 + g, :], identity=ident[:]),
                     reads=[B["qkn"], B["ident"]], writes=[B["pE"]])
            k.op("dve", lambda h: h.tensor_scalar(out=kT_all[:, :, i * 128:(i + 1) * 128], in0=pE[:, 0:4, :],
                                                  scalar1=gk_col[:, 0:1], scalar2=None, op0=ALU.mult),
                 reads=[B["pE"], B["gk_col"]], writes=[BkT[i]])

        cnt = [0]

        def phase2(i):
            q_t, bq = qT[i % 3], BqT[i % 3]
            blocks = [j for j in (i - 1, i, i + 1) if 0 <= j < NT]
            items = [(g, bi, j) for g in range(4) for bi, j in enumerate(blocks)]
            slots = []

            def emit_S(n):
                g, bi, j = items[n]
                c2 = cnt[0] % 2
                cnt[0] += 1
                slots.append(c2)
                k.op("pe", lambda h: h.matmul(
                    out=pGs[c2][:], lhsT=kT_all[:, g, j * 128:(j + 1) * 128],
                    rhs=q_t[:, 4 * g:4 * g + 4, :].rearrange("p h q -> p (h q)"), start=True, stop=True),
                    reads=[BkT[j], bq], writes=[BpG[c2]])
                k.op("dve", lambda h: h.tensor_tensor(
                    out=tS[c2][:], in0=pGs[c2][:], in1=BM[:, j - i + 1, 4 * g:4 * g + 4, :].rearrange("p h q -> p (h q)"), op=ALU.add),
                    reads=[BpG[c2], B["BM"]], writes=[BtS[c2]])
                k.op("act", lambda h: h.activation(out=PT[c2][:], in_=tS[c2][:], func=AF.Exp),
                     reads=[BtS[c2]], writes=[BPT[c2]])

            def emit_O(n):
                g, bi, j = items[n]
                c2 = slots[n]
                for hh in range(4):
                    k.op("pe", lambda h, hh=hh: h.matmul(
                        out=pH[:, hh, :], lhsT=PT[c2][:, hh * 128:(hh + 1) * 128], rhs=V_all[:, j, g, :],
                        start=(bi == 0 and hh == 0), stop=(bi == len(blocks) - 1 and hh == 3)),
                        reads=[BPT[c2], BV[j], B["V1"]], writes=[B["pH"]])
                if bi == len(blocks) - 1:
                    k.op("dve", lambda h: h.tensor_tensor(out=den[:], in0=pH[:, :, 64], in1=esink[:, 4 * g:4 * g + 4], op=ALU.add),
                         reads=[B["pH"], B["esink"]], writes=[B["den"]])
                    k.op("dve", lambda h: h.reciprocal(out=rden[:], in_=den[:]), reads=[B["den"]], writes=[B["rden"]])
                    k.op("dve", lambda h: h.tensor_tensor(
                        out=ot[:, 4 * g:4 * g + 4, :], in0=pH[:, :, 0:64],
                        in1=rden[:, :].unsqueeze(2).to_broadcast([128, 4, 64]), op=ALU.mult),
                        reads=[B["pH"], B["rden"]], writes=[B["ot"]])

            emit_S(0)
            for n in range(len(items)):
                k.step()
                if n + 1 < len(items):
                    emit_S(n + 1)
                emit_O(n)
            otf = ot[:].rearrange("p h d -> p (h d)")
            k.step()
            for kc in range(KC):
                k.op("pe", lambda h, kc=kc: h.transpose(out=pA[:, kc, :], in_=otf[:, kc * 128:(kc + 1) * 128], identity=ident[:]),
                     reads=[B["ot"], B["ident"]], writes=[B["pA"]])
            k.op("act", lambda h: h.copy(out=otT[:], in_=pA[:]), reads=[B["pA"]], writes=[B["otT"]])
            x_t, bx = xt[i % 3], Bxt[i % 3]
            xo, bxo = x1t[i % 2], Bx1[i % 2]
            for c, (pp, bn) in enumerate([(pB, "pB"), (pC, "pC")]):
                k.step()
                for kc in range(KC):
                    k.op("pe", lambda h, kc=kc, c=c, pp=pp: h.matmul(
                        out=pp[:], lhsT=otT[:, kc, :], rhs=w_out[:, kc, c * 512:(c + 1) * 512],
                        start=(kc == 0), stop=(kc == KC - 1)),
                        reads=[B["otT"], B["w_out"]], writes=[B[bn]])
                k.op("dve", lambda h, c=c, pp=pp: h.tensor_tensor(out=xo[:, c * 512:(c + 1) * 512], in0=pp[:],
                                                                  in1=x_t[:, c * 512:(c + 1) * 512], op=ALU.add),
                     reads=[B[bn], bx], writes=[bxo])
            k.dma("sp", lambda h: h.dma_start(out=XOUT[i * 128:(i + 1) * 128, :], in_=xo[:]), reads=[bxo], writes=[Bxout])

        for i in range(NT + 2):
            sa, sb_ = [], []
            if i < NT:
                k.begin_record()
                phase1(i)
                sa = k.end_record()
            if i >= 2:
                k.begin_record()
                phase2(i - 2)
                sb_ = k.end_record()
            k.replay_merged(sa, sb_)
        allb = []
        return allb


NE = 16
CAP = 512
N_BISECT = 32


def stage_moe(cx, XIN, XOUT, H, Bxin, Bxout, BH, P, layer):
    nc, k = cx.nc, cx.k
    W1, W3, W2 = P["expert_w1"], P["expert_w3"], P["expert_w2"]
    with ExitStack() as st0:
        sb0 = lambda shape, dt, name=None: cx.sb(st0, shape, dt, name)
        AFF = sb0([128, NE, NT], F32, "AFF")
        selm = sb0([128, NE, NT], F32, "selm")
        pos = sb0([128, NE, NT], F32, "pos")
        RH = sb0([128, NE, NT, 4], BF16, "RH")
        iota = sb0([128, CAP], F32, "iota")
        identb = sb0([128, 128], BF16, "identb")
        BA, Bsel, Bpos, BRH, Biota, Bidb = Buf("AFF"), Buf("selm"), Buf("pos"), Buf("RH"), Buf("iota"), Buf("identb")
        k.dma("sp", lambda h: h.dma_start(out=iota[:], in_=P["iota512"]), writes=[Biota])
        k.dma("pool", lambda h: h.dma_start(out=identb[:], in_=P["ident"]), writes=[Bidb])
        k.dma("pool", lambda h: h.dma_start(out=RH[:].rearrange("p e i c -> p (e i c)"), in_=P["rh_const"]), writes=[BRH])

        with ExitStack() as st:
            sb = lambda shape, dt, name=None: cx.sb(st, shape, dt, name)
            ps = lambda shape, dt, name=None: cx.ps(st, shape, dt, name)
            g_bc = sb([128, D], F32, "g_bc")
            wr = sb([128, KC, NE], F32, "wr")
            identf = sb([128, 128], F32, "identf")
            onesf = sb([128, 128], F32, "onesf")
            ustr = sb([128, 128], F32, "ustr")
            eps_t = sb([128, 1], F32, "eps")
            ones32 = sb([128, NT], F32, "ones32")
            xt = [sb([128, D], F32, "xt%d" % i) for i in range(2)]
            hn = [sb([128, D], F32, "hn%d" % i) for i in range(2)]
            hb = [sb([128, D], BF16, "hb%d" % i) for i in range(2)]
            hnT = sb([128, KC, 128], F32, "hnT")
            junk = sb([128, D], BF16, "junk")
            ss = sb([128, 1], F32, "ss")
            rstd = sb([128, 1], F32, "rstd")
            mx = sb([128, 1], F32, "mx")
            ex = sb([128, NE], F32, "ex")
            sm = sb([128, 1], F32, "sm")
            lo = sb([128, NE], F32, "lo")
            hi = sb([128, NE], F32, "hi")
            mid = sb([128, NE], F32, "mid")
            cmp_t = sb([128, NE, NT], F32, "cmp")
            cntp = sb([128, NE], F32, "cntp")
            mge = sb([128, NE], mybir.dt.uint32, "mge")
            mlt = sb([128, NE], mybir.dt.uint32, "mlt")
            incl = sb([128, NE, NT], F32, "incl")
            ahi = sb([128, NE, NT], BF16, "ahi")
            pR0 = ps([128, 4, 128], F32, "pR0")
            pR1 = ps([128, 4, 128], F32, "pR1")
            pL_full = ps([128, 512], F32, "pL")
            pCn_full = ps([128, 512], F32, "pCn")
            pL = pL_full[:, 0:NE]
            pCn = pCn_full[:, 0:NE]
            pP = ps([128, NE * NT], F32, "pP")
            B = {n: Buf(n) for n in ["g_bc", "wr", "identf", "onesf", "ustr", "eps", "ones32", "hnT", "junk", "ss", "rstd",
                                     "mx", "ex", "sm", "lo", "hi", "mid", "cmp", "cntp", "mge", "mlt", "incl", "ahi",
                                     "pR0", "pR1", "pL", "pCn", "pP"]}
            Bxt = [Buf() for _ in range(2)]
            Bhn = [Buf() for _ in range(2)]
            Bhb = [Buf() for _ in range(2)]
            k.dma("sp", lambda h: h.dma_start(out=g_bc[:], in_=P["ffn_norm_g"][layer:layer + 1, :].to_broadcast([128, D])), writes=[B["g_bc"]])
            k.dma("sp", lambda h: h.dma_start(out=wr[:], in_=P["router_w"][layer].rearrange("(kc p) e -> p kc e", p=128)), writes=[B["wr"]])
            k.dma("sp", lambda h: h.dma_start(out=identf[:], in_=P["ident"]), writes=[B["identf"]])
            k.dma("sp", lambda h: h.dma_start(out=ustr[:], in_=P["ustrict"]), writes=[B["ustr"]])
            k.op("dve", lambda h: h.memset(onesf[:], 1.0), writes=[B["onesf"]])
            k.op("dve", lambda h: h.memset(ones32[:], 1.0), writes=[B["ones32"]])
            k.op("dve", lambda h: h.memset(eps_t[:], EPS), writes=[B["eps"]])
            def pre_tile(i):
                s2 = i % 2
                x_t, bx = xt[s2], Bxt[s2]
                k.dma("sp", lambda h, x_t=x_t: h.dma_start(out=x_t[:], in_=XIN[i * 128:(i + 1) * 128, :]), reads=[Bxin], writes=[bx])
                k.dma("sp", lambda h, x_t=x_t: h.dma_start(out=XOUT[i * 128:(i + 1) * 128, :], in_=x_t[:]), reads=[bx], writes=[Bxout])
                k.op("act", lambda h, x_t=x_t: h.activation(out=junk[:], in_=x_t[:], func=AF.Square, accum_out=ss[:]),
                     reads=[bx], writes=[B["junk"], B["ss"]])
                k.op("act", lambda h: h.activation(out=rstd[:], in_=ss[:], func=AF.Ln, bias=eps_t[:], scale=1.0 / D),
                     reads=[B["ss"], B["eps"]], writes=[B["rstd"]])
                k.op("act", lambda h: h.activation(out=rstd[:], in_=rstd[:], func=AF.Exp, scale=-0.5), reads=[B["rstd"]], writes=[B["rstd"]])
                k.op("dve", lambda h, x_t=x_t, s2=s2: h.scalar_tensor_tensor(out=hn[s2][:], in0=x_t[:], scalar=rstd[:, 0:1], in1=g_bc[:],
                                                                         op0=ALU.mult, op1=ALU.mult),
                     reads=[bx, B["rstd"], B["g_bc"]], writes=[Bhn[s2]])
                k.op("pool", lambda h, s2=s2: h.tensor_copy(out=hb[s2][:], in_=hn[s2][:]), reads=[Bhn[s2]], writes=[Bhb[s2]])
                k.dma("sp", lambda h, s2=s2: h.dma_start(out=H[i * 128:(i + 1) * 128, :], in_=hb[s2][:]), reads=[Bhb[s2]], writes=[BH])

            def pre_tile_back(i):
                s2 = i % 2
                for kc in range(KC):
                    pp, bn = (pR0, "pR0") if kc < 4 else (pR1, "pR1")
                    k.op("pe", lambda h, kc=kc, pp=pp, s2=s2: h.transpose(out=pp[:, kc % 4, :], in_=hn[s2][:, kc * 128:(kc + 1) * 128], identity=identf[:]),
                         reads=[Bhn[s2], B["identf"]], writes=[B[bn]])
                k.op("act", lambda h: h.copy(out=hnT[:, 0:4, :], in_=pR0[:]), reads=[B["pR0"]], writes=[B["hnT"]])
                k.op("dve", lambda h: h.tensor_copy(out=hnT[:, 4:8, :], in_=pR1[:]), reads=[B["pR1"]], writes=[B["hnT"]])
                for kc in range(KC):
                    k.op("pe", lambda h, kc=kc: h.matmul(out=pL[:], lhsT=hnT[:, kc, :], rhs=wr[:, kc, :], start=(kc == 0), stop=(kc == KC - 1)),
                         reads=[B["hnT"], B["wr"]], writes=[B["pL"]])
                k.op("dve", lambda h: h.tensor_reduce(out=mx[:], in_=pL[:], axis=AX.X, op=ALU.max, negate=True),
                     reads=[B["pL"]], writes=[B["mx"]])
                k.op("act", lambda h: h.activation(out=ex[:], in_=pL[:], func=AF.Exp, bias=mx[:], scale=1.0, accum_out=sm[:]),
                     reads=[B["pL"], B["mx"]], writes=[B["ex"], B["sm"]])
                k.op("dve", lambda h: h.reciprocal(out=sm[:], in_=sm[:]), reads=[B["sm"]], writes=[B["sm"]])
                k.op("dve", lambda h, i=i: h.tensor_scalar(out=AFF[:, :, i], in0=ex[:], scalar1=sm[:, 0:1], scalar2=None, op0=ALU.mult),
                     reads=[B["ex"], B["sm"]], writes=[BA])
            pre_tile(0)
            for i in range(NT):
                if i + 1 < NT:
                    pre_tile(i + 1)
                pre_tile_back(i)
            k.op("dve", lambda h: h.memset(lo[:], 0.0), writes=[B["lo"]])
            k.op("dve", lambda h: h.memset(hi[:], 1.0), writes=[B["hi"]])
            for it in range(N_BISECT):
                wdt = 2.0 ** -(it + 1)
                k.op("dve", lambda h, wdt=wdt: h.tensor_scalar(out=mid[:], in0=lo[:], scalar1=wdt, scalar2=None, op0=ALU.add),
                     reads=[B["lo"]], writes=[B["mid"]])
                k.op("dve", lambda h: h.tensor_tensor(out=cmp_t[:], in0=AFF[:], in1=mid[:, :].unsqueeze(2).to_broadcast([128, NE, NT]), op=ALU.is_ge),
                     reads=[BA, B["mid"]], writes=[B["cmp"]])
                k.op("dve", lambda h: h.tensor_reduce(out=cntp[:], in_=cmp_t[:], axis=AX.X, op=ALU.add), reads=[B["cmp"]], writes=[B["cntp"]])
                k.op("pe", lambda h: h.matmul(out=pCn[:], lhsT=onesf[:], rhs=cntp[:], start=True, stop=True),
                     reads=[B["onesf"], B["cntp"]], writes=[B["pCn"]])
                k.op("dve", lambda h, wdt=wdt: h.tensor_scalar(out=hi[:], in0=pCn[:], scalar1=CAP - 0.5, scalar2=wdt, op0=ALU.is_ge, op1=ALU.mult),
                     reads=[B["pCn"]], writes=[B["hi"]])
                k.op("dve", lambda h: h.tensor_tensor(out=lo[:], in0=lo[:], in1=hi[:], op=ALU.add), reads=[B["lo"], B["hi"]], writes=[B["lo"]])
            k.op("dve", lambda h: h.tensor_tensor(out=selm[:], in0=AFF[:], in1=lo[:, :].unsqueeze(2).to_broadcast([128, NE, NT]), op=ALU.is_ge),
                 reads=[BA, B["lo"]], writes=[Bsel])
            for e in range(NE):
                k.op("dve", lambda h, e=e: h.tensor_tensor_scan(out=incl[:, e, :], data0=ones32[:], data1=selm[:, e, :], initial=0.0,
                                                              op0=ALU.mult, op1=ALU.add),
                     reads=[Bsel, B["ones32"]], writes=[B["incl"]])
            k.op("dve", lambda h: h.tensor_tensor(out=incl[:], in0=incl[:], in1=selm[:], op=ALU.subtract), reads=[B["incl"], Bsel], writes=[B["incl"]])
            k.op("pe", lambda h: h.matmul(out=pP[:], lhsT=ustr[:], rhs=selm[:].rearrange("p e i -> p (e i)"), start=True, stop=False),
                 reads=[B["ustr"], Bsel], writes=[B["pP"]])
            k.op("pe", lambda h: h.matmul(out=pP[:], lhsT=onesf[:], rhs=incl[:].rearrange("p e i -> p (e i)"), start=False, stop=True),
                 reads=[B["onesf"], B["incl"]], writes=[B["pP"]])
            k.op("act", lambda h: h.copy(out=pos[:].rearrange("p e i -> p (e i)"), in_=pP[:]), reads=[B["pP"]], writes=[Bpos])
            k.op("dve", lambda h: h.tensor_copy(out=ahi[:], in_=AFF[:]), reads=[BA], writes=[B["ahi"]])
            k.op("dve", lambda h: h.tensor_copy(out=RH[:, :, :, 2], in_=ahi[:]), reads=[B["ahi"]], writes=[BRH])
            k.op("dve", lambda h: h.tensor_tensor(out=RH[:, :, :, 3], in0=AFF[:], in1=ahi[:], op=ALU.subtract), reads=[BA, B["ahi"]], writes=[BRH])
            if "dbg_aff" in P and layer == 0:
                bd = Buf()
                k.dma("sp", lambda h: h.dma_start(out=P["dbg_aff"], in_=AFF[:].rearrange("p e i -> p (e i)")), reads=[BA], writes=[bd])
                k.dma("sp", lambda h: h.dma_start(out=P["dbg_sel"], in_=selm[:].rearrange("p e i -> p (e i)")), reads=[Bsel], writes=[bd])
                k.dma("sp", lambda h: h.dma_start(out=P["dbg_pos"], in_=pos[:].rearrange("p e i -> p (e i)")), reads=[Bpos], writes=[bd])
                k.dma("sp", lambda h: h.dma_start(out=P["dbg_lo"], in_=lo[:]), reads=[B["lo"]], writes=[bd])
                k.dma("sp", lambda h: h.dma_start(out=P["dbg_hi"], in_=hi[:]), reads=[B["hi"]], writes=[bd])
                P["_dbg_bufs"].append(bd)
        k.barrier()

        with ExitStack() as st:
            sb = lambda shape, dt, name=None: cx.sb(st, shape, dt, name)
            ps = lambda shape, dt, name=None: cx.ps(st, shape, dt, name)
            NCH = 4
            w1c = [sb([128, KC, 512], BF16, "w1c%d" % c) for c in range(NCH)]
            w3c = [sb([128, KC, 512], BF16, "w3c%d" % c) for c in range(NCH)]
            w2c = [sb([128, 4, D], BF16, "w2c%d" % c) for c in range(NCH)]
            Bw1 = [Buf() for _ in range(NCH)]
            Bw3 = [Buf() for _ in range(NCH)]
            Bw2 = [Buf() for _ in range(NCH)]
            Pm = [sb([128, CAP], BF16, "Pm%d" % c) for c in range(4)]
            BPm = [Buf() for _ in range(4)]
            idxf = sb([128, 4], F32, "idxf")
            pIs = sb([128, 4, 4], F32, "pIs")
            BpIs = Buf()
            idxi = [sb([128, 4], I32, "idxi%d" % c) for c in range(2)]
            gt = [sb([128, 4], F32, "gt%d" % c) for c in range(2)]
            Bidxf = Buf()
            Bidx = [Buf() for _ in range(2)]
            Bgt = [Buf() for _ in range(2)]
            xs = [sb([128, D], BF16, "xs%d" % c) for c in range(4)]
            Bxs = [Buf() for _ in range(4)]
            xsT = [sb([128, KC, CAP], BF16, "xsT%d" % c) for c in range(2)]
            BxsT = [Buf() for _ in range(2)]
            actT = sb([128, 16, CAP], BF16, "actT")
            BactT = [Buf() for _ in range(16)]
            sa = [sb([128, CAP], F32, "sa%d" % c) for c in range(2)]
            Bsa = [Buf() for _ in range(2)]
            yt = [sb([128, D], F32, "yt%d" % c) for c in range(2)]
            Byt = [Buf() for _ in range(2)]
            pT = ps([128, KC, 128], BF16, "pT")
            pI = ps([128, 4, 4], F32, "pI")
            pa = [ps([128, CAP], F32, "pa%d" % c) for c in range(2)]
            pu = [ps([128, CAP], F32, "pu%d" % c) for c in range(2)]
            py = [ps([128, 512], F32, "py%d" % c) for c in range(2)]
            BpT, BpI = Buf(), Buf()
            Bpa = [Buf() for _ in range(2)]
            Bpu = [Buf() for _ in range(2)]
            Bpy = [Buf() for _ in range(2)]
            cnt = {"stg": 0, "cast": 0, "y": 0}
            cast_engs = ["act", "dve", "act", "pool"]

            def load_chunk(src_ap, dst, bdst):
                k.dma("pool", lambda h: h.dma_start(out=dst[:], in_=src_ap), writes=[bdst])

            def load_w13(e, c):
                load_chunk(W1[layer, e].rearrange("(kc p) f -> p kc f", p=128)[:, :, c * 512:(c + 1) * 512], w1c[c], Bw1[c])
                load_chunk(W3[layer, e].rearrange("(kc p) f -> p kc f", p=128)[:, :, c * 512:(c + 1) * 512], w3c[c], Bw3[c])

            def load_w2(e, c):
                load_chunk(W2[layer, e].rearrange("(fc p) d -> p fc d", p=128)[:, c * 4:(c + 1) * 4, :], w2c[c], Bw2[c])

            def build_steps(e):
                par = e % 2
                stepsA, stepsB = [], []

                def emit_pm(i):
                    pm, bpm = Pm[i % 4], BPm[i % 4]
                    k.op("dve", lambda h: h.tensor_scalar(out=pm[:], in0=iota[:], scalar1=pos[:, e, i:i + 1], scalar2=selm[:, e, i:i + 1],
                                                          op0=ALU.is_equal, op1=ALU.mult),
                         reads=[Biota, Bpos, Bsel], writes=[bpm])

                def step_i(i):
                    def f():
                        if i == 0:
                            emit_pm(0)
                            emit_pm(1)
                        if i + 2 < NT:
                            emit_pm(i + 2)
                        pm, bpm = Pm[i % 4], BPm[i % 4]
                        for c in range(4):
                            k.op("pe", lambda h, c=c: h.matmul(out=pI[:, c, :], lhsT=pm[:, c * 128:(c + 1) * 128], rhs=RH[:, e, i, :],
                                                               start=(i == 0 and c == 0), stop=(i == NT - 1 and c == 3)),
                                 reads=[bpm, BRH], writes=[BpI])
                    return f

                for i in range(NT):
                    stepsA.append(step_i(i))

                def fin_a():
                    k.op("dve", lambda h: h.tensor_copy(out=pIs[:], in_=pI[:]), reads=[BpI], writes=[BpIs])
                    k.op("dve", lambda h: h.scalar_tensor_tensor(out=idxf[:], in0=pIs[:, :, 1], scalar=128.0, in1=pIs[:, :, 0],
                                                                 op0=ALU.mult, op1=ALU.add),
                         reads=[BpIs], writes=[Bidxf])
                    k.op("dve", lambda h: h.tensor_copy(out=idxi[par][:], in_=idxf[:]), reads=[Bidxf], writes=[Bidx[par]])
                    if "dbg_idx" in P and layer == 0:
                        bd = Buf()
                        k.dma("sp", lambda h: h.dma_start(out=P["dbg_idx"][:, e * 4:(e + 1) * 4], in_=idxf[:]), reads=[Bidxf], writes=[bd])
                        k.dma("sp", lambda h: h.dma_start(out=P["dbg_pis"][:, e * 16:(e + 1) * 16], in_=pIs[:].rearrange("p a b -> p (a b)")), reads=[BpIs], writes=[bd])
                        P["_dbg_bufs"].append(bd)
                    k.op("dve", lambda h: h.tensor_tensor(out=gt[par][:], in0=pIs[:, :, 2], in1=pIs[:, :, 3], op=ALU.add),
                         reads=[BpIs], writes=[Bgt[par]])
                    for c in range(4):
                        k.dma("pool", lambda h, c=c: h.indirect_dma_start(
                            out=xs[c][:], out_offset=None, in_=H, in_offset=bass.IndirectOffsetOnAxis(ap=idxi[par][:, c:c + 1], axis=0)),
                            reads=[BH, Bidx[par]], writes=[Bxs[c]])
                stepsA.append(fin_a)

                def fin_b(c):
                    def f():
                        for kc in range(KC):
                            k.op("pe", lambda h, kc=kc: h.transpose(out=pT[:, kc, :], in_=xs[c][:, kc * 128:(kc + 1) * 128], identity=identb[:]),
                                 reads=[Bxs[c], Bidb], writes=[BpT])
                        k.op("act", lambda h: h.copy(out=xsT[par][:, :, c * 128:(c + 1) * 128], in_=pT[:]),
                             reads=[BpT], writes=[BxsT[par]])
                    return f
                for c in range(4):
                    stepsB.append(fin_b(c))
                return stepsA, stepsB

            def ffn(e, inter, interB):
                par = e % 2
                n_inter = len(inter)
                done = 0
                doneB = 0
                for fc in range(16):
                    c = fc // 4
                    s2 = fc % 2
                    for (wc, bw, pp, bp) in [(w1c[c], Bw1[c], pa[s2], Bpa[s2]), (w3c[c], Bw3[c], pu[s2], Bpu[s2])]:
                        for kc in range(KC):
                            k.op("pe", lambda h, kc=kc, wc=wc, pp=pp, fc=fc: h.matmul(
                                out=pp[:], lhsT=wc[:, kc, (fc % 4) * 128:(fc % 4 + 1) * 128], rhs=xsT[par][:, kc, :],
                                start=(kc == 0), stop=(kc == KC - 1)),
                                reads=[bw, BxsT[par]], writes=[bp])
                    k.op("act", lambda h, s2=s2: h.activation(out=sa[s2][:], in_=pa[s2][:], func=AF.Silu), reads=[Bpa[s2]], writes=[Bsa[s2]])
                    k.op("dve", lambda h, s2=s2, fc=fc: h.tensor_tensor(out=actT[:, fc, :], in0=sa[s2][:], in1=pu[s2][:], op=ALU.mult),
                         reads=[Bsa[s2], Bpu[s2]], writes=[BactT[fc]])
                    if fc % 4 == 3 and e + 1 < NE:
                        load_w13(e + 1, c)
                    target = (n_inter * (fc + 1)) // 16
                    while done < target:
                        inter[done]()
                        done += 1
                for c in range(4):
                    for dc in range(2):
                        q = cnt["y"] % 2
                        cnt["y"] += 1
                        for fc in range(16):
                            k.op("pe", lambda h, fc=fc, c=c, dc=dc, q=q: h.matmul(
                                out=py[q][:], lhsT=actT[:, fc, c * 128:(c + 1) * 128], rhs=w2c[fc // 4][:, fc % 4, dc * 512:(dc + 1) * 512],
                                start=(fc == 0), stop=(fc == 15)),
                                reads=[BactT[fc], Bw2[fc // 4]], writes=[Bpy[q]])
                        eng = "act" if dc == 0 else "dve"
                        if eng == "act":
                            k.op("act", lambda h, c=c, dc=dc, q=q: h.activation(out=yt[c % 2][:, dc * 512:(dc + 1) * 512], in_=py[q][:], func=AF.Copy,
                                                                                  scale=gt[par][:, c:c + 1]),
                                 reads=[Bpy[q], Bgt[par]], writes=[Byt[c % 2]])
                        else:
                            k.op("dve", lambda h, c=c, dc=dc, q=q: h.tensor_scalar(out=yt[c % 2][:, dc * 512:(dc + 1) * 512], in0=py[q][:],
                                                                                    scalar1=gt[par][:, c:c + 1], scalar2=None, op0=ALU.mult),
                                 reads=[Bpy[q], Bgt[par]], writes=[Byt[c % 2]])
                    if doneB < len(interB):
                        interB[doneB]()
                        doneB += 1
                    k.dma("pool", lambda h, c=c: h.indirect_dma_start(
                        out=XOUT, out_offset=bass.IndirectOffsetOnAxis(ap=idxi[par][:, c:c + 1], axis=0),
                        in_=yt[c % 2][:], in_offset=None, compute_op=ALU.add),
                        reads=[Byt[c % 2], Bidx[par]], writes=[Bxout])
                    if c == 3 and e + 1 < NE:
                        for cc in range(NCH):
                            load_w2(e + 1, cc)

            for c in range(NCH):
                load_w13(0, c)
            for c in range(NCH):
                load_w2(0, c)
            sA, sB = build_steps(0)
            for f in sA + sB:
                f()
            for e in range(NE):
                sA, sB = build_steps(e + 1) if e + 1 < NE else ([], [])
                ffn(e, sA, sB)
        k.barrier()


NH = 8
MIN = 3104
VW = 132


def stage_mlstm(cx, XIN, XOUT, Bxin, Bxout, P):
    nc, k = cx.nc, cx.k
    W = P["mlstm_w_in"]
    KS, VS, OGS = P["KS"], P["VS"], P["OGS"]
    HF = XOUT
    BKS, BVS, BOGS, BHF = Buf("KS"), Buf("VS"), Buf("OGS"), Buf("HF")
    with ExitStack() as st0:
        sb0 = lambda shape, dt, name=None: cx.sb(st0, shape, dt, name)
        QT = sb0([128, 4, S], BF16, "QT")
        KT = sb0([128, 4, S], BF16, "KT")
        G = sb0([128, NT, 32], F32, "G")
        identb = sb0([128, 128], BF16, "identb")
        identf = sb0([128, 128], F32, "identf")
        eps_t = sb0([128, 1], F32, "eps")
        one_t = sb0([128, 1], F32, "one")
        g_bc = sb0([128, D], F32, "g_bc")
        BQT = [Buf() for _ in range(NT)]
        BKT = [Buf() for _ in range(NT)]
        BG = [Buf() for _ in range(NT)]
        Bidb, Bidf, Beps, Bone, Bg = Buf(), Buf(), Buf(), Buf(), Buf()
        k.dma("pool", lambda h: h.dma_start(out=identb[:], in_=P["ident"]), writes=[Bidb])
        k.dma("sp", lambda h: h.dma_start(out=identf[:], in_=P["ident"]), writes=[Bidf])
        k.dma("sp", lambda h: h.dma_start(out=g_bc[:], in_=P["mlstm_norm_g"].to_broadcast([128, D])), writes=[Bg])
        k.op("dve", lambda h: h.memset(eps_t[:], EPS), writes=[Beps])
        k.op("dve", lambda h: h.memset(one_t[:], 1.0), writes=[Bone])

        with ExitStack() as st:
            sb = lambda shape, dt, name=None: cx.sb(st, shape, dt, name)
            ps = lambda shape, dt, name=None: cx.ps(st, shape, dt, name)
            w_in = sb([128, KC, MIN], BF16, "w_in")
            bgate = sb([128, 32], F32, "bgate")
            xt = [sb([128, D], F32, "xt%d" % i) for i in range(2)]
            junk = sb([128, D], BF16, "junk")
            xn = sb([128, D], BF16, "xn")
            xnT2 = [sb([128, KC, 128], BF16, "xnT%d" % i) for i in range(2)]
            BxnT2 = [Buf() for _ in range(2)]
            ss = sb([128, 1], F32, "ss")
            rstd = sb([128, 1], F32, "rstd")
            ktok = [sb([128, 512], BF16, "ktok%d" % i) for i in range(2)]
            vtile = [sb([128, NH, VW], BF16, "vt%d" % i) for i in range(2)]
            ogt = [sb([128, D], BF16, "ogt%d" % i) for i in range(2)]
            zt = sb([128, 32], F32, "zt")
            et = sb([128, 32], F32, "et")
            pA = ps([128, KC, 128], BF16, "pA")
            pQ = ps([128, 4, 128], F32, "pQ")
            pK = ps([128, 4, 128], F32, "pK")
            pTk = ps([128, 512], F32, "pTk")
            pV0 = ps([128, 512], F32, "pV0")
            pV1 = ps([128, 512], F32, "pV1")
            pGf = ps([128, 512], F32, "pG")
            pG = pGf[:, 0:32]
            B = {n: Buf(n) for n in ["w_in", "bgate", "junk", "xn", "xnT", "ss", "rstd", "zt", "et",
                                     "pA", "pQ", "pK", "pTk", "pV0", "pV1", "pG"]}
            Bxt = [Buf() for _ in range(2)]
            Bkt = [Buf() for _ in range(2)]
            Bvt = [Buf() for _ in range(2)]
            Bog = [Buf() for _ in range(2)]
            wv = W.rearrange("(kc p) n -> p kc n", p=128)
            for c0 in range(0, MIN, 776):
                k.dma("pool", lambda h, c0=c0: h.dma_start(out=w_in[:, :, c0:c0 + 776], in_=wv[:, :, c0:c0 + 776]), writes=[B["w_in"]])
            k.dma("sp", lambda h: h.dma_start(out=bgate[:], in_=P["mlstm_gate_bias"].to_broadcast([128, 32])), writes=[B["bgate"]])
            for i2 in range(2):
                k.op("pool", lambda h, i2=i2: h.memset(vtile[i2][:], 0.0), writes=[Bvt[i2]])
                k.op("pool", lambda h, i2=i2: h.memset(vtile[i2][:, :, 128:129], 1.0), writes=[Bvt[i2]])

            def p0_tile(i):
                s2 = i % 2
                x_t, bx = xt[s2], Bxt[s2]
                k.dma("sp", lambda h: h.dma_start(out=x_t[:], in_=XIN[i * 128:(i + 1) * 128, :]), reads=[Bxin], writes=[bx])
                k.op("act", lambda h: h.activation(out=junk[:], in_=x_t[:], func=AF.Square, accum_out=ss[:]),
                     reads=[bx], writes=[B["junk"], B["ss"]])
                k.op("act", lambda h: h.activation(out=rstd[:], in_=ss[:], func=AF.Ln, bias=eps_t[:], scale=1.0 / D),
                     reads=[B["ss"], Beps], writes=[B["rstd"]])
                k.op("act", lambda h: h.activation(out=rstd[:], in_=rstd[:], func=AF.Exp, scale=-0.5), reads=[B["rstd"]], writes=[B["rstd"]])
                k.op("dve", lambda h: h.scalar_tensor_tensor(out=xn[:], in0=x_t[:], scalar=rstd[:, 0:1], in1=g_bc[:],
                                                             op0=ALU.mult, op1=ALU.mult),
                     reads=[bx, B["rstd"], Bg], writes=[B["xn"]])
                for kc in range(KC):
                    k.op("pe", lambda h, kc=kc: h.transpose(out=pA[:, kc, :], in_=xn[:, kc * 128:(kc + 1) * 128], identity=identb[:]),
                         reads=[B["xn"], Bidb], writes=[B["pA"]])
                k.op("act", lambda h: h.copy(out=xnT2[s2][:], in_=pA[:]), reads=[B["pA"]], writes=[BxnT2[s2]])

            def p0_back(i):
                s2 = i % 2
                xnT = xnT2[s2]
                B["xnT"] = BxnT2[s2]
                for (pp, bn, col0) in [(pQ, "pQ", 0), (pK, "pK", 512)]:
                    for j in range(4):
                        for kc in range(KC):
                            k.op("pe", lambda h, kc=kc, j=j, pp=pp, col0=col0: h.matmul(
                                out=pp[:, j, :], lhsT=w_in[:, kc, col0 + j * 128:col0 + (j + 1) * 128], rhs=xnT[:, kc, :],
                                start=(kc == 0), stop=(kc == KC - 1)),
                                reads=[B["w_in"], B["xnT"]], writes=[B[bn]])
                k.op("act", lambda h: h.copy(out=QT[:, :, i * 128:(i + 1) * 128], in_=pQ[:]), reads=[B["pQ"]], writes=[BQT[i]])
                k.op("act", lambda h: h.activation(out=KT[:, :, i * 128:(i + 1) * 128], in_=pK[:], func=AF.Copy, scale=0.125),
                     reads=[B["pK"]], writes=[BKT[i]])
                for kc in range(KC):
                    k.op("pe", lambda h, kc=kc: h.matmul(out=pTk[:], lhsT=xnT[:, kc, :], rhs=w_in[:, kc, 512:1024],
                                                         start=(kc == 0), stop=(kc == KC - 1)),
                         reads=[B["w_in"], B["xnT"]], writes=[B["pTk"]])
                k.op("dve", lambda h: h.tensor_scalar(out=ktok[s2][:], in0=pTk[:], scalar1=0.125, scalar2=None, op0=ALU.mult),
                     reads=[B["pTk"]], writes=[Bkt[s2]])
                k.dma("sp", lambda h: h.dma_start(out=KS[i * 128:(i + 1) * 128, :], in_=ktok[s2][:]), reads=[Bkt[s2]], writes=[BKS])
                for half, (pp, bn) in enumerate([(pV0, "pV0"), (pV1, "pV1")]):
                    for kc in range(KC):
                        k.op("pe", lambda h, kc=kc, pp=pp, half=half: h.matmul(
                            out=pp[:], lhsT=xnT[:, kc, :], rhs=w_in[:, kc, 1024 + half * 512:1024 + (half + 1) * 512],
                            start=(kc == 0), stop=(kc == KC - 1)),
                            reads=[B["w_in"], B["xnT"]], writes=[B[bn]])
                    eng = "act" if half == 0 else "dve"
                    if eng == "act":
                        k.op("act", lambda h, pp=pp, half=half: h.copy(out=vtile[s2][:, half * 4:(half + 1) * 4, 0:128],
                                                                       in_=pp[:].rearrange("p (h d) -> p h d", d=128)),
                             reads=[B[bn]], writes=[Bvt[s2]])
                    else:
                        k.op("dve", lambda h, pp=pp, half=half: h.tensor_copy(out=vtile[s2][:, half * 4:(half + 1) * 4, 0:128],
                                                                              in_=pp[:].rearrange("p (h d) -> p h d", d=128)),
                             reads=[B[bn]], writes=[Bvt[s2]])
                k.dma("sp", lambda h: h.dma_start(out=VS[i * 128:(i + 1) * 128, :, :], in_=vtile[s2][:]), reads=[Bvt[s2]], writes=[BVS])
                for half, (pp, bn) in enumerate([(pV0, "pV0"), (pV1, "pV1")]):
                    for kc in range(KC):
                        k.op("pe", lambda h, kc=kc, pp=pp, half=half: h.matmul(
                            out=pp[:], lhsT=xnT[:, kc, :], rhs=w_in[:, kc, 2048 + half * 512:2048 + (half + 1) * 512],
                            start=(kc == 0), stop=(kc == KC - 1)),
                            reads=[B["w_in"], B["xnT"]], writes=[B[bn]])
                    k.op("act", lambda h, pp=pp, half=half: h.activation(out=ogt[s2][:, half * 512:(half + 1) * 512], in_=pp[:], func=AF.Sigmoid),
                         reads=[B[bn]], writes=[Bog[s2]])
                k.dma("sp", lambda h: h.dma_start(out=OGS[i * 128:(i + 1) * 128, :], in_=ogt[s2][:]), reads=[Bog[s2]], writes=[BOGS])
                for kc in range(KC):
                    k.op("pe", lambda h, kc=kc: h.matmul(out=pG, lhsT=xnT[:, kc, :], rhs=w_in[:, kc, 3072:3104],
                                                         start=(kc == 0), stop=(kc == KC - 1)),
                         reads=[B["w_in"], B["xnT"]], writes=[B["pG"]])
                k.op("dve", lambda h: h.tensor_tensor(out=zt[:], in0=pG, in1=bgate[:], op=ALU.add), reads=[B["pG"], B["bgate"]], writes=[B["zt"]])
                k.op("act", lambda h: h.activation(out=et[:], in_=zt[:], func=AF.Exp, scale=-1.0), reads=[B["zt"]], writes=[B["et"]])
                k.op("act", lambda h: h.activation(out=et[:], in_=et[:], func=AF.Ln, bias=one_t[:], scale=1.0), reads=[B["et"], Bone], writes=[B["et"]])
                zv = zt[:].rearrange("p (a b) -> p a b", b=8)
                ev = et[:].rearrange("p (a b) -> p a b", b=8)
                gv = G[:, i, :].rearrange("p (a b) -> p a b", b=8)
                k.op("dve", lambda h: h.tensor_copy(out=gv[:, 0:4:2, :], in_=zv[:, 0:4:2, :]), reads=[B["zt"]], writes=[BG[i]])
                k.op("dve", lambda h: h.tensor_scalar(out=gv[:, 1:4:2, :], in0=ev[:, 1:4:2, :], scalar1=-1.0, scalar2=None, op0=ALU.mult),
                     reads=[B["et"]], writes=[BG[i]])

            p0_tile(0)
            for i in range(NT):
                if i + 1 < NT:
                    p0_tile(i + 1)
                p0_back(i)
        k.barrier()
        if ML_STOP == "p0":
            return

        with ExitStack() as st:
            sb = lambda shape, dt, name=None: cx.sb(st, shape, dt, name)
            ps = lambda shape, dt, name=None: cx.ps(st, shape, dt, name)
            HB = P["HB"]
            BHB = Buf("HB")
            tri = [sb([128, 128], F32, "tri%d" % d_) for d_ in range(2)]
            negm = [sb([128, 128], F32, "negm%d" % d_) for d_ in range(2)]
            ones_c = sb([128, 1], BF16, "ones_c")
            w_out = sb([128, KC, D], BF16, "w_out")
            og_bc = sb([128, D], F32, "og_bc")
            tmpn = sb([128, 4, 128], F32, "tmpn")
            dsm = sb([128, 16], F32, "dsm")
            dtm = sb([128, NH], F32, "dtm")
            den = sb([128, NH], F32, "den")
            kw = sb([128, NH, 64], BF16, "kw")
            ssh = sb([128, NH], F32, "ssh")
            yb = sb([128, D], BF16, "yb")
            ybT = sb([128, KC, 128], BF16, "ybT")
            xo = sb([128, D], F32, "xo")
            def per_dir(shape, dt, name):
                return [sb(shape, dt, "%s_%d" % (name, d_)) for d_ in range(2)]
            Cst_d = per_dir([128, NH, VW], F32, "Cst")
            Cbf_d = per_dir([128, NH, VW], BF16, "Cbf")
            LFB_d = per_dir([128, NH, 128], F32, "LFB")
            bias_d = per_dir([128, NH], F32, "bias_s")
            eb_d = per_dir([128, NH], F32, "eb")
            ebL_d = per_dir([128, NH], F32, "ebL")
            DT_d = per_dir([128, NH, 128], F32, "DT")
            SwT_d = per_dir([128, NH, 128], BF16, "SwT")
            hd_d = [[sb([128, NH, 128], F32, "hdir%d%d" % (d_, i)) for i in range(2)] for d_ in range(2)]
            ktl_d = [[sb([128, 512], BF16, "ktl%d%d" % (d_, i)) for i in range(2)] for d_ in range(2)]
            vtl_d = [[sb([128, NH, VW], BF16, "vtl%d%d" % (d_, i)) for i in range(2)] for d_ in range(2)]
            hfl_s = sb([128, D], F32, "hfl")
            ogl_s = sb([128, D], BF16, "ogl")
            xl_s = sb([128, D], F32, "xl")
            ktm_d = [[sb([128, 4, 128], BF16, "ktm%d%d" % (d_, i)) for i in range(2)] for d_ in range(2)]
            pSs = [ps([128, 4, 128], F32, "pS%d" % i) for i in range(2)]
            pBMs = [ps([128, 4, 128], F32, "pBM%d" % i) for i in range(2)]
            pNi = ps([128, 4, 128], F32, "pNi")
            pNe = ps([128, 4, 128], F32, "pNe")
            pSmCu = ps([128, 512], F32, "pSmCu")
            pCu = pSmCu[:, 0:3 * VW].rearrange("p (a b) -> p a b", b=VW)
            pSm_d = [pSmCu[:, 400 + 24 * d_:424 + 24 * d_] for d_ in range(2)]
            pA = ps([128, KC, 128], BF16, "pA")
            pY = pNi[:].rearrange("p a b -> p (a b)")
            Bsh = {n: Buf(n) for n in ["tri", "negm", "ones_c", "w_out", "og_bc", "tmpn", "dsm", "dtm", "den", "kw", "ssh",
                                       "yb", "ybT", "xo", "pNi", "pNe", "pSm", "pA", "hfl", "ogl", "xl"]}
            Bsh["pCu"] = Bsh["pSm"]
            Bsh["pY"] = Bsh["pNi"]
            Bd = []
            for d_ in range(2):
                bb = dict(Bsh)
                for n in ["Cst", "Cbf", "LFB", "bias_s", "eb", "ktm", "pS", "pBM"]:
                    bb[n] = Buf(n + str(d_))
                bb["hd"] = [Buf(), Buf()]
                bb["Csth"] = [Buf() for _ in range(NH)]
                bb["DT"] = [Buf(), Buf()]
                bb["SwT"] = [Buf(), Buf()]
                bb["ebL"] = [Buf(), Buf()]
                bb["ktl"] = [Buf(), Buf()]
                bb["vtl"] = [Buf(), Buf()]
                Bd.append(bb)
            k.dma("sp", lambda h: h.dma_start(out=tri[0][:], in_=P["tri_f"]), writes=[Bsh["tri"]])
            k.dma("sp", lambda h: h.dma_start(out=tri[1][:], in_=P["tri_b"]), writes=[Bsh["tri"]])
            k.dma("sp", lambda h: h.dma_start(out=negm[0][:], in_=P["negm_f"]), writes=[Bsh["negm"]])
            k.dma("sp", lambda h: h.dma_start(out=negm[1][:], in_=P["negm_b"]), writes=[Bsh["negm"]])
            k.dma("pool", lambda h: h.dma_start(out=w_out[:], in_=P["mlstm_w_out"].rearrange("(kc p) n -> p kc n", p=128)), writes=[Bsh["w_out"]])
            k.dma("sp", lambda h: h.dma_start(out=og_bc[:], in_=P["mlstm_out_norm_g"].to_broadcast([128, D])), writes=[Bsh["og_bc"]])
            k.op("pool", lambda h: h.memset(ones_c[:], 1.0), writes=[Bsh["ones_c"]])
            for d_ in range(2):
                k.op("pool", lambda h, d_=d_: h.memset(ktm_d[d_][0][:], 0.0), writes=[Bd[d_]["ktm"]])
                k.op("pool", lambda h, d_=d_: h.memset(ktm_d[d_][1][:], 0.0), writes=[Bd[d_]["ktm"]])
                k.op("dve", lambda h, d_=d_: h.memset(Cst_d[d_][:], 0.0), writes=Bd[d_]["Csth"])
                k.op("pool", lambda h, d_=d_: h.memset(Cbf_d[d_][:], 0.0), writes=[Bd[d_]["Cbf"]])

            def chunk(dr, c, step):
                B = Bd[dr]
                s2 = step % 2
                late = step >= NT // 2
                l_last = 127 if dr == 0 else 0
                Cst, Cbf, LFB, bias_s, eb, ebL = Cst_d[dr], Cbf_d[dr], LFB_d[dr], bias_d[dr], eb_d[dr], ebL_d[dr]
                DT, SwT, hd, ktm = DT_d[dr], SwT_d[dr], hd_d[dr][s2], ktm_d[dr]
                hfl, ogl, xl = hfl_s, ogl_s, xl_s
                pS, pBM = pSs[dr], pBMs[dr]
                pSm = pSm_d[dr]
                lf = G[:, c, 8 + 16 * dr:16 + 16 * dr]
                ig = G[:, c, 16 * dr:8 + 16 * dr]
                kt_, bkt = ktl_d[dr][s2], B["ktl"][s2]
                vt_, bvt = vtl_d[dr][s2], B["vtl"][s2]
                bhd = B["hd"][s2]
                csl = slice(c * 128, (c + 1) * 128)
                k.dma("sp", lambda h: h.dma_start(out=kt_[:], in_=KS[csl, :]), reads=[BKS], writes=[bkt])
                k.dma("sp", lambda h: h.dma_start(out=vt_[:], in_=VS[csl, :, :]), reads=[BVS], writes=[bvt])
                k.op("act", lambda h: h.copy(out=ktm[0][0:64, :, :], in_=KT[0:64, :, csl]), reads=[BKT[c]], writes=[B["ktm"]])
                k.op("pool", lambda h: h.tensor_copy(out=ktm[1][64:128, :, :], in_=KT[64:128, :, csl]), reads=[BKT[c]], writes=[B["ktm"]])
                k.op("pe", lambda h: h.matmul(out=pSm[:, 0:8], lhsT=tri[dr][:], rhs=lf, start=True, stop=True),
                     reads=[B["tri"], BG[c]], writes=[B["pSm"]])
                k.op("dve", lambda h: h.tensor_copy(out=LFB[:], in_=lf.unsqueeze(2).to_broadcast([128, NH, 128])), reads=[BG[c]], writes=[B["LFB"]])
                k.op("dve", lambda h: h.tensor_tensor(out=bias_s[:], in0=ig, in1=pSm[:, 0:8], op=ALU.subtract),
                     reads=[BG[c], B["pSm"]], writes=[B["bias_s"]])
                k.op("act", lambda h: h.activation(out=eb[:], in_=pSm[:, 0:8], func=AF.Exp), reads=[B["pSm"]], writes=[B["eb"]])

                def half_front(h0, hf):
                    k.step()
                    for hh in range(4):
                        k.op("pe", lambda h, hh=hh: h.matmul(out=pBM[:, hh, :], lhsT=LFB[:, h0 + hh, :], rhs=tri[dr][:],
                                                            start=(hh == 0), stop=False),
                             reads=[B["LFB"], B["tri"]], writes=[B["pBM"]])
                        k.op("pe", lambda h, hh=hh: h.matmul(out=pBM[:, hh, :], lhsT=identf[:], rhs=negm[dr][:],
                                                            start=False, stop=(hh == 3)),
                             reads=[Bidf, B["negm"]], writes=[B["pBM"]])
                    for hh in range(4):
                        hd_ = h0 + hh
                        j, r = hd_ // 2, hd_ % 2
                        k.op("pe", lambda h, hh=hh, j=j, r=r: h.matmul(
                            out=pS[:, hh, :], lhsT=ktm[r][:, j, :], rhs=QT[:, j, csl],
                            start=(hh == 0), stop=(hh == 3)),
                            reads=[B["ktm"], BQT[c]], writes=[B["pS"]])
                    for hh in range(4):
                        k.op("act", lambda h, hh=hh: h.activation(out=DT[:, h0 + hh, :], in_=pBM[:, hh, :], func=AF.Exp,
                                                                  bias=bias_s[:, h0 + hh:h0 + hh + 1], scale=1.0),
                             reads=[B["pBM"], B["bias_s"]], writes=[B["DT"][hf]])
                    k.op("act", lambda h: h.activation(out=ebL[:, h0:h0 + 4], in_=pBM[:, :, l_last], func=AF.Exp),
                         reads=[B["pBM"]], writes=[B["ebL"][hf]])
                    k.op("dve", lambda h: h.tensor_tensor(out=SwT[:, h0:h0 + 4, :], in0=pS[:], in1=DT[:, h0:h0 + 4, :], op=ALU.mult),
                         reads=[B["pS"], B["DT"][hf]], writes=[B["SwT"][hf]])

                def half_back(h0, hf):
                    k.step()
                    for hh in range(4):
                        hd_ = h0 + hh
                        j, r = hd_ // 2, hd_ % 2
                        k.op("pe", lambda h, hh=hh, hd_=hd_: h.matmul(out=pNi[:, hh, :], lhsT=SwT[:, hd_, :], rhs=vt_[:, hd_, 0:128],
                                                                     start=(hh == 0), stop=(hh == 3)),
                             reads=[B["SwT"][hf], bvt], writes=[B["pNi"]])
                        k.op("pe", lambda h, hh=hh, hd_=hd_, j=j, r=r: h.matmul(
                            out=pNe[:, hh, :], lhsT=QT[:, j, csl], rhs=Cbf[:, hd_, 0:128],
                            start=(hh == 0), stop=(hh == 3)),
                            reads=[BQT[c], B["Cbf"]], writes=[B["pNe"]])
                        k.op("pe", lambda h, hh=hh, hd_=hd_: h.matmul(out=pSm[:, 8 + hd_:9 + hd_], lhsT=SwT[:, hd_, :], rhs=ones_c[:],
                                                                     start=True, stop=True),
                             reads=[B["SwT"][hf], B["ones_c"]], writes=[B["pSm"]])
                        k.op("pe", lambda h, hh=hh, hd_=hd_, j=j, r=r: h.matmul(
                            out=pSm[:, 16 + hd_:17 + hd_], lhsT=QT[:, j, csl], rhs=Cbf[:, hd_, 128:129],
                            start=True, stop=True),
                            reads=[BQT[c], B["Cbf"]], writes=[B["pSm"]])
                    ebh = eb[:, h0:h0 + 4]
                    k.op("dve", lambda h: h.tensor_tensor(out=tmpn[:], in0=pNe[:], in1=ebh.unsqueeze(2).to_broadcast([128, 4, 128]), op=ALU.mult),
                         reads=[B["pNe"], B["eb"]], writes=[B["tmpn"]])
                    k.op("dve", lambda h: h.tensor_tensor(out=hd[:, h0:h0 + 4, :], in0=pNi[:], in1=tmpn[:], op=ALU.add),
                         reads=[B["pNi"], B["tmpn"]], writes=[bhd])

                def finish_den():
                    k.op("dve", lambda h: h.tensor_copy(out=dsm[:], in_=pSm[:, 8:24]), reads=[B["pSm"]], writes=[B["dsm"]])
                    k.op("dve", lambda h: h.tensor_tensor(out=dtm[:], in0=dsm[:, 8:16], in1=eb[:], op=ALU.mult), reads=[B["dsm"], B["eb"]], writes=[B["dtm"]])
                    k.op("dve", lambda h: h.tensor_tensor(out=den[:], in0=dtm[:], in1=dsm[:, 0:8], op=ALU.add), reads=[B["dtm"], B["dsm"]], writes=[B["den"]])
                    k.op("dve", lambda h: h.scalar_tensor_tensor(out=dtm[:], in0=den[:], scalar=-1.0, in1=den[:], op0=ALU.mult, op1=ALU.max),
                         reads=[B["den"]], writes=[B["dtm"]])
                    k.op("dve", lambda h: h.tensor_scalar(out=den[:], in0=dtm[:], scalar1=1.0, scalar2=None, op0=ALU.max), reads=[B["dtm"]], writes=[B["den"]])
                    k.op("dve", lambda h: h.reciprocal(out=den[:], in_=den[:]), reads=[B["den"]], writes=[B["den"]])
                    k.op("dve", lambda h: h.tensor_tensor(out=hd[:], in0=hd[:], in1=den[:, :].unsqueeze(2).to_broadcast([128, NH, 128]), op=ALU.mult),
                         reads=[bhd, B["den"]], writes=[bhd])

                half_front(0, 0)
                half_back(0, 0)
                half_front(4, 1)
                half_back(4, 1)
                finish_den()
                k.step()
                k.op("dve", lambda h: h.tensor_tensor(out=kw[:], in0=kt_[:].rearrange("p (h d) -> p h d", d=64),
                                                     in1=DT[:, :, l_last:l_last + 1].to_broadcast([128, NH, 64]), op=ALU.mult),
                     reads=[bkt, B["DT"][0], B["DT"][1]], writes=[B["kw"]])
                kwp = kw[:].rearrange("p (j r) d -> p j (r d)", r=2)
                cu_banks = [(pCu, B["pCu"]),
                            (pS[:].rearrange("p a b -> p (a b)")[:, 0:3 * VW].rearrange("p (a b) -> p a b", b=VW), B["pS"]),
                            (pBM[:].rearrange("p a b -> p (a b)")[:, 0:3 * VW].rearrange("p (a b) -> p a b", b=VW), B["pBM"])]
                for hd_ in range(NH):
                    j = hd_ // 2
                    cu, bcu = cu_banks[hd_ // 3]
                    slot = hd_ % 3
                    k.op("pe", lambda h, hd_=hd_, j=j, slot=slot, cu=cu: h.matmul(out=cu[:, slot, 0:129], lhsT=kwp[:, j, :], rhs=vt_[:, hd_, 0:129],
                                                                               start=True, stop=True),
                         reads=[B["kw"], bvt], writes=[bcu])
                for hd_ in range(NH):
                    r = hd_ % 2
                    cu, bcu = cu_banks[hd_ // 3]
                    slot = hd_ % 3
                    rs = slice(r * 64, (r + 1) * 64)
                    k.op("dve", lambda h, hd_=hd_, slot=slot, rs=rs, cu=cu: h.scalar_tensor_tensor(
                        out=Cst[rs, hd_, 0:129], in0=Cst[rs, hd_, 0:129], scalar=ebL[rs, hd_:hd_ + 1], in1=cu[rs, slot, 0:129],
                        op0=ALU.mult, op1=ALU.add),
                        reads=[B["Csth"][hd_], B["ebL"][hd_ // 4], bcu], writes=[B["Csth"][hd_]])
                k.op("act", lambda h: h.copy(out=Cbf[:], in_=Cst[:]), reads=B["Csth"], writes=[B["Cbf"]])
                k.step()
                hs = hd[:].rearrange("p h d -> p (h d)")
                if not late:
                    park, bpark = (HF, BHF) if dr == 0 else (HB, BHB)
                    k.dma("sp", lambda h: h.dma_start(out=park[csl, :], in_=hs), reads=[bhd], writes=[bpark])
                    return
                pending.append((dr, c, s2))

            def epilogue(dr, c, s2):
                B = Bd[dr]
                hd, bhd = hd_d[dr][s2], B["hd"][s2]
                hfl, ogl, xl = hfl_s, ogl_s, xl_s
                csl = slice(c * 128, (c + 1) * 128)
                hs = hd[:].rearrange("p h d -> p (h d)")
                sqh = xo[:]
                other, bother = (HB, BHB) if dr == 0 else (HF, BHF)
                k.dma("sp", lambda h: h.dma_start(out=hfl[:], in_=other[csl, :]), reads=[bother], writes=[B["hfl"]])
                k.dma("sp", lambda h: h.dma_start(out=ogl[:], in_=OGS[csl, :]), reads=[BOGS], writes=[B["ogl"]])
                k.dma("sp", lambda h: h.dma_start(out=xl[:], in_=XIN[csl, :]), reads=[Bxin], writes=[B["xl"]])
                k.step()
                k.op("dve", lambda h: h.tensor_tensor(out=hs, in0=hs, in1=hfl[:], op=ALU.add), reads=[bhd, B["hfl"]], writes=[bhd])
                k.op("act", lambda h: h.activation(out=sqh, in_=hs, func=AF.Square), reads=[bhd], writes=[B["xo"]])
                k.op("dve", lambda h: h.tensor_reduce(out=ssh[:], in_=xo[:].rearrange("p (h d) -> p h d", d=128), axis=AX.X, op=ALU.add),
                     reads=[B["xo"]], writes=[B["ssh"]])
                k.op("act", lambda h: h.activation(out=ssh[:], in_=ssh[:], func=AF.Ln, bias=eps_t[:], scale=1.0 / 128),
                     reads=[B["ssh"], Beps], writes=[B["ssh"]])
                k.op("act", lambda h: h.activation(out=ssh[:], in_=ssh[:], func=AF.Exp, scale=-0.5), reads=[B["ssh"]], writes=[B["ssh"]])
                k.step()
                k.op("dve", lambda h: h.tensor_tensor(out=hd[:], in0=hd[:], in1=ssh[:, :].unsqueeze(2).to_broadcast([128, NH, 128]), op=ALU.mult),
                     reads=[bhd, B["ssh"]], writes=[bhd])
                k.op("dve", lambda h: h.tensor_tensor(out=hs, in0=hs, in1=og_bc[:], op=ALU.mult), reads=[bhd, B["og_bc"]], writes=[bhd])
                k.op("dve", lambda h: h.tensor_tensor(out=yb[:], in0=hs, in1=ogl[:], op=ALU.mult), reads=[bhd, B["ogl"]], writes=[B["yb"]])
                k.step()
                for kc in range(KC):
                    k.op("pe", lambda h, kc=kc: h.transpose(out=pA[:, kc, :], in_=yb[:, kc * 128:(kc + 1) * 128], identity=identb[:]),
                         reads=[B["yb"], Bidb], writes=[B["pA"]])
                k.op("act", lambda h: h.copy(out=ybT[:], in_=pA[:]), reads=[B["pA"]], writes=[B["ybT"]])
                for half in range(2):
                    k.step()
                    for kc in range(KC):
                        k.op("pe", lambda h, kc=kc, half=half: h.matmul(out=pY, lhsT=ybT[:, kc, :], rhs=w_out[:, kc, half * 512:(half + 1) * 512],
                                                                        start=(kc == 0), stop=(kc == KC - 1)),
                             reads=[B["ybT"], B["w_out"]], writes=[B["pY"]])
                    k.op("dve", lambda h, half=half: h.tensor_tensor(out=xo[:, half * 512:(half + 1) * 512], in0=pY,
                                                                     in1=xl[:, half * 512:(half + 1) * 512], op=ALU.add),
                         reads=[B["pY"], B["xl"]], writes=[B["xo"]])
                k.step()
                k.dma("sp", lambda h: h.dma_start(out=XOUT[csl, :], in_=xo[:]), reads=[B["xo"]], writes=[Bxout])

            pending = []

            def merge3(a, b, e):
                lists = [l for l in (a, b, e) if l]
                pos = [0] * len(lists)
                while any(p < len(l) for p, l in zip(pos, lists)):
                    best = min((i for i in range(len(lists)) if pos[i] < len(lists[i])),
                               key=lambda i: (pos[i] / len(lists[i]), i))
                    k.replay(lists[best][pos[best]])
                    pos[best] += 1

            for step in range(NT + 1):
                todo, pending = pending, []
                sa = sb_ = []
                if step < NT:
                    k.begin_record()
                    chunk(0, step, step)
                    sa = k.end_record()
                    k.begin_record()
                    chunk(1, NT - 1 - step, step)
                    sb_ = k.end_record()
                k.begin_record()
                for (dr_, c_, s2_) in todo:
                    epilogue(dr_, c_, s2_)
                    k.step()
                se = k.end_record()
                merge3(sa, sb_, se)
        k.barrier()


def _t5_bucket(rel):
    nb = 16
    ret = (rel > 0).astype(np.int32) * nb
    n = np.abs(rel)
    max_exact = nb // 2
    nf = np.maximum(n, 1).astype(np.float32)
    large = max_exact + (np.log(nf / max_exact) / math.log(128 / max_exact) * (nb - max_exact)).astype(np.int32)
    large = np.minimum(large, nb - 1)
    return ret + np.where(n < max_exact, n, large)


def _attn_tables(rel_bias):
    p = np.arange(128)[:, None, None]
    j = np.arange(3)[None, :, None]
    q = np.arange(128)[None, None, :]
    rel = j * 128 + p - 128 - q
    bucket = _t5_bucket(rel)
    bias = rel_bias[bucket]
    bias = np.ascontiguousarray(bias.transpose(0, 1, 3, 2)).astype(np.float32)
    mask = np.where(np.abs(rel) <= 128, 0.0, NEG).astype(np.float32)
    mask = np.ascontiguousarray(np.broadcast_to(mask[:, :, None, :], (128, 3, 16, 128)))
    return bias, mask


N_CORES = 4
N_STAGES = 4
DEBUG = False
ML_SKIP = set()
ML_STOP = None
CHAIN = None
LAST = {}


def build_program(n_stages=None):
    n_stages = N_STAGES if n_stages is None else n_stages
    nc = bass.Bass("TRN2", target_bir_lowering=False)
    P = {}

    def inp(name, shape, dt=F32):
        P[name] = nc.dram_tensor(name, list(shape), dt, kind="ExternalInput").ap()

    inp("x", [S, D])
    inp("attn_norm_g", [1, D])
    inp("attn_w_in", [D, 1536])
    inp("attn_q_norm_g", [1, 64])
    inp("attn_k_norm_g", [1, 64])
    inp("attn_sink", [1, 16])
    inp("attn_w_out", [D, D])
    inp("attn_bias", [128, 3, 16, 128])
    inp("attn_mask", [128, 3, 16, 128])
    inp("ident", [128, 128])
    inp("ustrict", [128, 128])
    inp("iota512", [128, CAP])
    inp("rh_const", [128, NE * NT * 4])
    inp("mlstm_norm_g", [1, D])
    inp("mlstm_w_in", [D, MIN])
    inp("mlstm_gate_bias", [1, 32])
    inp("mlstm_out_norm_g", [1, D])
    inp("mlstm_w_out", [D, D])
    inp("tri_f", [128, 128])
    inp("tri_b", [128, 128])
    inp("negm_f", [128, 128])
    inp("negm_b", [128, 128])
    inp("ffn_norm_g", [2, D])
    inp("router_w", [2, D, NE])
    inp("expert_w1", [2, NE, D, 2 * D])
    inp("expert_w3", [2, NE, D, 2 * D])
    inp("expert_w2", [2, NE, 2 * D, D])
    out = nc.dram_tensor("out", [S, D], F32, kind="ExternalOutput").ap()
    if DEBUG is True:
        for n, shp in [("dbg_aff", [128, 512]), ("dbg_sel", [128, 512]), ("dbg_pos", [128, 512]), ("dbg_lo", [128, 16]),
                       ("dbg_hi", [128, 16]), ("dbg_idx", [128, 64]), ("dbg_pis", [128, 256])]:
            P[n] = nc.dram_tensor(n, shp, F32, kind="ExternalOutput").ap()
    P["_dbg_bufs"] = []
    if DEBUG == "ml":
        for n in ["dbg_hf", "dbg_hs"]:
            P[n] = nc.dram_tensor(n, [S, D], F32, kind="ExternalOutput").ap()
    scr = {}
    for n in ["XA", "XB"]:
        scr[n] = nc.dram_tensor(n, [S, D], F32, kind="Internal").ap()
    scr["XC"] = scr["XA"]
    H = nc.dram_tensor("Hs", [S, D], BF16, kind="Internal").ap()
    P["KS"] = nc.dram_tensor("KS", [S, 512], BF16, kind="Internal").ap()
    P["VS"] = nc.dram_tensor("VS", [S, NH, VW], BF16, kind="Internal").ap()
    P["OGS"] = nc.dram_tensor("OGS", [S, D], BF16, kind="Internal").ap()
    with ExitStack() as st:
        cx = Ctx(nc, st)
        k = cx.k
        Bx = Buf("x")
        chain = [("attn", P["x"], scr["XA"]), ("moe0", scr["XA"], scr["XB"]), ("mlstm", scr["XB"], scr["XC"]),
                 ("moe1", scr["XC"], out)][:n_stages]
        if CHAIN is not None:
            chain = [(CHAIN[0], P["x"], out)]
        chain[-1] = (chain[-1][0], chain[-1][1], out)
        bin_ = Bx
        BH = Buf("H")
        for (name, src, dst) in chain:
            bout = Buf(name + "_out")
            if name == "attn":
                stage_attn(cx, src, dst, bin_, bout, P)
                k.barrier()
            elif name == "moe0":
                stage_moe(cx, src, dst, H, bin_, bout, BH, P, 0)
            elif name == "moe1":
                stage_moe(cx, src, dst, H, bin_, bout, BH, P, 1)
            elif name == "mlstm":
                P["HB"] = out if dst is not out else scr["XB"]
                stage_mlstm(cx, src, dst, bin_, bout, P)
            bin_ = bout
        k.wait_bufs("sp", [bin_] + P["_dbg_bufs"])
        k.emit()
    print("program: insts=%d waits=%d per-engine=%s" % (k.n_inst, k.n_wait, {e: len(k.prog[e]) for e in ENGS}))
    return nc


def _consts():
    ident = np.eye(128, dtype=np.float32)
    ustrict = np.triu(np.ones((128, 128), dtype=np.float32), 1)
    iota512 = np.ascontiguousarray(np.broadcast_to(np.arange(CAP, dtype=np.float32), (128, CAP)))
    rh = np.zeros((128, NE, NT, 4), dtype=np.float32)
    rh[:, :, :, 0] = np.arange(128, dtype=np.float32)[:, None, None]
    rh[:, :, :, 1] = np.arange(NT, dtype=np.float32)[None, None, :]
    tri_f = np.triu(np.ones((128, 128), dtype=np.float32), 0)
    tri_b = np.tril(np.ones((128, 128), dtype=np.float32), 0)
    negm_f = np.where(np.arange(128)[:, None] > np.arange(128)[None, :], NEG, 0.0).astype(np.float32)
    negm_b = np.where(np.arange(128)[:, None] < np.arange(128)[None, :], NEG, 0.0).astype(np.float32)
    return {"ident": ident, "ustrict": ustrict, "iota512": iota512, "rh_const": rh.reshape(128, -1),
            "tri_f": tri_f, "tri_b": tri_b, "negm_f": negm_f, "negm_b": negm_b}


def kernel(**inputs):
    inputs = {k_: np.asarray(v) for k_, v in inputs.items()}
    x = inputs["x"].astype(np.float32, copy=False)
    bias, mask = _attn_tables(inputs["rel_bias"].astype(np.float32))
    consts = _consts()
    bi, bf = inputs["mlstm_b_i"][0].astype(np.float32), inputs["mlstm_b_f"][0].astype(np.float32)
    gate_bias = np.ascontiguousarray(np.concatenate([bi[0], bf[0], bi[1], bf[1]])[None, :])
    nc = build_program()
    in_maps = []
    for c in range(N_CORES):
        m = {
            "x": np.ascontiguousarray(x[c]),
            "attn_norm_g": np.ascontiguousarray(inputs["attn_norm_g"][0:1]),
            "attn_w_in": np.ascontiguousarray(inputs["attn_w_in"][0]),
            "attn_q_norm_g": np.ascontiguousarray(inputs["attn_q_norm_g"][0:1]),
            "attn_k_norm_g": np.ascontiguousarray(inputs["attn_k_norm_g"][0:1]),
            "attn_sink": np.ascontiguousarray(inputs["attn_sink"][0:1]),
            "attn_w_out": np.ascontiguousarray(inputs["attn_w_out"][0]),
            "attn_bias": bias, "attn_mask": mask,
            "mlstm_norm_g": np.ascontiguousarray(inputs["mlstm_norm_g"][0:1]),
            "mlstm_w_in": np.ascontiguousarray(inputs["mlstm_w_in"][0]),
            "mlstm_gate_bias": gate_bias,
            "mlstm_out_norm_g": np.ascontiguousarray(inputs["mlstm_out_norm_g"][0:1]),
            "mlstm_w_out": np.ascontiguousarray(inputs["mlstm_w_out"][0]),
            "ffn_norm_g": inputs["ffn_norm_g"], "router_w": inputs["router_w"],
            "expert_w1": inputs["expert_w1"], "expert_w3": inputs["expert_w3"], "expert_w2": inputs["expert_w2"],
        }
        m.update(consts)
        in_maps.append(m)
    res = run_bass_kernel_spmd(nc, in_maps, core_ids=list(range(N_CORES)))
    LAST["res"] = res.results
    return np.stack([r["out"] for r in res.results], axis=0)
```

```python
import math
from contextlib import ExitStack

import numpy as np
import concourse.bass as bass
import concourse.mybir as mybir
from concourse.bass_utils import run_bass_kernel_spmd

F32 = mybir.dt.float32
BF16 = mybir.dt.bfloat16
I32 = mybir.dt.int32
ALU = mybir.AluOpType
AF = mybir.ActivationFunctionType
AX = mybir.AxisListType

ENGS = ["pe", "dve", "act", "pool", "sp"]

S = 4096
D = 1024
NT = S // 128
KC = D // 128
NEG = -30000.0
EPS = 1e-6


class Buf:
    __slots__ = ("name", "w", "r")

    def __init__(self, name=""):
        self.name = name
        self.w = None
        self.r = []


class KB:
    N_DMA_SEMS = 56
    N_HW_SEMS = 40

    def __init__(self, nc, stack):
        self.nc = nc
        self.stack = stack
        self.handles = {"pe": nc.tensor, "dve": nc.vector, "act": nc.scalar,
                        "pool": nc.gpsimd, "sp": nc.sync}
        self.prog = {e: [] for e in ENGS}
        self.epoch = 0
        self.sems = {}
        self.count = {}
        self._new_epoch_sems()
        self.dma_sems = [stack.enter_context(nc.semaphore("d%d" % i)) for i in range(self.N_DMA_SEMS)]
        self.dma_val = [0] * self.N_DMA_SEMS
        self.dma_next = 0
        self.dma_next_sw = 0
        self.seen = {e: {} for e in ENGS}
        self.n_inst = 0
        self.n_wait = 0
        self._rec = None

    def _new_epoch_sems(self):
        for e in ["pe", "dve", "act", "pool"]:
            self.sems[(e, self.epoch)] = self.stack.enter_context(self.nc.semaphore("s_%s_%d" % (e, self.epoch)))
            self.count[e] = 0

    def _sem_of(self, key):
        return self.sems[key] if isinstance(key, tuple) else self.dma_sems[key]

    def _wait(self, eng, toks):
        need = {}
        for t in toks:
            if t is None:
                continue
            k, v = t
            if isinstance(k, tuple):
                if k[1] < self.epoch:
                    continue
                if k[0] == "pe" and eng == "pe":
                    continue
            if need.get(k, 0) < v:
                need[k] = v
        for k, v in need.items():
            if self.seen[eng].get(k, 0) >= v:
                continue
            self.seen[eng][k] = v
            self.prog[eng].append(("wait", self._sem_of(k), v))
            self.n_wait += 1

    def _deps(self, eng, reads, writes):
        toks = []
        for b in reads:
            toks.append(b.w)
        for b in writes:
            toks.append(b.w)
            for t in b.r:
                if isinstance(t[0], tuple) and t[0][0] == eng:
                    continue
                toks.append(t)
        return toks

    def _commit(self, tok, reads, writes):
        for b in reads:
            b.r.append(tok)
            if len(b.r) > 64:
                best = {}
                for k, v in b.r:
                    if best.get(k, 0) < v:
                        best[k] = v
                b.r = list(best.items())
        for b in writes:
            b.w = tok
            b.r = []
        self.n_inst += 1

    def begin_record(self):
        self._rec = [[]]

    def step(self):
        if self._rec is not None and self._rec[-1]:
            self._rec.append([])

    def end_record(self):
        r, self._rec = self._rec, None
        return [st_ for st_ in r if st_]

    def replay(self, step_):
        for (kind, eng, fn, reads, writes) in step_:
            (self.op if kind == "op" else self.dma)(eng, fn, reads, writes)

    def replay_merged(self, a, b):
        ia = ib = 0
        while ia < len(a) or ib < len(b):
            if ib >= len(b) or (ia < len(a) and ia * len(b) <= ib * len(a)):
                self.replay(a[ia]); ia += 1
            else:
                self.replay(b[ib]); ib += 1

    def op(self, eng, fn, reads=(), writes=()):
        if self._rec is not None:
            self._rec[-1].append(("op", eng, fn, list(reads), list(writes)))
            return None
        self._wait(eng, self._deps(eng, reads, writes))
        self.count[eng] += 1
        key = (eng, self.epoch)
        tok = (key, self.count[eng])
        self.prog[eng].append(("op", fn, self.sems[key], 1))
        self._commit(tok, reads, writes)
        return tok

    def dma(self, eng, fn, reads=(), writes=()):
        if self._rec is not None:
            self._rec[-1].append(("dma", eng, fn, list(reads), list(writes)))
            return None
        if eng == "pool":
            j = self.N_HW_SEMS + self.dma_next_sw
            self.dma_next_sw = (self.dma_next_sw + 1) % (self.N_DMA_SEMS - self.N_HW_SEMS)
        else:
            j = self.dma_next
            self.dma_next = (j + 1) % self.N_HW_SEMS
        toks = self._deps("dma", reads, writes)
        if self.dma_val[j] > 0:
            toks.append((j, self.dma_val[j]))
        self._wait(eng, toks)
        self.dma_val[j] += 16
        tok = (j, self.dma_val[j])
        self.prog[eng].append(("op", fn, self.dma_sems[j], 16))
        self._commit(tok, reads, writes)
        return tok

    def wait_bufs(self, eng, bufs):
        toks = []
        for b in bufs:
            toks.append(b.w)
            toks.extend(b.r)
        self._wait(eng, toks)

    def barrier(self):
        toks = [((e, self.epoch), self.count[e]) for e in ["pe", "dve", "act", "pool"] if self.count[e] > 0]
        toks += [(j, v) for j, v in enumerate(self.dma_val) if v > 0]
        for e in ENGS:
            self._wait(e, [t for t in toks if not (isinstance(t[0], tuple) and t[0][0] == e)])
        self.epoch += 1
        self._new_epoch_sems()

    def emit(self):
        nc = self.nc
        prog = self.prog

        def run(e):
            def body(h):
                for item in prog[e]:
                    if item[0] == "wait":
                        h.wait_ge(item[1], item[2])
                    else:
                        item[1](h).then_inc(item[2], item[3])
            return body

        with nc.Block() as block:
            block.tensor(run("pe"))
            block.vector(run("dve"))
            block.scalar(run("act"))
            block.gpsimd(run("pool"))
            block.sync(run("sp"))


class Ctx:
    def __init__(self, nc, stack):
        self.nc = nc
        self.k = KB(nc, stack)
        self.uid = 0

    def sb(self, st, shape, dt, name=None):
        self.uid += 1
        t = st.enter_context(self.nc.sbuf_tensor("%s_%d" % (name or "t", self.uid), list(shape), dt))
        return t

    def ps(self, st, shape, dt, name=None):
        self.uid += 1
        t = st.enter_context(self.nc.psum_tensor("%s_%d" % (name or "p", self.uid), list(shape), dt))
        return t


def stage_attn(cx, XIN, XOUT, Bxin, Bxout, P):
    nc, k = cx.nc, cx.k
    with ExitStack() as st:
        sb = lambda shape, dt, name=None: cx.sb(st, shape, dt, name)
        ps = lambda shape, dt, name=None: cx.ps(st, shape, dt, name)
        w_in = sb([128, KC, 1536], BF16, "w_in")
        w_out = sb([128, KC, 1024], BF16, "w_out")
        g_bc = sb([128, D], F32, "g_bc")
        ident = sb([128, 128], BF16, "ident")
        BM = sb([128, 3, 16, 128], F32, "BM")
        gqk = sb([128, 20, 64], F32, "gqk")
        esink = sb([128, 16], F32, "esink")
        eps_t = sb([128, 1], F32, "eps")
        V_all = sb([128, NT, 4, 65], BF16, "V_all")
        kT_all = sb([64, 4, S], BF16, "kT_all")
        xt = [sb([128, D], F32, "xt%d" % i) for i in range(3)]
        sq = sb([128, 1280], F32, "sq")
        junk = sb([128, D], BF16, "junk")
        xn = sb([128, D], BF16, "xn")
        xnT = sb([128, KC, 128], BF16, "xnT")
        tmpq = sb([128, 20, 64], F32, "tmpq")
        qkn = sb([128, 20, 64], BF16, "qkn")
        qT = [sb([64, 16, 128], BF16, "qT%d" % i) for i in range(3)]
        tS = [sb([128, 512], F32, "tS%d" % i) for i in range(2)]
        PT = [sb([128, 512], BF16, "PT%d" % i) for i in range(2)]
        ot = sb([128, 16, 64], BF16, "ot")
        otT = sb([128, KC, 128], BF16, "otT")
        x1t = [sb([128, D], F32, "x1t%d" % i) for i in range(2)]
        ss = sb([128, 1], F32, "ss")
        rstd = sb([128, 1], F32, "rstd")
        ssq = sb([128, 20], F32, "ssq")
        rqk = sb([128, 20], F32, "rqk")
        den = sb([128, 4], F32, "den")
        rden = sb([128, 4], F32, "rden")
        pA = ps([128, KC, 128], BF16, "pA")
        pB = ps([128, 512], F32, "pB")
        pC = ps([128, 512], F32, "pC")
        pD = ps([128, 512], F32, "pD")
        pE = ps([64, 8, 128], BF16, "pE")
        pGs = [ps([128, 512], F32, "pG%d" % i) for i in range(2)]
        pH = ps([128, 4, 65], F32, "pH")

        B = {}
        for n in ["w_in", "w_out", "g_bc", "ident", "BM", "gqk", "esink", "eps", "V1", "sq", "junk",
                  "xn", "xnT", "tmpq", "qkn", "ot", "otT", "ss", "rstd", "ssq", "rqk", "den", "rden",
                  "pA", "pB", "pC", "pD", "pE", "pH", "gq_s", "gk_s", "mask_s", "sink_s"]:
            B[n] = Buf(n)
        Bxt = [Buf("xt%d" % i) for i in range(3)]
        BqT = [Buf("qT%d" % i) for i in range(3)]
        BtS = [Buf() for _ in range(2)]
        BpG = [Buf() for _ in range(2)]
        BPT = [Buf() for _ in range(2)]
        Bx1 = [Buf() for _ in range(2)]
        BV = [Buf("V%d" % i) for i in range(NT)]
        BkT = [Buf("kT%d" % i) for i in range(NT)]

        k.dma("pool", lambda h: h.dma_start(out=w_in[:], in_=P["attn_w_in"].rearrange("(kc p) n -> p kc n", p=128)),
              writes=[B["w_in"]])
        k.dma("pool", lambda h: h.dma_start(out=w_out[:], in_=P["attn_w_out"].rearrange("(kc p) n -> p kc n", p=128)),
              writes=[B["w_out"]])
        k.dma("pool", lambda h: h.dma_start(out=ident[:], in_=P["ident"]), writes=[B["ident"]])
        k.dma("sp", lambda h: h.dma_start(out=g_bc[:], in_=P["attn_norm_g"].to_broadcast([128, D])), writes=[B["g_bc"]])
        k.dma("sp", lambda h: h.dma_start(out=BM[:], in_=P["attn_bias"]), writes=[B["BM"]])
        mstage = [xt[0], xt[1], x1t[0], x1t[1], xt[2], sq]
        for j in range(3):
            for hh in range(2):
                buf = mstage[j * 2 + hh]
                bb = Buf()
                k.dma("sp", lambda h, j=j, hh=hh, buf=buf: h.dma_start(
                    out=buf[:, 0:1024], in_=P["attn_mask"][:, j, hh * 8:(hh + 1) * 8, :].rearrange("p h q -> p (h q)")),
                    writes=[bb])
                k.op("dve", lambda h, j=j, hh=hh, buf=buf: h.tensor_tensor(
                    out=BM[:, j, hh * 8:(hh + 1) * 8, :], in0=BM[:, j, hh * 8:(hh + 1) * 8, :],
                    in1=buf[:, 0:1024].rearrange("p (h q) -> p h q", q=128), op=ALU.add),
                    reads=[bb], writes=[B["BM"]])
                for tb in (Bxt + Bx1 + [B["sq"]]):
                    pass
        for tb in Bxt + Bx1 + [B["sq"]]:
            tb.r.append(B["BM"].w)
        gq_s = sb([128, 64], F32, "gq_s")
        gk_s = sb([128, 64], F32, "gk_s")
        k.dma("sp", lambda h: h.dma_start(out=gq_s[:], in_=P["attn_q_norm_g"].to_broadcast([128, 64])), writes=[B["gq_s"]])
        k.dma("sp", lambda h: h.dma_start(out=gk_s[:], in_=P["attn_k_norm_g"].to_broadcast([128, 64])), writes=[B["gk_s"]])
        k.op("dve", lambda h: h.tensor_scalar(out=gqk[:, 0:16, :], in0=gq_s[:, :].unsqueeze(1).to_broadcast([128, 16, 64]),
                                              scalar1=0.125, scalar2=None, op0=ALU.mult),
             reads=[B["gq_s"]], writes=[B["gqk"]])
        k.op("dve", lambda h: h.tensor_copy(out=gqk[:, 16:20, :], in_=gk_s[:, :].unsqueeze(1).to_broadcast([128, 4, 64])),
             reads=[B["gk_s"]], writes=[B["gqk"]])
        gq_col = sb([64, 1], F32, "gq_col")
        gk_col = sb([64, 1], F32, "gk_col")
        B["gq_col"], B["gk_col"] = Buf(), Buf()
        k.dma("sp", lambda h: h.dma_start(out=gq_col[:], in_=P["attn_q_norm_g"].rearrange("o d -> d o")), writes=[B["gq_col"]])
        k.dma("sp", lambda h: h.dma_start(out=gk_col[:], in_=P["attn_k_norm_g"].rearrange("o d -> d o")), writes=[B["gk_col"]])
        k.op("dve", lambda h: h.tensor_scalar(out=gq_col[:], in0=gq_col[:], scalar1=0.125, scalar2=None, op0=ALU.mult),
             reads=[B["gq_col"]], writes=[B["gq_col"]])
        k.dma("sp", lambda h: h.dma_start(out=esink[:], in_=P["attn_sink"].to_broadcast([128, 16])), writes=[B["esink"]])
        k.op("act", lambda h: h.activation(out=esink[:], in_=esink[:], func=AF.Exp), reads=[B["esink"]], writes=[B["esink"]])
        k.op("dve", lambda h: h.memset(eps_t[:], EPS), writes=[B["eps"]])
        k.op("pool", lambda h: h.memset(V_all[:, :, :, 64:65], 1.0), writes=[B["V1"]])

        def phase1(i):
            s3 = i % 3
            x_t, bx = xt[s3], Bxt[s3]
            k.dma("sp", lambda h: h.dma_start(out=x_t[:], in_=XIN[i * 128:(i + 1) * 128, :]), reads=[Bxin], writes=[bx])
            k.op("act", lambda h: h.activation(out=junk[:], in_=x_t[:], func=AF.Square, accum_out=ss[:]),
                 reads=[bx], writes=[B["junk"], B["ss"]])
            k.op("act", lambda h: h.activation(out=rstd[:], in_=ss[:], func=AF.Ln, bias=eps_t[:], scale=1.0 / D),
                 reads=[B["ss"], B["eps"]], writes=[B["rstd"]])
            k.op("act", lambda h: h.activation(out=rstd[:], in_=rstd[:], func=AF.Exp, scale=-0.5), reads=[B["rstd"]], writes=[B["rstd"]])
            k.op("dve", lambda h: h.scalar_tensor_tensor(out=xn[:], in0=x_t[:], scalar=rstd[:, 0:1], in1=g_bc[:],
                                                         op0=ALU.mult, op1=ALU.mult),
                 reads=[bx, B["rstd"], B["g_bc"]], writes=[B["xn"]])
            k.step()
            for kc in range(KC):
                k.op("pe", lambda h, kc=kc: h.transpose(out=pA[:, kc, :], in_=xn[:, kc * 128:(kc + 1) * 128], identity=ident[:]),
                     reads=[B["xn"], B["ident"]], writes=[B["pA"]])
            k.op("act", lambda h: h.copy(out=xnT[:], in_=pA[:]), reads=[B["pA"]], writes=[B["xnT"]])
            k.step()
            for c, (pp, bn) in enumerate([(pB, "pB"), (pC, "pC"), (pD, "pD")]):
                for kc in range(KC):
                    k.op("pe", lambda h, kc=kc, c=c, pp=pp: h.matmul(
                        out=pp[:], lhsT=xnT[:, kc, :], rhs=w_in[:, kc, c * 512:(c + 1) * 512],
                        start=(kc == 0), stop=(kc == KC - 1)),
                        reads=[B["xnT"], B["w_in"]], writes=[B[bn]])
            k.op("act", lambda h: h.activation(out=sq[:, 0:512], in_=pB[:], func=AF.Square), reads=[B["pB"]], writes=[B["sq"]])
            k.op("act", lambda h: h.activation(out=sq[:, 512:1024], in_=pC[:], func=AF.Square), reads=[B["pC"]], writes=[B["sq"]])
            k.op("act", lambda h: h.activation(out=sq[:, 1024:1280], in_=pD[:, 0:256], func=AF.Square), reads=[B["pD"]], writes=[B["sq"]])
            k.op("dve", lambda h: h.tensor_reduce(out=ssq[:], in_=sq[:].rearrange("p (h d) -> p h d", d=64), axis=AX.X, op=ALU.add),
                 reads=[B["sq"]], writes=[B["ssq"]])
            k.op("act", lambda h: h.activation(out=rqk[:], in_=ssq[:], func=AF.Ln, bias=eps_t[:], scale=1.0 / 64),
                 reads=[B["ssq"], B["eps"]], writes=[B["rqk"]])
            k.op("act", lambda h: h.activation(out=rqk[:], in_=rqk[:], func=AF.Exp, scale=-0.5), reads=[B["rqk"]], writes=[B["rqk"]])
            for (pp, bn, h0, nh) in [(pB, "pB", 0, 8), (pC, "pC", 8, 8), (pD, "pD", 16, 4)]:
                k.op("dve", lambda h, pp=pp, h0=h0, nh=nh: h.tensor_tensor(
                    out=qkn[:, h0:h0 + nh, :], in0=pp[:, 0:nh * 64].rearrange("p (h d) -> p h d", d=64),
                    in1=rqk[:, h0:h0 + nh].unsqueeze(2).to_broadcast([128, nh, 64]), op=ALU.mult),
                    reads=[B[bn], B["rqk"]], writes=[B["qkn"]])
            k.op("act", lambda h: h.copy(out=V_all[:, i, :, 0:64], in_=pD[:, 256:512].rearrange("p (g d) -> p g d", d=64)),
                 reads=[B["pD"]], writes=[BV[i]])

            q_t, bq = qT[i % 3], BqT[i % 3]
            for half in range(2):
                k.step()
                for hh in range(8):
                    k.op("pe", lambda h, half=half, hh=hh: h.transpose(out=pE[:, hh, :], in_=qkn[:, half * 8 + hh, :], identity=ident[:]),
                         reads=[B["qkn"], B["ident"]], writes=[B["pE"]])
                k.op("act", lambda h, half=half: h.activation(out=q_t[:, half * 8:(half + 1) * 8, :], in_=pE[:], func=AF.Copy,
                                                               scale=gq_col[:, 0:1]),
                     reads=[B["pE"], B["gq_col"]], writes=[bq])
            k.step()
            for g in range(4):
                k.op("pe", lambda h, g=g: h.transpose(out=pE[:, g, :], in_=qkn[:, 16 + g, :], identity=ident[:]),
                     reads=[B["qkn"], B["ident"]], writes=[B["pE"]])
            k.op("dve", lambda h: h.tensor_scalar(out=kT_all[:, :, i * 128:(i + 1) * 128], in0=pE[:, 0:4, :],
                                                  scalar1=gk_col[:, 0:1], scalar2=None, op0=ALU.mult),
                 reads=[B["pE"], B["gk_col"]], writes=[BkT[i]])

        cnt = [0]

        def phase2(i):
            q_t, bq = qT[i % 3], BqT[i % 3]
            blocks = [j for j in (i - 1, i, i + 1) if 0 <= j < NT]
            items = [(g, bi, j) for g in range(4) for bi, j in enumerate(blocks)]
            slots = []

            def emit_S(n):
                g, bi, j = items[n]
                c2 = cnt[0] % 2
                cnt[0] += 1
                slots.append(c2)
                k.op("pe", lambda h: h.matmul(
                    out=pGs[c2][:], lhsT=kT_all[:, g, j * 128:(j + 1) * 128],
                    rhs=q_t[:, 4 * g:4 * g + 4, :].rearrange("p h q -> p (h q)"), start=True, stop=True),
                    reads=[BkT[j], bq], writes=[BpG[c2]])
                k.op("dve", lambda h: h.tensor_tensor(
                    out=tS[c2][:], in0=pGs[c2][:], in1=BM[:, j - i + 1, 4 * g:4 * g + 4, :].rearrange("p h q -> p (h q)"), op=ALU.add),
                    reads=[BpG[c2], B["BM"]], writes=[BtS[c2]])
                k.op("act", lambda h: h.activation(out=PT[c2][:], in_=tS[c2][:], func=AF.Exp),
                     reads=[BtS[c2]], writes=[BPT[c2]])

            def emit_O(n):
                g, bi, j = items[n]
                c2 = slots[n]
                for hh in range(4):
                    k.op("pe", lambda h, hh=hh: h.matmul(
                        out=pH[:, hh, :], lhsT=PT[c2][:, hh * 128:(hh + 1) * 128], rhs=V_all[:, j, g, :],
                        start=(bi == 0 and hh == 0), stop=(bi == len(blocks) - 1 and hh == 3)),
                        reads=[BPT[c2], BV[j], B["V1"]], writes=[B["pH"]])
                if bi == len(blocks) - 1:
                    k.op("dve", lambda h: h.tensor_tensor(out=den[:], in0=pH[:, :, 64], in1=esink[:, 4 * g:4 * g + 4], op=ALU.add),
                         reads=[B["pH"], B["esink"]], writes=[B["den"]])
                    k.op("dve", lambda h: h.reciprocal(out=rden[:], in_=den[:]), reads=[B["den"]], writes=[B["rden"]])
                    k.op("dve", lambda h: h.tensor_tensor(
                        out=ot[:, 4 * g:4 * g + 4, :], in0=pH[:, :, 0:64],
                        in1=rden[:, :].unsqueeze(2).to_broadcast([128, 4, 64]), op=ALU.mult),
                        reads=[B["pH"], B["rden"]], writes=[B["ot"]])

            emit_S(0)
            for n in range(len(items)):
                k.step()
                if n + 1 < len(items):
                    emit_S(n + 1)
                emit_O(n)
            otf = ot[:].rearrange("p h d -> p (h d)")
            k.step()
            for kc in range(KC):
                k.op("pe", lambda h, kc=kc: h.transpose(out=pA[:, kc, :], in_=otf[:, kc * 128:(kc + 1) * 128], identity=ident[:]),
                     reads=[B["ot"], B["ident"]], writes=[B["pA"]])
            k.op("act", lambda h: h.copy(out=otT[:], in_=pA[:]), reads=[B["pA"]], writes=[B["otT"]])
            x_t, bx = xt[i % 3], Bxt[i % 3]
            xo, bxo = x1t[i % 2], Bx1[i % 2]
            for c, (pp, bn) in enumerate([(pB, "pB"), (pC, "pC")]):
                k.step()
                for kc in range(KC):
                    k.op("pe", lambda h, kc=kc, c=c, pp=pp: h.matmul(
                        out=pp[:], lhsT=otT[:, kc, :], rhs=w_out[:, kc, c * 512:(c + 1) * 512],
                        start=(kc == 0), stop=(kc == KC - 1)),
                        reads=[B["otT"], B["w_out"]], writes=[B[bn]])
                k.op("dve", lambda h, c=c, pp=pp: h.tensor_tensor(out=xo[:, c * 512:(c + 1) * 512], in0=pp[:],
                                                                  in1=x_t[:, c * 512:(c + 1) * 512], op=ALU.add),
                     reads=[B[bn], bx], writes=[bxo])
            k.dma("sp", lambda h: h.dma_start(out=XOUT[i * 128:(i + 1) * 128, :], in_=xo[:]), reads=[bxo], writes=[Bxout])

        for i in range(NT + 2):
            sa, sb_ = [], []
            if i < NT:
                k.begin_record()
                phase1(i)
                sa = k.end_record()
            if i >= 2:
                k.begin_record()
                phase2(i - 2)
                sb_ = k.end_record()
            k.replay_merged(sa, sb_)
        allb = []
        return allb


NE = 16
CAP = 512
N_BISECT = 32


def stage_moe(cx, XIN, XOUT, H, Bxin, Bxout, BH, P, layer):
    nc, k = cx.nc, cx.k
    W1, W3, W2 = P["expert_w1"], P["expert_w3"], P["expert_w2"]
    with ExitStack() as st0:
        sb0 = lambda shape, dt, name=None: cx.sb(st0, shape, dt, name)
        AFF = sb0([128, NE, NT], F32, "AFF")
        selm = sb0([128, NE, NT], F32, "selm")
        pos = sb0([128, NE, NT], F32, "pos")
        RH = sb0([128, NE, NT, 4], BF16, "RH")
        iota = sb0([128, CAP], F32, "iota")
        identb = sb0([128, 128], BF16, "identb")
        BA, Bsel, Bpos, BRH, Biota, Bidb = Buf("AFF"), Buf("selm"), Buf("pos"), Buf("RH"), Buf("iota"), Buf("identb")
        k.dma("sp", lambda h: h.dma_start(out=iota[:], in_=P["iota512"]), writes=[Biota])
        k.dma("pool", lambda h: h.dma_start(out=identb[:], in_=P["ident"]), writes=[Bidb])
        k.dma("pool", lambda h: h.dma_start(out=RH[:].rearrange("p e i c -> p (e i c)"), in_=P["rh_const"]), writes=[BRH])

        NCH = 4
        w1c = [sb0([128, KC, 512], BF16, "w1c%d" % c) for c in range(NCH)]
        w3c = [sb0([128, KC, 512], BF16, "w3c%d" % c) for c in range(NCH)]
        w2c = [sb0([128, 4, D], BF16, "w2c%d" % c) for c in range(NCH)]
        Bw1 = [Buf() for _ in range(NCH)]
        Bw3 = [Buf() for _ in range(NCH)]
        Bw2 = [Buf() for _ in range(NCH)]

        def load_chunk(src_ap, dst, bdst):
            k.dma("pool", lambda h: h.dma_start(out=dst[:], in_=src_ap), writes=[bdst])

        def load_w13(e, c):
            load_chunk(W1[layer, e].rearrange("(kc p) f -> p kc f", p=128)[:, :, c * 512:(c + 1) * 512], w1c[c], Bw1[c])
            load_chunk(W3[layer, e].rearrange("(kc p) f -> p kc f", p=128)[:, :, c * 512:(c + 1) * 512], w3c[c], Bw3[c])

        def load_w2(e, c):
            load_chunk(W2[layer, e].rearrange("(fc p) d -> p fc d", p=128)[:, c * 4:(c + 1) * 4, :], w2c[c], Bw2[c])

        for c in range(NCH):
            load_w13(0, c)
        for c in range(NCH):
            load_w2(0, c)

        with ExitStack() as st:
            sb = lambda shape, dt, name=None: cx.sb(st, shape, dt, name)
            ps = lambda shape, dt, name=None: cx.ps(st, shape, dt, name)
            g_bc = sb([128, D], F32, "g_bc")
            wr = sb([128, KC, NE], F32, "wr")
            identf = sb([128, 128], F32, "identf")
            onesf = sb([128, 128], F32, "onesf")
            ustr = sb([128, 128], F32, "ustr")
            eps_t = sb([128, 1], F32, "eps")
            ones32 = sb([128, NT], F32, "ones32")
            xt = [sb([128, D], F32, "xt%d" % i) for i in range(2)]
            hn = [sb([128, D], F32, "hn%d" % i) for i in range(2)]
            hb = [sb([128, D], BF16, "hb%d" % i) for i in range(2)]
            hnT = sb([128, KC, 128], F32, "hnT")
            junk = sb([128, D], BF16, "junk")
            ss = sb([128, 1], F32, "ss")
            rstd = sb([128, 1], F32, "rstd")
            mx = sb([128, 1], F32, "mx")
            ex = sb([128, NE], F32, "ex")
            sm = sb([128, 1], F32, "sm")
            lo = sb([128, NE], F32, "lo")
            hi = sb([128, NE], F32, "hi")
            mid = sb([128, NE], F32, "mid")
            cmp_t = sb([128, NE, NT], F32, "cmp")
            cntp = sb([128, NE], F32, "cntp")
            mge = sb([128, NE], mybir.dt.uint32, "mge")
            mlt = sb([128, NE], mybir.dt.uint32, "mlt")
            incl = sb([128, NE, NT], F32, "incl")
            ahi = sb([128, NE, NT], BF16, "ahi")
            pR0 = ps([128, 4, 128], F32, "pR0")
            pR1 = ps([128, 4, 128], F32, "pR1")
            pL_full = ps([128, 512], F32, "pL")
            pCn_full = ps([128, 512], F32, "pCn")
            pL = pL_full[:, 0:NE]
            pCn = pCn_full[:, 0:NE]
            pP = ps([128, NE * NT], F32, "pP")
            B = {n: Buf(n) for n in ["g_bc", "wr", "identf", "onesf", "ustr", "eps", "ones32", "hnT", "junk", "ss", "rstd",
                                     "mx", "ex", "sm", "lo", "hi", "mid", "cmp", "cntp", "mge", "mlt", "incl", "ahi",
                                     "pR0", "pR1", "pL", "pCn", "pP"]}
            Bxt = [Buf() for _ in range(2)]
            Bhn = [Buf() for _ in range(2)]
            Bhb = [Buf() for _ in range(2)]
            k.dma("sp", lambda h: h.dma_start(out=g_bc[:], in_=P["ffn_norm_g"][layer:layer + 1, :].to_broadcast([128, D])), writes=[B["g_bc"]])
            k.dma("sp", lambda h: h.dma_start(out=wr[:], in_=P["router_w"][layer].rearrange("(kc p) e -> p kc e", p=128)), writes=[B["wr"]])
            k.dma("sp", lambda h: h.dma_start(out=identf[:], in_=P["ident"]), writes=[B["identf"]])
            k.dma("sp", lambda h: h.dma_start(out=ustr[:], in_=P["ustrict"]), writes=[B["ustr"]])
            k.op("dve", lambda h: h.memset(onesf[:], 1.0), writes=[B["onesf"]])
            k.op("dve", lambda h: h.memset(ones32[:], 1.0), writes=[B["ones32"]])
            k.op("dve", lambda h: h.memset(eps_t[:], EPS), writes=[B["eps"]])
            def pre_tile(i):
                s2 = i % 2
                x_t, bx = xt[s2], Bxt[s2]
                k.dma("sp", lambda h, x_t=x_t: h.dma_start(out=x_t[:], in_=XIN[i * 128:(i + 1) * 128, :]), reads=[Bxin], writes=[bx])
                k.dma("sp", lambda h, x_t=x_t: h.dma_start(out=XOUT[i * 128:(i + 1) * 128, :], in_=x_t[:]), reads=[bx], writes=[Bxout])
                k.op("act", lambda h, x_t=x_t: h.activation(out=junk[:], in_=x_t[:], func=AF.Square, accum_out=ss[:]),
                     reads=[bx], writes=[B["junk"], B["ss"]])
                k.op("act", lambda h: h.activation(out=rstd[:], in_=ss[:], func=AF.Ln, bias=eps_t[:], scale=1.0 / D),
                     reads=[B["ss"], B["eps"]], writes=[B["rstd"]])
                k.op("act", lambda h: h.activation(out=rstd[:], in_=rstd[:], func=AF.Exp, scale=-0.5), reads=[B["rstd"]], writes=[B["rstd"]])
                k.op("dve", lambda h, x_t=x_t, s2=s2: h.scalar_tensor_tensor(out=hn[s2][:], in0=x_t[:], scalar=rstd[:, 0:1], in1=g_bc[:],
                                                                         op0=ALU.mult, op1=ALU.mult),
                     reads=[bx, B["rstd"], B["g_bc"]], writes=[Bhn[s2]])
                k.op("pool", lambda h, s2=s2: h.tensor_copy(out=hb[s2][:], in_=hn[s2][:]), reads=[Bhn[s2]], writes=[Bhb[s2]])
                k.dma("sp", lambda h, s2=s2: h.dma_start(out=H[i * 128:(i + 1) * 128, :], in_=hb[s2][:]), reads=[Bhb[s2]], writes=[BH])

            def pre_tile_back(i):
                s2 = i % 2
                for kc in range(KC):
                    pp, bn = (pR0, "pR0") if kc < 4 else (pR1, "pR1")
                    k.op("pe", lambda h, kc=kc, pp=pp, s2=s2: h.transpose(out=pp[:, kc % 4, :], in_=hn[s2][:, kc * 128:(kc + 1) * 128], identity=identf[:]),
                         reads=[Bhn[s2], B["identf"]], writes=[B[bn]])
                k.op("act", lambda h: h.copy(out=hnT[:, 0:4, :], in_=pR0[:]), reads=[B["pR0"]], writes=[B["hnT"]])
                k.op("dve", lambda h: h.tensor_copy(out=hnT[:, 4:8, :], in_=pR1[:]), reads=[B["pR1"]], writes=[B["hnT"]])
                for kc in range(KC):
                    k.op("pe", lambda h, kc=kc: h.matmul(out=pL[:], lhsT=hnT[:, kc, :], rhs=wr[:, kc, :], start=(kc == 0), stop=(kc == KC - 1)),
                         reads=[B["hnT"], B["wr"]], writes=[B["pL"]])
                k.op("dve", lambda h: h.tensor_reduce(out=mx[:], in_=pL[:], axis=AX.X, op=ALU.max, negate=True),
                     reads=[B["pL"]], writes=[B["mx"]])
                k.op("act", lambda h: h.activation(out=ex[:], in_=pL[:], func=AF.Exp, bias=mx[:], scale=1.0, accum_out=sm[:]),
                     reads=[B["pL"], B["mx"]], writes=[B["ex"], B["sm"]])
                k.op("dve", lambda h: h.reciprocal(out=sm[:], in_=sm[:]), reads=[B["sm"]], writes=[B["sm"]])
                k.op("dve", lambda h, i=i: h.tensor_scalar(out=AFF[:, :, i], in0=ex[:], scalar1=sm[:, 0:1], scalar2=None, op0=ALU.mult),
                     reads=[B["ex"], B["sm"]], writes=[BA])
            pre_tile(0)
            for i in range(NT):
                if i + 1 < NT:
                    pre_tile(i + 1)
                pre_tile_back(i)
            k.op("dve", lambda h: h.memset(lo[:], 0.0), writes=[B["lo"]])
            k.op("dve", lambda h: h.memset(hi[:], 1.0), writes=[B["hi"]])
            for it in range(N_BISECT):
                wdt = 2.0 ** -(it + 1)
                k.op("dve", lambda h, wdt=wdt: h.tensor_scalar(out=mid[:], in0=lo[:], scalar1=wdt, scalar2=None, op0=ALU.add),
                     reads=[B["lo"]], writes=[B["mid"]])
                k.op("dve", lambda h: h.tensor_tensor(out=cmp_t[:], in0=AFF[:], in1=mid[:, :].unsqueeze(2).to_broadcast([128, NE, NT]), op=ALU.is_ge),
                     reads=[BA, B["mid"]], writes=[B["cmp"]])
                k.op("dve", lambda h: h.tensor_reduce(out=cntp[:], in_=cmp_t[:], axis=AX.X, op=ALU.add), reads=[B["cmp"]], writes=[B["cntp"]])
                k.op("pe", lambda h: h.matmul(out=pCn[:], lhsT=onesf[:], rhs=cntp[:], start=True, stop=True),
                     reads=[B["onesf"], B["cntp"]], writes=[B["pCn"]])
                k.op("dve", lambda h, wdt=wdt: h.tensor_scalar(out=hi[:], in0=pCn[:], scalar1=CAP - 0.5, scalar2=wdt, op0=ALU.is_ge, op1=ALU.mult),
                     reads=[B["pCn"]], writes=[B["hi"]])
                k.op("dve", lambda h: h.tensor_tensor(out=lo[:], in0=lo[:], in1=hi[:], op=ALU.add), reads=[B["lo"], B["hi"]], writes=[B["lo"]])
            k.op("dve", lambda h: h.tensor_tensor(out=selm[:], in0=AFF[:], in1=lo[:, :].unsqueeze(2).to_broadcast([128, NE, NT]), op=ALU.is_ge),
                 reads=[BA, B["lo"]], writes=[Bsel])
            for e in range(NE):
                k.op("dve", lambda h, e=e: h.tensor_tensor_scan(out=incl[:, e, :], data0=ones32[:], data1=selm[:, e, :], initial=0.0,
                                                              op0=ALU.mult, op1=ALU.add),
                     reads=[Bsel, B["ones32"]], writes=[B["incl"]])
            k.op("dve", lambda h: h.tensor_tensor(out=incl[:], in0=incl[:], in1=selm[:], op=ALU.subtract), reads=[B["incl"], Bsel], writes=[B["incl"]])
            k.op("pe", lambda h: h.matmul(out=pP[:], lhsT=ustr[:], rhs=selm[:].rearrange("p e i -> p (e i)"), start=True, stop=False),
                 reads=[B["ustr"], Bsel], writes=[B["pP"]])
            k.op("pe", lambda h: h.matmul(out=pP[:], lhsT=onesf[:], rhs=incl[:].rearrange("p e i -> p (e i)"), start=False, stop=True),
                 reads=[B["onesf"], B["incl"]], writes=[B["pP"]])
            k.op("act", lambda h: h.copy(out=pos[:].rearrange("p e i -> p (e i)"), in_=pP[:]), reads=[B["pP"]], writes=[Bpos])
            k.op("dve", lambda h: h.tensor_copy(out=ahi[:], in_=AFF[:]), reads=[BA], writes=[B["ahi"]])
            k.op("dve", lambda h: h.tensor_copy(out=RH[:, :, :, 2], in_=ahi[:]), reads=[B["ahi"]], writes=[BRH])
            k.op("dve", lambda h: h.tensor_tensor(out=RH[:, :, :, 3], in0=AFF[:], in1=ahi[:], op=ALU.subtract), reads=[BA, B["ahi"]], writes=[BRH])
            if "dbg_aff" in P and layer == 0:
                bd = Buf()
                k.dma("sp", lambda h: h.dma_start(out=P["dbg_aff"], in_=AFF[:].rearrange("p e i -> p (e i)")), reads=[BA], writes=[bd])
                k.dma("sp", lambda h: h.dma_start(out=P["dbg_sel"], in_=selm[:].rearrange("p e i -> p (e i)")), reads=[Bsel], writes=[bd])
                k.dma("sp", lambda h: h.dma_start(out=P["dbg_pos"], in_=pos[:].rearrange("p e i -> p (e i)")), reads=[Bpos], writes=[bd])
                k.dma("sp", lambda h: h.dma_start(out=P["dbg_lo"], in_=lo[:]), reads=[B["lo"]], writes=[bd])
                k.dma("sp", lambda h: h.dma_start(out=P["dbg_hi"], in_=hi[:]), reads=[B["hi"]], writes=[bd])
                P["_dbg_bufs"].append(bd)
        k.barrier()

        with ExitStack() as st:
            sb = lambda shape, dt, name=None: cx.sb(st, shape, dt, name)
            ps = lambda shape, dt, name=None: cx.ps(st, shape, dt, name)
            Pm = [sb([128, CAP], BF16, "Pm%d" % c) for c in range(4)]
            BPm = [Buf() for _ in range(4)]
            idxf = sb([128, 4], F32, "idxf")
            pIs = sb([128, 4, 4], F32, "pIs")
            BpIs = Buf()
            idxi = [sb([128, 4], I32, "idxi%d" % c) for c in range(2)]
            gt = [sb([128, 4], F32, "gt%d" % c) for c in range(2)]
            Bidxf = Buf()
            Bidx = [Buf() for _ in range(2)]
            Bgt = [Buf() for _ in range(2)]
            xs = [sb([128, D], BF16, "xs%d" % c) for c in range(4)]
            Bxs = [Buf() for _ in range(4)]
            xsT = [sb([128, KC, CAP], BF16, "xsT%d" % c) for c in range(2)]
            BxsT = [Buf() for _ in range(2)]
            actT = sb([128, 16, CAP], BF16, "actT")
            BactT = [Buf() for _ in range(16)]
            sa = [sb([128, CAP], F32, "sa%d" % c) for c in range(2)]
            Bsa = [Buf() for _ in range(2)]
            yt = [sb([128, D], F32, "yt%d" % c) for c in range(2)]
            Byt = [Buf() for _ in range(2)]
            pT = ps([128, KC, 128], BF16, "pT")
            pI = ps([128, 4, 4], F32, "pI")
            pa = [ps([128, CAP], F32, "pa%d" % c) for c in range(2)]
            pu = [ps([128, CAP], F32, "pu%d" % c) for c in range(2)]
            py = [ps([128, 512], F32, "py%d" % c) for c in range(2)]
            BpT, BpI = Buf(), Buf()
            Bpa = [Buf() for _ in range(2)]
            Bpu = [Buf() for _ in range(2)]
            Bpy = [Buf() for _ in range(2)]
            cnt = {"stg": 0, "cast": 0, "y": 0}
            cast_engs = ["act", "dve", "act", "pool"]

            def build_steps(e):
                par = e % 2
                stepsA, stepsB = [], []

                def emit_pm(i):
                    pm, bpm = Pm[i % 4], BPm[i % 4]
                    k.op("dve", lambda h: h.tensor_scalar(out=pm[:], in0=iota[:], scalar1=pos[:, e, i:i + 1], scalar2=selm[:, e, i:i + 1],
                                                          op0=ALU.is_equal, op1=ALU.mult),
                         reads=[Biota, Bpos, Bsel], writes=[bpm])

                def step_i(i):
                    def f():
                        if i == 0:
                            emit_pm(0)
                            emit_pm(1)
                        if i + 2 < NT:
                            emit_pm(i + 2)
                        pm, bpm = Pm[i % 4], BPm[i % 4]
                        for c in range(4):
                            k.op("pe", lambda h, c=c: h.matmul(out=pI[:, c, :], lhsT=pm[:, c * 128:(c + 1) * 128], rhs=RH[:, e, i, :],
                                                               start=(i == 0 and c == 0), stop=(i == NT - 1 and c == 3)),
                                 reads=[bpm, BRH], writes=[BpI])
                    return f

                for i in range(NT):
                    stepsA.append(step_i(i))

                def fin_a():
                    k.op("dve", lambda h: h.tensor_copy(out=pIs[:], in_=pI[:]), reads=[BpI], writes=[BpIs])
                    k.op("dve", lambda h: h.scalar_tensor_tensor(out=idxf[:], in0=pIs[:, :, 1], scalar=128.0, in1=pIs[:, :, 0],
                                                                 op0=ALU.mult, op1=ALU.add),
                         reads=[BpIs], writes=[Bidxf])
                    k.op("dve", lambda h: h.tensor_copy(out=idxi[par][:], in_=idxf[:]), reads=[Bidxf], writes=[Bidx[par]])
                    if "dbg_idx" in P and layer == 0:
                        bd = Buf()
                        k.dma("sp", lambda h: h.dma_start(out=P["dbg_idx"][:, e * 4:(e + 1) * 4], in_=idxf[:]), reads=[Bidxf], writes=[bd])
                        k.dma("sp", lambda h: h.dma_start(out=P["dbg_pis"][:, e * 16:(e + 1) * 16], in_=pIs[:].rearrange("p a b -> p (a b)")), reads=[BpIs], writes=[bd])
                        P["_dbg_bufs"].append(bd)
                    k.op("dve", lambda h: h.tensor_tensor(out=gt[par][:], in0=pIs[:, :, 2], in1=pIs[:, :, 3], op=ALU.add),
                         reads=[BpIs], writes=[Bgt[par]])
                    for c in range(4):
                        k.dma("pool", lambda h, c=c: h.indirect_dma_start(
                            out=xs[c][:], out_offset=None, in_=H, in_offset=bass.IndirectOffsetOnAxis(ap=idxi[par][:, c:c + 1], axis=0)),
                            reads=[BH, Bidx[par]], writes=[Bxs[c]])
                stepsA.append(fin_a)

                def fin_b(c):
                    def f():
                        for kc in range(KC):
                            k.op("pe", lambda h, kc=kc: h.transpose(out=pT[:, kc, :], in_=xs[c][:, kc * 128:(kc + 1) * 128], identity=identb[:]),
                                 reads=[Bxs[c], Bidb], writes=[BpT])
                        k.op("act", lambda h: h.copy(out=xsT[par][:, :, c * 128:(c + 1) * 128], in_=pT[:]),
                             reads=[BpT], writes=[BxsT[par]])
                    return f
                for c in range(4):
                    stepsB.append(fin_b(c))
                return stepsA, stepsB

            def ffn(e, inter, interB):
                par = e % 2
                n_inter = len(inter)
                done = 0
                doneB = 0
                for fc in range(16):
                    c = fc // 4
                    s2 = fc % 2
                    for (wc, bw, pp, bp) in [(w1c[c], Bw1[c], pa[s2], Bpa[s2]), (w3c[c], Bw3[c], pu[s2], Bpu[s2])]:
                        for kc in range(KC):
                            k.op("pe", lambda h, kc=kc, wc=wc, pp=pp, fc=fc: h.matmul(
                                out=pp[:], lhsT=wc[:, kc, (fc % 4) * 128:(fc % 4 + 1) * 128], rhs=xsT[par][:, kc, :],
                                start=(kc == 0), stop=(kc == KC - 1)),
                                reads=[bw, BxsT[par]], writes=[bp])
                    k.op("act", lambda h, s2=s2: h.activation(out=sa[s2][:], in_=pa[s2][:], func=AF.Silu), reads=[Bpa[s2]], writes=[Bsa[s2]])
                    k.op("dve", lambda h, s2=s2, fc=fc: h.tensor_tensor(out=actT[:, fc, :], in0=sa[s2][:], in1=pu[s2][:], op=ALU.mult),
                         reads=[Bsa[s2], Bpu[s2]], writes=[BactT[fc]])
                    if fc % 4 == 3 and e + 1 < NE:
                        load_w13(e + 1, c)
                    target = (n_inter * (fc + 1)) // 16
                    while done < target:
                        inter[done]()
                        done += 1
                for c in range(4):
                    for dc in range(2):
                        q = cnt["y"] % 2
                        cnt["y"] += 1
                        for fc in range(16):
                            k.op("pe", lambda h, fc=fc, c=c, dc=dc, q=q: h.matmul(
                                out=py[q][:], lhsT=actT[:, fc, c * 128:(c + 1) * 128], rhs=w2c[fc // 4][:, fc % 4, dc * 512:(dc + 1) * 512],
                                start=(fc == 0), stop=(fc == 15)),
                                reads=[BactT[fc], Bw2[fc // 4]], writes=[Bpy[q]])
                        eng = "act" if dc == 0 else "dve"
                        if eng == "act":
                            k.op("act", lambda h, c=c, dc=dc, q=q: h.activation(out=yt[c % 2][:, dc * 512:(dc + 1) * 512], in_=py[q][:], func=AF.Copy,
                                                                                  scale=gt[par][:, c:c + 1]),
                                 reads=[Bpy[q], Bgt[par]], writes=[Byt[c % 2]])
                        else:
                            k.op("dve", lambda h, c=c, dc=dc, q=q: h.tensor_scalar(out=yt[c % 2][:, dc * 512:(dc + 1) * 512], in0=py[q][:],
                                                                                    scalar1=gt[par][:, c:c + 1], scalar2=None, op0=ALU.mult),
                                 reads=[Bpy[q], Bgt[par]], writes=[Byt[c % 2]])
                    if doneB < len(interB):
                        interB[doneB]()
                        doneB += 1
                    k.dma("pool", lambda h, c=c: h.indirect_dma_start(
                        out=XOUT, out_offset=bass.IndirectOffsetOnAxis(ap=idxi[par][:, c:c + 1], axis=0),
                        in_=yt[c % 2][:], in_offset=None, compute_op=ALU.add),
                        reads=[Byt[c % 2], Bidx[par]], writes=[Bxout])
                    if c == 3 and e + 1 < NE:
                        for cc in range(NCH):
                            load_w2(e + 1, cc)

            sA, sB = build_steps(0)
            for f in sA + sB:
                f()
            for e in range(NE):
                sA, sB = build_steps(e + 1) if e + 1 < NE else ([], [])
                ffn(e, sA, sB)
        k.barrier()


NH = 8
MIN = 3104
VW = 132


def stage_mlstm(cx, XIN, XOUT, Bxin, Bxout, P):
    nc, k = cx.nc, cx.k
    W = P["mlstm_w_in"]
    KS, VS, OGS = P["KS"], P["VS"], P["OGS"]
    HF = XOUT
    BKS, BVS, BOGS, BHF = Buf("KS"), Buf("VS"), Buf("OGS"), Buf("HF")
    with ExitStack() as st0:
        sb0 = lambda shape, dt, name=None: cx.sb(st0, shape, dt, name)
        QT = sb0([128, 4, S], BF16, "QT")
        KT = sb0([128, 4, S], BF16, "KT")
        G = sb0([128, NT, 32], F32, "G")
        identb = sb0([128, 128], BF16, "identb")
        identf = sb0([128, 128], F32, "identf")
        eps_t = sb0([128, 1], F32, "eps")
        one_t = sb0([128, 1], F32, "one")
        g_bc = sb0([128, D], F32, "g_bc")
        BQT = [Buf() for _ in range(NT)]
        BKT = [Buf() for _ in range(NT)]
        BG = [Buf() for _ in range(NT)]
        Bidb, Bidf, Beps, Bone, Bg = Buf(), Buf(), Buf(), Buf(), Buf()
        k.dma("pool", lambda h: h.dma_start(out=identb[:], in_=P["ident"]), writes=[Bidb])
        k.dma("sp", lambda h: h.dma_start(out=identf[:], in_=P["ident"]), writes=[Bidf])
        k.dma("sp", lambda h: h.dma_start(out=g_bc[:], in_=P["mlstm_norm_g"].to_broadcast([128, D])), writes=[Bg])
        k.op("dve", lambda h: h.memset(eps_t[:], EPS), writes=[Beps])
        k.op("dve", lambda h: h.memset(one_t[:], 1.0), writes=[Bone])

        with ExitStack() as st:
            sb = lambda shape, dt, name=None: cx.sb(st, shape, dt, name)
            ps = lambda shape, dt, name=None: cx.ps(st, shape, dt, name)
            w_in = sb([128, KC, MIN], BF16, "w_in")
            bgate = sb([128, 32], F32, "bgate")
            xt = [sb([128, D], F32, "xt%d" % i) for i in range(2)]
            junk = sb([128, D], BF16, "junk")
            xn = sb([128, D], BF16, "xn")
            xnT2 = [sb([128, KC, 128], BF16, "xnT%d" % i) for i in range(2)]
            BxnT2 = [Buf() for _ in range(2)]
            ss = sb([128, 1], F32, "ss")
            rstd = sb([128, 1], F32, "rstd")
            ktok = [sb([128, 512], BF16, "ktok%d" % i) for i in range(2)]
            vtile = [sb([128, NH, VW], BF16, "vt%d" % i) for i in range(2)]
            ogt = [sb([128, D], BF16, "ogt%d" % i) for i in range(2)]
            zt = sb([128, 32], F32, "zt")
            et = sb([128, 32], F32, "et")
            pA = ps([128, KC, 128], BF16, "pA")
            pQ = ps([128, 4, 128], F32, "pQ")
            pK = ps([128, 4, 128], F32, "pK")
            pTk = ps([128, 512], F32, "pTk")
            pV0 = ps([128, 512], F32, "pV0")
            pV1 = ps([128, 512], F32, "pV1")
            pGf = ps([128, 512], F32, "pG")
            pG = pGf[:, 0:32]
            B = {n: Buf(n) for n in ["w_in", "bgate", "junk", "xn", "xnT", "ss", "rstd", "zt", "et",
                                     "pA", "pQ", "pK", "pTk", "pV0", "pV1", "pG"]}
            Bxt = [Buf() for _ in range(2)]
            Bkt = [Buf() for _ in range(2)]
            Bvt = [Buf() for _ in range(2)]
            Bog = [Buf() for _ in range(2)]
            wv = W.rearrange("(kc p) n -> p kc n", p=128)
            for c0 in range(0, MIN, 776):
                k.dma("pool", lambda h, c0=c0: h.dma_start(out=w_in[:, :, c0:c0 + 776], in_=wv[:, :, c0:c0 + 776]), writes=[B["w_in"]])
            k.dma("sp", lambda h: h.dma_start(out=bgate[:], in_=P["mlstm_gate_bias"].to_broadcast([128, 32])), writes=[B["bgate"]])
            for i2 in range(2):
                k.op("pool", lambda h, i2=i2: h.memset(vtile[i2][:], 0.0), writes=[Bvt[i2]])
                k.op("pool", lambda h, i2=i2: h.memset(vtile[i2][:, :, 128:129], 1.0), writes=[Bvt[i2]])

            def p0_tile(i):
                s2 = i % 2
                x_t, bx = xt[s2], Bxt[s2]
                k.dma("sp", lambda h: h.dma_start(out=x_t[:], in_=XIN[i * 128:(i + 1) * 128, :]), reads=[Bxin], writes=[bx])
                k.op("act", lambda h: h.activation(out=junk[:], in_=x_t[:], func=AF.Square, accum_out=ss[:]),
                     reads=[bx], writes=[B["junk"], B["ss"]])
                k.op("act", lambda h: h.activation(out=rstd[:], in_=ss[:], func=AF.Ln, bias=eps_t[:], scale=1.0 / D),
                     reads=[B["ss"], Beps], writes=[B["rstd"]])
                k.op("act", lambda h: h.activation(out=rstd[:], in_=rstd[:], func=AF.Exp, scale=-0.5), reads=[B["rstd"]], writes=[B["rstd"]])
                k.op("dve", lambda h: h.scalar_tensor_tensor(out=xn[:], in0=x_t[:], scalar=rstd[:, 0:1], in1=g_bc[:],
                                                             op0=ALU.mult, op1=ALU.mult),
                     reads=[bx, B["rstd"], Bg], writes=[B["xn"]])
                for kc in range(KC):
                    k.op("pe", lambda h, kc=kc: h.transpose(out=pA[:, kc, :], in_=xn[:, kc * 128:(kc + 1) * 128], identity=identb[:]),
                         reads=[B["xn"], Bidb], writes=[B["pA"]])
                k.op("act", lambda h: h.copy(out=xnT2[s2][:], in_=pA[:]), reads=[B["pA"]], writes=[BxnT2[s2]])

            def p0_back(i):
                s2 = i % 2
                xnT = xnT2[s2]
                B["xnT"] = BxnT2[s2]
                for (pp, bn, col0) in [(pQ, "pQ", 0), (pK, "pK", 512)]:
                    for j in range(4):
                        for kc in range(KC):
                            k.op("pe", lambda h, kc=kc, j=j, pp=pp, col0=col0: h.matmul(
                                out=pp[:, j, :], lhsT=w_in[:, kc, col0 + j * 128:col0 + (j + 1) * 128], rhs=xnT[:, kc, :],
                                start=(kc == 0), stop=(kc == KC - 1)),
                                reads=[B["w_in"], B["xnT"]], writes=[B[bn]])
                k.op("act", lambda h: h.copy(out=QT[:, :, i * 128:(i + 1) * 128], in_=pQ[:]), reads=[B["pQ"]], writes=[BQT[i]])
                k.op("act", lambda h: h.activation(out=KT[:, :, i * 128:(i + 1) * 128], in_=pK[:], func=AF.Copy, scale=0.125),
                     reads=[B["pK"]], writes=[BKT[i]])
                for kc in range(KC):
                    k.op("pe", lambda h, kc=kc: h.matmul(out=pTk[:], lhsT=xnT[:, kc, :], rhs=w_in[:, kc, 512:1024],
                                                         start=(kc == 0), stop=(kc == KC - 1)),
                         reads=[B["w_in"], B["xnT"]], writes=[B["pTk"]])
                k.op("dve", lambda h: h.tensor_scalar(out=ktok[s2][:], in0=pTk[:], scalar1=0.125, scalar2=None, op0=ALU.mult),
                     reads=[B["pTk"]], writes=[Bkt[s2]])
                k.dma("sp", lambda h: h.dma_start(out=KS[i * 128:(i + 1) * 128, :], in_=ktok[s2][:]), reads=[Bkt[s2]], writes=[BKS])
                for half, (pp, bn) in enumerate([(pV0, "pV0"), (pV1, "pV1")]):
                    for kc in range(KC):
                        k.op("pe", lambda h, kc=kc, pp=pp, half=half: h.matmul(
                            out=pp[:], lhsT=xnT[:, kc, :], rhs=w_in[:, kc, 1024 + half * 512:1024 + (half + 1) * 512],
                            start=(kc == 0), stop=(kc == KC - 1)),
                            reads=[B["w_in"], B["xnT"]], writes=[B[bn]])
                    eng = "act" if half == 0 else "dve"
                    if eng == "act":
                        k.op("act", lambda h, pp=pp, half=half: h.copy(out=vtile[s2][:, half * 4:(half + 1) * 4, 0:128],
                                                                       in_=pp[:].rearrange("p (h d) -> p h d", d=128)),
                             reads=[B[bn]], writes=[Bvt[s2]])
                    else:
                        k.op("dve", lambda h, pp=pp, half=half: h.tensor_copy(out=vtile[s2][:, half * 4:(half + 1) * 4, 0:128],
                                                                              in_=pp[:].rearrange("p (h d) -> p h d", d=128)),
                             reads=[B[bn]], writes=[Bvt[s2]])
                k.dma("sp", lambda h: h.dma_start(out=VS[i * 128:(i + 1) * 128, :, :], in_=vtile[s2][:]), reads=[Bvt[s2]], writes=[BVS])
                for half, (pp, bn) in enumerate([(pV0, "pV0"), (pV1, "pV1")]):
                    for kc in range(KC):
                        k.op("pe", lambda h, kc=kc, pp=pp, half=half: h.matmul(
                            out=pp[:], lhsT=xnT[:, kc, :], rhs=w_in[:, kc, 2048 + half * 512:2048 + (half + 1) * 512],
                            start=(kc == 0), stop=(kc == KC - 1)),
                            reads=[B["w_in"], B["xnT"]], writes=[B[bn]])
                    k.op("act", lambda h, pp=pp, half=half: h.activation(out=ogt[s2][:, half * 512:(half + 1) * 512], in_=pp[:], func=AF.Sigmoid),
                         reads=[B[bn]], writes=[Bog[s2]])
                k.dma("sp", lambda h: h.dma_start(out=OGS[i * 128:(i + 1) * 128, :], in_=ogt[s2][:]), reads=[Bog[s2]], writes=[BOGS])
                for kc in range(KC):
                    k.op("pe", lambda h, kc=kc: h.matmul(out=pG, lhsT=xnT[:, kc, :], rhs=w_in[:, kc, 3072:3104],
                                                         start=(kc == 0), stop=(kc == KC - 1)),
                         reads=[B["w_in"], B["xnT"]], writes=[B["pG"]])
                k.op("dve", lambda h: h.tensor_tensor(out=zt[:], in0=pG, in1=bgate[:], op=ALU.add), reads=[B["pG"], B["bgate"]], writes=[B["zt"]])
                k.op("act", lambda h: h.activation(out=et[:], in_=zt[:], func=AF.Exp, scale=-1.0), reads=[B["zt"]], writes=[B["et"]])
                k.op("act", lambda h: h.activation(out=et[:], in_=et[:], func=AF.Ln, bias=one_t[:], scale=1.0), reads=[B["et"], Bone], writes=[B["et"]])
                zv = zt[:].rearrange("p (a b) -> p a b", b=8)
                ev = et[:].rearrange("p (a b) -> p a b", b=8)
                gv = G[:, i, :].rearrange("p (a b) -> p a b", b=8)
                k.op("dve", lambda h: h.tensor_copy(out=gv[:, 0:4:2, :], in_=zv[:, 0:4:2, :]), reads=[B["zt"]], writes=[BG[i]])
                k.op("dve", lambda h: h.tensor_scalar(out=gv[:, 1:4:2, :], in0=ev[:, 1:4:2, :], scalar1=-1.0, scalar2=None, op0=ALU.mult),
                     reads=[B["et"]], writes=[BG[i]])

            p0_tile(0)
            for i in range(NT):
                if i + 1 < NT:
                    p0_tile(i + 1)
                p0_back(i)
        k.barrier()
        if ML_STOP == "p0":
            return

        with ExitStack() as st:
            sb = lambda shape, dt, name=None: cx.sb(st, shape, dt, name)
            ps = lambda shape, dt, name=None: cx.ps(st, shape, dt, name)
            HB = P["HB"]
            BHB = Buf("HB")
            tri = [sb([128, 128], F32, "tri%d" % d_) for d_ in range(2)]
            negm = [sb([128, 128], F32, "negm%d" % d_) for d_ in range(2)]
            ones_c = sb([128, 1], BF16, "ones_c")
            w_out = sb([128, KC, D], BF16, "w_out")
            og_bc = sb([128, D], F32, "og_bc")
            tmpn = sb([128, 4, 128], F32, "tmpn")
            dsm = sb([128, 16], F32, "dsm")
            dtm = sb([128, NH], F32, "dtm")
            den = sb([128, NH], F32, "den")
            kw = sb([128, NH, 64], BF16, "kw")
            ssh = sb([128, NH], F32, "ssh")
            yb = sb([128, D], BF16, "yb")
            ybT = sb([128, KC, 128], BF16, "ybT")
            xo = sb([128, D], F32, "xo")
            def per_dir(shape, dt, name):
                return [sb(shape, dt, "%s_%d" % (name, d_)) for d_ in range(2)]
            Cst_d = per_dir([128, NH, VW], F32, "Cst")
            Cbf_d = per_dir([128, NH, VW], BF16, "Cbf")
            LFB_d = per_dir([128, NH, 128], F32, "LFB")
            bias_d = per_dir([128, NH], F32, "bias_s")
            eb_d = per_dir([128, NH], F32, "eb")
            ebL_d = per_dir([128, NH], F32, "ebL")
            DT_d = per_dir([128, NH, 128], F32, "DT")
            SwT_d = per_dir([128, NH, 128], BF16, "SwT")
            hd_d = [[sb([128, NH, 128], F32, "hdir%d%d" % (d_, i)) for i in range(2)] for d_ in range(2)]
            ktl_d = [[sb([128, 512], BF16, "ktl%d%d" % (d_, i)) for i in range(2)] for d_ in range(2)]
            vtl_d = [[sb([128, NH, VW], BF16, "vtl%d%d" % (d_, i)) for i in range(2)] for d_ in range(2)]
            hfl_s = sb([128, D], F32, "hfl")
            ogl_s = sb([128, D], BF16, "ogl")
            xl_s = sb([128, D], F32, "xl")
            ktm_d = [[sb([128, 4, 128], BF16, "ktm%d%d" % (d_, i)) for i in range(2)] for d_ in range(2)]
            pSs = [ps([128, 4, 128], F32, "pS%d" % i) for i in range(2)]
            pBMs = [ps([128, 4, 128], F32, "pBM%d" % i) for i in range(2)]
            pNi = ps([128, 4, 128], F32, "pNi")
            pNe = ps([128, 4, 128], F32, "pNe")
            pSmCu = ps([128, 512], F32, "pSmCu")
            pCu = pSmCu[:, 0:3 * VW].rearrange("p (a b) -> p a b", b=VW)
            pSm_d = [pSmCu[:, 400 + 24 * d_:424 + 24 * d_] for d_ in range(2)]
            pA = ps([128, KC, 128], BF16, "pA")
            pY = pNi[:].rearrange("p a b -> p (a b)")
            Bsh = {n: Buf(n) for n in ["tri", "negm", "ones_c", "w_out", "og_bc", "tmpn", "dsm", "dtm", "den", "kw", "ssh",
                                       "yb", "ybT", "xo", "pNi", "pNe", "pSm", "pA", "hfl", "ogl", "xl"]}
            Bsh["pCu"] = Bsh["pSm"]
            Bsh["pY"] = Bsh["pNi"]
            Bd = []
            for d_ in range(2):
                bb = dict(Bsh)
                for n in ["Cst", "Cbf", "LFB", "bias_s", "eb", "ktm", "pS", "pBM"]:
                    bb[n] = Buf(n + str(d_))
                bb["hd"] = [Buf(), Buf()]
                bb["Csth"] = [Buf() for _ in range(NH)]
                bb["DT"] = [Buf(), Buf()]
                bb["SwT"] = [Buf(), Buf()]
                bb["ebL"] = [Buf(), Buf()]
                bb["ktl"] = [Buf(), Buf()]
                bb["vtl"] = [Buf(), Buf()]
                Bd.append(bb)
            k.dma("sp", lambda h: h.dma_start(out=tri[0][:], in_=P["tri_f"]), writes=[Bsh["tri"]])
            k.dma("sp", lambda h: h.dma_start(out=tri[1][:], in_=P["tri_b"]), writes=[Bsh["tri"]])
            k.dma("sp", lambda h: h.dma_start(out=negm[0][:], in_=P["negm_f"]), writes=[Bsh["negm"]])
            k.dma("sp", lambda h: h.dma_start(out=negm[1][:], in_=P["negm_b"]), writes=[Bsh["negm"]])
            k.dma("pool", lambda h: h.dma_start(out=w_out[:], in_=P["mlstm_w_out"].rearrange("(kc p) n -> p kc n", p=128)), writes=[Bsh["w_out"]])
            k.dma("sp", lambda h: h.dma_start(out=og_bc[:], in_=P["mlstm_out_norm_g"].to_broadcast([128, D])), writes=[Bsh["og_bc"]])
            k.op("pool", lambda h: h.memset(ones_c[:], 1.0), writes=[Bsh["ones_c"]])
            for d_ in range(2):
                k.op("pool", lambda h, d_=d_: h.memset(ktm_d[d_][0][:], 0.0), writes=[Bd[d_]["ktm"]])
                k.op("pool", lambda h, d_=d_: h.memset(ktm_d[d_][1][:], 0.0), writes=[Bd[d_]["ktm"]])
                k.op("dve", lambda h, d_=d_: h.memset(Cst_d[d_][:], 0.0), writes=Bd[d_]["Csth"])
                k.op("pool", lambda h, d_=d_: h.memset(Cbf_d[d_][:], 0.0), writes=[Bd[d_]["Cbf"]])

            def chunk(dr, c, step):
                B = Bd[dr]
                s2 = step % 2
                late = step >= NT // 2
                l_last = 127 if dr == 0 else 0
                Cst, Cbf, LFB, bias_s, eb, ebL = Cst_d[dr], Cbf_d[dr], LFB_d[dr], bias_d[dr], eb_d[dr], ebL_d[dr]
                DT, SwT, hd, ktm = DT_d[dr], SwT_d[dr], hd_d[dr][s2], ktm_d[dr]
                hfl, ogl, xl = hfl_s, ogl_s, xl_s
                pS, pBM = pSs[dr], pBMs[dr]
                pSm = pSm_d[dr]
                lf = G[:, c, 8 + 16 * dr:16 + 16 * dr]
                ig = G[:, c, 16 * dr:8 + 16 * dr]
                kt_, bkt = ktl_d[dr][s2], B["ktl"][s2]
                vt_, bvt = vtl_d[dr][s2], B["vtl"][s2]
                bhd = B["hd"][s2]
                csl = slice(c * 128, (c + 1) * 128)
                k.dma("sp", lambda h: h.dma_start(out=kt_[:], in_=KS[csl, :]), reads=[BKS], writes=[bkt])
                k.dma("sp", lambda h: h.dma_start(out=vt_[:], in_=VS[csl, :, :]), reads=[BVS], writes=[bvt])
                k.op("act", lambda h: h.copy(out=ktm[0][0:64, :, :], in_=KT[0:64, :, csl]), reads=[BKT[c]], writes=[B["ktm"]])
                k.op("pool", lambda h: h.tensor_copy(out=ktm[1][64:128, :, :], in_=KT[64:128, :, csl]), reads=[BKT[c]], writes=[B["ktm"]])
                k.op("pe", lambda h: h.matmul(out=pSm[:, 0:8], lhsT=tri[dr][:], rhs=lf, start=True, stop=True),
                     reads=[B["tri"], BG[c]], writes=[B["pSm"]])
                k.op("dve", lambda h: h.tensor_copy(out=LFB[:], in_=lf.unsqueeze(2).to_broadcast([128, NH, 128])), reads=[BG[c]], writes=[B["LFB"]])
                k.op("dve", lambda h: h.tensor_tensor(out=bias_s[:], in0=ig, in1=pSm[:, 0:8], op=ALU.subtract),
                     reads=[BG[c], B["pSm"]], writes=[B["bias_s"]])
                k.op("act", lambda h: h.activation(out=eb[:], in_=pSm[:, 0:8], func=AF.Exp), reads=[B["pSm"]], writes=[B["eb"]])

                def half_front(h0, hf):
                    k.step()
                    for hh in range(4):
                        k.op("pe", lambda h, hh=hh: h.matmul(out=pBM[:, hh, :], lhsT=LFB[:, h0 + hh, :], rhs=tri[dr][:],
                                                            start=(hh == 0), stop=False),
                             reads=[B["LFB"], B["tri"]], writes=[B["pBM"]])
                        k.op("pe", lambda h, hh=hh: h.matmul(out=pBM[:, hh, :], lhsT=identf[:], rhs=negm[dr][:],
                                                            start=False, stop=(hh == 3)),
                             reads=[Bidf, B["negm"]], writes=[B["pBM"]])
                    for hh in range(4):
                        hd_ = h0 + hh
                        j, r = hd_ // 2, hd_ % 2
                        k.op("pe", lambda h, hh=hh, j=j, r=r: h.matmul(
                            out=pS[:, hh, :], lhsT=ktm[r][:, j, :], rhs=QT[:, j, csl],
                            start=(hh == 0), stop=(hh == 3)),
                            reads=[B["ktm"], BQT[c]], writes=[B["pS"]])
                    for hh in range(4):
                        k.op("act", lambda h, hh=hh: h.activation(out=DT[:, h0 + hh, :], in_=pBM[:, hh, :], func=AF.Exp,
                                                                  bias=bias_s[:, h0 + hh:h0 + hh + 1], scale=1.0),
                             reads=[B["pBM"], B["bias_s"]], writes=[B["DT"][hf]])
                    k.op("act", lambda h: h.activation(out=ebL[:, h0:h0 + 4], in_=pBM[:, :, l_last], func=AF.Exp),
                         reads=[B["pBM"]], writes=[B["ebL"][hf]])
                    k.op("dve", lambda h: h.tensor_tensor(out=SwT[:, h0:h0 + 4, :], in0=pS[:], in1=DT[:, h0:h0 + 4, :], op=ALU.mult),
                         reads=[B["pS"], B["DT"][hf]], writes=[B["SwT"][hf]])

                def half_back(h0, hf):
                    k.step()
                    for hh in range(4):
                        hd_ = h0 + hh
                        j, r = hd_ // 2, hd_ % 2
                        k.op("pe", lambda h, hh=hh, hd_=hd_: h.matmul(out=pNi[:, hh, :], lhsT=SwT[:, hd_, :], rhs=vt_[:, hd_, 0:128],
                                                                     start=(hh == 0), stop=(hh == 3)),
                             reads=[B["SwT"][hf], bvt], writes=[B["pNi"]])
                        k.op("pe", lambda h, hh=hh, hd_=hd_, j=j, r=r: h.matmul(
                            out=pNe[:, hh, :], lhsT=QT[:, j, csl], rhs=Cbf[:, hd_, 0:128],
                            start=(hh == 0), stop=(hh == 3)),
                            reads=[BQT[c], B["Cbf"]], writes=[B["pNe"]])
                        k.op("pe", lambda h, hh=hh, hd_=hd_: h.matmul(out=pSm[:, 8 + hd_:9 + hd_], lhsT=SwT[:, hd_, :], rhs=ones_c[:],
                                                                     start=True, stop=True),
                             reads=[B["SwT"][hf], B["ones_c"]], writes=[B["pSm"]])
                        k.op("pe", lambda h, hh=hh, hd_=hd_, j=j, r=r: h.matmul(
                            out=pSm[:, 16 + hd_:17 + hd_], lhsT=QT[:, j, csl], rhs=Cbf[:, hd_, 128:129],
                            start=True, stop=True),
                            reads=[BQT[c], B["Cbf"]], writes=[B["pSm"]])
                    ebh = eb[:, h0:h0 + 4]
                    k.op("dve", lambda h: h.tensor_tensor(out=tmpn[:], in0=pNe[:], in1=ebh.unsqueeze(2).to_broadcast([128, 4, 128]), op=ALU.mult),
                         reads=[B["pNe"], B["eb"]], writes=[B["tmpn"]])
                    k.op("dve", lambda h: h.tensor_tensor(out=hd[:, h0:h0 + 4, :], in0=pNi[:], in1=tmpn[:], op=ALU.add),
                         reads=[B["pNi"], B["tmpn"]], writes=[bhd])

                def finish_den():
                    k.op("dve", lambda h: h.tensor_copy(out=dsm[:], in_=pSm[:, 8:24]), reads=[B["pSm"]], writes=[B["dsm"]])
                    k.op("dve", lambda h: h.tensor_tensor(out=dtm[:], in0=dsm[:, 8:16], in1=eb[:], op=ALU.mult), reads=[B["dsm"], B["eb"]], writes=[B["dtm"]])
                    k.op("dve", lambda h: h.tensor_tensor(out=den[:], in0=dtm[:], in1=dsm[:, 0:8], op=ALU.add), reads=[B["dtm"], B["dsm"]], writes=[B["den"]])
                    k.op("dve", lambda h: h.scalar_tensor_tensor(out=dtm[:], in0=den[:], scalar=-1.0, in1=den[:], op0=ALU.mult, op1=ALU.max),
                         reads=[B["den"]], writes=[B["dtm"]])
                    k.op("dve", lambda h: h.tensor_scalar(out=den[:], in0=dtm[:], scalar1=1.0, scalar2=None, op0=ALU.max), reads=[B["dtm"]], writes=[B["den"]])
                    k.op("dve", lambda h: h.reciprocal(out=den[:], in_=den[:]), reads=[B["den"]], writes=[B["den"]])
                    k.op("dve", lambda h: h.tensor_tensor(out=hd[:], in0=hd[:], in1=den[:, :].unsqueeze(2).to_broadcast([128, NH, 128]), op=ALU.mult),
                         reads=[bhd, B["den"]], writes=[bhd])

                half_front(0, 0)
                half_back(0, 0)
                half_front(4, 1)
                half_back(4, 1)
                finish_den()
                k.step()
                k.op("dve", lambda h: h.tensor_tensor(out=kw[:], in0=kt_[:].rearrange("p (h d) -> p h d", d=64),
                                                     in1=DT[:, :, l_last:l_last + 1].to_broadcast([128, NH, 64]), op=ALU.mult),
                     reads=[bkt, B["DT"][0], B["DT"][1]], writes=[B["kw"]])
                kwp = kw[:].rearrange("p (j r) d -> p j (r d)", r=2)
                cu_banks = [(pCu, B["pCu"]),
                            (pS[:].rearrange("p a b -> p (a b)")[:, 0:3 * VW].rearrange("p (a b) -> p a b", b=VW), B["pS"]),
                            (pBM[:].rearrange("p a b -> p (a b)")[:, 0:3 * VW].rearrange("p (a b) -> p a b", b=VW), B["pBM"])]
                for hd_ in range(NH):
                    j = hd_ // 2
                    cu, bcu = cu_banks[hd_ // 3]
                    slot = hd_ % 3
                    k.op("pe", lambda h, hd_=hd_, j=j, slot=slot, cu=cu: h.matmul(out=cu[:, slot, 0:129], lhsT=kwp[:, j, :], rhs=vt_[:, hd_, 0:129],
                                                                               start=True, stop=True),
                         reads=[B["kw"], bvt], writes=[bcu])
                for hd_ in range(NH):
                    r = hd_ % 2
                    cu, bcu = cu_banks[hd_ // 3]
                    slot = hd_ % 3
                    rs = slice(r * 64, (r + 1) * 64)
                    k.op("dve", lambda h, hd_=hd_, slot=slot, rs=rs, cu=cu: h.scalar_tensor_tensor(
                        out=Cst[rs, hd_, 0:129], in0=Cst[rs, hd_, 0:129], scalar=ebL[rs, hd_:hd_ + 1], in1=cu[rs, slot, 0:129],
                        op0=ALU.mult, op1=ALU.add),
                        reads=[B["Csth"][hd_], B["ebL"][hd_ // 4], bcu], writes=[B["Csth"][hd_]])
                k.op("act", lambda h: h.copy(out=Cbf[:], in_=Cst[:]), reads=B["Csth"], writes=[B["Cbf"]])
                k.step()
                hs = hd[:].rearrange("p h d -> p (h d)")
                if not late:
                    park, bpark = (HF, BHF) if dr == 0 else (HB, BHB)
                    k.dma("sp", lambda h: h.dma_start(out=park[csl, :], in_=hs), reads=[bhd], writes=[bpark])
                    return
                pending.append((dr, c, s2))

            def epilogue(dr, c, s2):
                B = Bd[dr]
                hd, bhd = hd_d[dr][s2], B["hd"][s2]
                hfl, ogl, xl = hfl_s, ogl_s, xl_s
                csl = slice(c * 128, (c + 1) * 128)
                hs = hd[:].rearrange("p h d -> p (h d)")
                sqh = xo[:]
                other, bother = (HB, BHB) if dr == 0 else (HF, BHF)
                k.dma("sp", lambda h: h.dma_start(out=hfl[:], in_=other[csl, :]), reads=[bother], writes=[B["hfl"]])
                k.dma("sp", lambda h: h.dma_start(out=ogl[:], in_=OGS[csl, :]), reads=[BOGS], writes=[B["ogl"]])
                k.dma("sp", lambda h: h.dma_start(out=xl[:], in_=XIN[csl, :]), reads=[Bxin], writes=[B["xl"]])
                k.step()
                k.op("dve", lambda h: h.tensor_tensor(out=hs, in0=hs, in1=hfl[:], op=ALU.add), reads=[bhd, B["hfl"]], writes=[bhd])
                k.op("act", lambda h: h.activation(out=sqh, in_=hs, func=AF.Square), reads=[bhd], writes=[B["xo"]])
                k.op("dve", lambda h: h.tensor_reduce(out=ssh[:], in_=xo[:].rearrange("p (h d) -> p h d", d=128), axis=AX.X, op=ALU.add),
                     reads=[B["xo"]], writes=[B["ssh"]])
                k.op("act", lambda h: h.activation(out=ssh[:], in_=ssh[:], func=AF.Ln, bias=eps_t[:], scale=1.0 / 128),
                     reads=[B["ssh"], Beps], writes=[B["ssh"]])
                k.op("act", lambda h: h.activation(out=ssh[:], in_=ssh[:], func=AF.Exp, scale=-0.5), reads=[B["ssh"]], writes=[B["ssh"]])
                k.step()
                k.op("dve", lambda h: h.tensor_tensor(out=hd[:], in0=hd[:], in1=ssh[:, :].unsqueeze(2).to_broadcast([128, NH, 128]), op=ALU.mult),
                     reads=[bhd, B["ssh"]], writes=[bhd])
                k.op("dve", lambda h: h.tensor_tensor(out=hs, in0=hs, in1=og_bc[:], op=ALU.mult), reads=[bhd, B["og_bc"]], writes=[bhd])
                k.op("dve", lambda h: h.tensor_tensor(out=yb[:], in0=hs, in1=ogl[:], op=ALU.mult), reads=[bhd, B["ogl"]], writes=[B["yb"]])
                k.step()
                for kc in range(KC):
                    k.op("pe", lambda h, kc=kc: h.transpose(out=pA[:, kc, :], in_=yb[:, kc * 128:(kc + 1) * 128], identity=identb[:]),
                         reads=[B["yb"], Bidb], writes=[B["pA"]])
                k.op("act", lambda h: h.copy(out=ybT[:], in_=pA[:]), reads=[B["pA"]], writes=[B["ybT"]])
                for half in range(2):
                    k.step()
                    for kc in range(KC):
                        k.op("pe", lambda h, kc=kc, half=half: h.matmul(out=pY, lhsT=ybT[:, kc, :], rhs=w_out[:, kc, half * 512:(half + 1) * 512],
                                                                        start=(kc == 0), stop=(kc == KC - 1)),
                             reads=[B["ybT"], B["w_out"]], writes=[B["pY"]])
                    k.op("dve", lambda h, half=half: h.tensor_tensor(out=xo[:, half * 512:(half + 1) * 512], in0=pY,
                                                                     in1=xl[:, half * 512:(half + 1) * 512], op=ALU.add),
                         reads=[B["pY"], B["xl"]], writes=[B["xo"]])
                k.step()
                k.dma("sp", lambda h: h.dma_start(out=XOUT[csl, :], in_=xo[:]), reads=[B["xo"]], writes=[Bxout])

            pending = []

            def merge3(a, b, e):
                lists = [l for l in (a, b, e) if l]
                pos = [0] * len(lists)
                while any(p < len(l) for p, l in zip(pos, lists)):
                    best = min((i for i in range(len(lists)) if pos[i] < len(lists[i])),
                               key=lambda i: (pos[i] / len(lists[i]), i))
                    k.replay(lists[best][pos[best]])
                    pos[best] += 1

            for step in range(NT + 1):
                todo, pending = pending, []
                sa = sb_ = []
                if step < NT:
                    k.begin_record()
                    chunk(0, step, step)
                    sa = k.end_record()
                    k.begin_record()
                    chunk(1, NT - 1 - step, step)
                    sb_ = k.end_record()
                k.begin_record()
                for (dr_, c_, s2_) in todo:
                    epilogue(dr_, c_, s2_)
                    k.step()
                se = k.end_record()
                merge3(sa, sb_, se)
        k.barrier()


def _t5_bucket(rel):
    nb = 16
    ret = (rel > 0).astype(np.int32) * nb
    n = np.abs(rel)
    max_exact = nb // 2
    nf = np.maximum(n, 1).astype(np.float32)
    large = max_exact + (np.log(nf / max_exact) / math.log(128 / max_exact) * (nb - max_exact)).astype(np.int32)
    large = np.minimum(large, nb - 1)
    return ret + np.where(n < max_exact, n, large)


def _attn_tables(rel_bias):
    p = np.arange(128)[:, None, None]
    j = np.arange(3)[None, :, None]
    q = np.arange(128)[None, None, :]
    rel = j * 128 + p - 128 - q
    bucket = _t5_bucket(rel)
    bias = rel_bias[bucket]
    bias = np.ascontiguousarray(bias.transpose(0, 1, 3, 2)).astype(np.float32)
    mask = np.where(np.abs(rel) <= 128, 0.0, NEG).astype(np.float32)
    mask = np.ascontiguousarray(np.broadcast_to(mask[:, :, None, :], (128, 3, 16, 128)))
    return bias, mask


N_CORES = 4
N_STAGES = 4
DEBUG = False
ML_SKIP = set()
ML_STOP = None
CHAIN = None
LAST = {}


def build_program(n_stages=None):
    n_stages = N_STAGES if n_stages is None else n_stages
    nc = bass.Bass("TRN2", target_bir_lowering=False)
    P = {}

    def inp(name, shape, dt=F32):
        P[name] = nc.dram_tensor(name, list(shape), dt, kind="ExternalInput").ap()

    inp("x", [S, D])
    inp("attn_norm_g", [1, D])
    inp("attn_w_in", [D, 1536])
    inp("attn_q_norm_g", [1, 64])
    inp("attn_k_norm_g", [1, 64])
    inp("attn_sink", [1, 16])
    inp("attn_w_out", [D, D])
    inp("attn_bias", [128, 3, 16, 128])
    inp("attn_mask", [128, 3, 16, 128])
    inp("ident", [128, 128])
    inp("ustrict", [128, 128])
    inp("iota512", [128, CAP])
    inp("rh_const", [128, NE * NT * 4])
    inp("mlstm_norm_g", [1, D])
    inp("mlstm_w_in", [D, MIN])
    inp("mlstm_gate_bias", [1, 32])
    inp("mlstm_out_norm_g", [1, D])
    inp("mlstm_w_out", [D, D])
    inp("tri_f", [128, 128])
    inp("tri_b", [128, 128])
    inp("negm_f", [128, 128])
    inp("negm_b", [128, 128])
    inp("ffn_norm_g", [2, D])
    inp("router_w", [2, D, NE])
    inp("expert_w1", [2, NE, D, 2 * D])
    inp("expert_w3", [2, NE, D, 2 * D])
    inp("expert_w2", [2, NE, 2 * D, D])
    out = nc.dram_tensor("out", [S, D], F32, kind="ExternalOutput").ap()
    if DEBUG is True:
        for n, shp in [("dbg_aff", [128, 512]), ("dbg_sel", [128, 512]), ("dbg_pos", [128, 512]), ("dbg_lo", [128, 16]),
                       ("dbg_hi", [128, 16]), ("dbg_idx", [128, 64]), ("dbg_pis", [128, 256])]:
            P[n] = nc.dram_tensor(n, shp, F32, kind="ExternalOutput").ap()
    P["_dbg_bufs"] = []
    if DEBUG == "ml":
        for n in ["dbg_hf", "dbg_hs"]:
            P[n] = nc.dram_tensor(n, [S, D], F32, kind="ExternalOutput").ap()
    scr = {}
    for n in ["XA", "XB"]:
        scr[n] = nc.dram_tensor(n, [S, D], F32, kind="Internal").ap()
    scr["XC"] = scr["XA"]
    H = nc.dram_tensor("Hs", [S, D], BF16, kind="Internal").ap()
    P["KS"] = nc.dram_tensor("KS", [S, 512], BF16, kind="Internal").ap()
    P["VS"] = nc.dram_tensor("VS", [S, NH, VW], BF16, kind="Internal").ap()
    P["OGS"] = nc.dram_tensor("OGS", [S, D], BF16, kind="Internal").ap()
    with ExitStack() as st:
        cx = Ctx(nc, st)
        k = cx.k
        Bx = Buf("x")
        chain = [("attn", P["x"], scr["XA"]), ("moe0", scr["XA"], scr["XB"]), ("mlstm", scr["XB"], scr["XC"]),
                 ("moe1", scr["XC"], out)][:n_stages]
        if CHAIN is not None:
            chain = [(CHAIN[0], P["x"], out)]
        chain[-1] = (chain[-1][0], chain[-1][1], out)
        bin_ = Bx
        BH = Buf("H")
        for (name, src, dst) in chain:
            bout = Buf(name + "_out")
            if name == "attn":
                stage_attn(cx, src, dst, bin_, bout, P)
                k.barrier()
            elif name == "moe0":
                stage_moe(cx, src, dst, H, bin_, bout, BH, P, 0)
            elif name == "moe1":
                stage_moe(cx, src, dst, H, bin_, bout, BH, P, 1)
            elif name == "mlstm":
                P["HB"] = out if dst is not out else scr["XB"]
                stage_mlstm(cx, src, dst, bin_, bout, P)
            bin_ = bout
        k.wait_bufs("sp", [bin_] + P["_dbg_bufs"])
        k.emit()
    print("program: insts=%d waits=%d per-engine=%s" % (k.n_inst, k.n_wait, {e: len(k.prog[e]) for e in ENGS}))
    return nc


def _consts():
    ident = np.eye(128, dtype=np.float32)
    ustrict = np.triu(np.ones((128, 128), dtype=np.float32), 1)
    iota512 = np.ascontiguousarray(np.broadcast_to(np.arange(CAP, dtype=np.float32), (128, CAP)))
    rh = np.zeros((128, NE, NT, 4), dtype=np.float32)
    rh[:, :, :, 0] = np.arange(128, dtype=np.float32)[:, None, None]
    rh[:, :, :, 1] = np.arange(NT, dtype=np.float32)[None, None, :]
    tri_f = np.triu(np.ones((128, 128), dtype=np.float32), 0)
    tri_b = np.tril(np.ones((128, 128), dtype=np.float32), 0)
    negm_f = np.where(np.arange(128)[:, None] > np.arange(128)[None, :], NEG, 0.0).astype(np.float32)
    negm_b = np.where(np.arange(128)[:, None] < np.arange(128)[None, :], NEG, 0.0).astype(np.float32)
    return {"ident": ident, "ustrict": ustrict, "iota512": iota512, "rh_const": rh.reshape(128, -1),
            "tri_f": tri_f, "tri_b": tri_b, "negm_f": negm_f, "negm_b": negm_b}


def kernel(**inputs):
    inputs = {k_: np.asarray(v) for k_, v in inputs.items()}
    x = inputs["x"].astype(np.float32, copy=False)
    bias, mask = _attn_tables(inputs["rel_bias"].astype(np.float32))
    consts = _consts()
    bi, bf = inputs["mlstm_b_i"][0].astype(np.float32), inputs["mlstm_b_f"][0].astype(np.float32)
    gate_bias = np.ascontiguousarray(np.concatenate([bi[0], bf[0], bi[1], bf[1]])[None, :])
    nc = build_program()
    in_maps = []
    for c in range(N_CORES):
        m = {
            "x": np.ascontiguousarray(x[c]),
            "attn_norm_g": np.ascontiguousarray(inputs["attn_norm_g"][0:1]),
            "attn_w_in": np.ascontiguousarray(inputs["attn_w_in"][0]),
            "attn_q_norm_g": np.ascontiguousarray(inputs["attn_q_norm_g"][0:1]),
            "attn_k_norm_g": np.ascontiguousarray(inputs["attn_k_norm_g"][0:1]),
            "attn_sink": np.ascontiguousarray(inputs["attn_sink"][0:1]),
            "attn_w_out": np.ascontiguousarray(inputs["attn_w_out"][0]),
            "attn_bias": bias, "attn_mask": mask,
            "mlstm_norm_g": np.ascontiguousarray(inputs["mlstm_norm_g"][0:1]),
            "mlstm_w_in": np.ascontiguousarray(inputs["mlstm_w_in"][0]),
            "mlstm_gate_bias": gate_bias,
            "mlstm_out_norm_g": np.ascontiguousarray(inputs["mlstm_out_norm_g"][0:1]),
            "mlstm_w_out": np.ascontiguousarray(inputs["mlstm_w_out"][0]),
            "ffn_norm_g": inputs["ffn_norm_g"], "router_w": inputs["router_w"],
            "expert_w1": inputs["expert_w1"], "expert_w3": inputs["expert_w3"], "expert_w2": inputs["expert_w2"],
        }
        m.update(consts)
        in_maps.append(m)
    res = run_bass_kernel_spmd(nc, in_maps, core_ids=list(range(N_CORES)))
    LAST["res"] = res.results
    return np.stack([r["out"] for r in res.results], axis=0)
```
